# Optimizing a Trainium2 kernel written in Bass

```python
import jax, jax.numpy as jnp
from jax import lax

D_MODEL = 2048
BATCH = 4
SEQ = 4096
DEPTH = 2

CHUNK = 64
Q_BLOCK = 128
ROPE_BASE = 10000.0
EPS = 1e-6
F32 = jnp.float32

MLA_HEADS = 8
MLA_Q_RANK = 512
MLA_KV_RANK = 256
MLA_NOPE = 128
MLA_ROPE = 64
MLA_V = 128
RET_HEADS = 8
RET_DK = 128
RET_DV = 128
MIX_WIDTH = MLA_HEADS * MLA_V + RET_HEADS * RET_DV

IN_SIZES = (MLA_Q_RANK, MLA_KV_RANK, MLA_ROPE,
            RET_HEADS * RET_DK, RET_HEADS * RET_DK, RET_HEADS * RET_DV, RET_HEADS * RET_DV)
IN_WIDTH = sum(IN_SIZES)
IN_OFFSETS = tuple(sum(IN_SIZES[:i + 1]) for i in range(len(IN_SIZES) - 1))

N_GROUPS = 4
EXPERTS_PER_GROUP = 8
N_EXPERTS = N_GROUPS * EXPERTS_PER_GROUP
TOP_K = 2
D_EXPERT = 512

kernel_name = 'hybrid_mla_retention_hmoe_block'


def rms_norm(x, g):
    xf = x.astype(F32)
    y = xf * lax.rsqrt(jnp.mean(xf * xf, axis=-1, keepdims=True) + EPS)
    return (y * g.astype(F32)).astype(x.dtype)


def rope(x, pos):
    half = x.shape[-1] // 2
    inv = ROPE_BASE ** (-jnp.arange(half, dtype=F32) / half)
    ang = pos.astype(F32)[:, None] * inv[None, :]
    cos = jnp.cos(ang)[None, :, None, :]
    sin = jnp.sin(ang)[None, :, None, :]
    xf = x.astype(F32)
    x1, x2 = xf[..., :half], xf[..., half:]
    return jnp.concatenate([x1 * cos - x2 * sin, x1 * sin + x2 * cos], axis=-1).astype(x.dtype)


def chunk_block_attention(q, k, v):
    S = q.shape[1]
    scale = q.shape[-1] ** -0.5
    outs = []
    for j in range(S // Q_BLOCK):
        q0, q1 = j * Q_BLOCK, (j + 1) * Q_BLOCK
        qb, kb, vb = q[:, q0:q1], k[:, :q1], v[:, :q1]
        s = jnp.einsum('bqhd,bkhd->bhqk', qb, kb, preferred_element_type=F32) * scale
        q_chunk = (q0 + jnp.arange(Q_BLOCK)) // CHUNK
        k_chunk = jnp.arange(q1) // CHUNK
        s = jnp.where(k_chunk[None, :] <= q_chunk[:, None], s, -jnp.inf)
        p = jax.nn.softmax(s, axis=-1).astype(vb.dtype)
        outs.append(jnp.einsum('bhqk,bkhd->bqhd', p, vb))
    return jnp.concatenate(outs, axis=1)


def chunkwise_retention(q, k, v):
    B, S, H, dk = q.shape
    dv = v.shape[-1]
    N = S // CHUNK
    log_g = jnp.log1p(-jnp.exp2(-5.0 - jnp.arange(H, dtype=F32)))
    q = q.astype(F32).reshape(B, N, CHUNK, H, dk)
    k = (k.astype(F32) * dk ** -0.5).reshape(B, N, CHUNK, H, dk)
    v = v.astype(F32).reshape(B, N, CHUNK, H, dv)
    idx = jnp.arange(CHUNK, dtype=F32)
    rel = idx[:, None] - idx[None, :]
    decay = jnp.where(rel[None] >= 0, jnp.exp(jnp.maximum(rel, 0.0)[None] * log_g[:, None, None]), 0.0)
    scores = jnp.einsum('bnqhd,bnkhd->bnhqk', q, k) * decay[None, None]
    y_intra = jnp.einsum('bnhqk,bnkhe->bnqhe', scores, v)
    k_dec = k * jnp.exp((CHUNK - 1.0 - idx)[:, None] * log_g[None, :])[None, None, :, :, None]
    U = jnp.einsum('bnkhd,bnkhe->nbhde', k_dec, v)
    chunk_decay = jnp.exp(CHUNK * log_g)[None, :, None, None]

    def step(state, u):
        return state * chunk_decay + u, state

    _, S_before = lax.scan(step, jnp.zeros((B, H, dk, dv), F32), U)
    q_dec = q * jnp.exp((idx + 1.0)[:, None] * log_g[None, :])[None, None, :, :, None]
    y_inter = jnp.einsum('bnqhd,nbhde->bnqhe', q_dec, S_before)
    return (y_intra + y_inter).reshape(B, S, H, dv)


def head_group_norm(y):
    mu = jnp.mean(y, axis=-1, keepdims=True)
    var = jnp.mean(jnp.square(y - mu), axis=-1, keepdims=True)
    return (y - mu) * lax.rsqrt(var + EPS)


def hybrid_mixer(h, w_in, q_norm_g, w_uq, kv_norm_g, w_ukv, w_o):
    B, S, _ = h.shape
    pos = jnp.arange(S, dtype=jnp.int32)
    proj = h @ w_in
    c_q, c_kv, k_pe, q_r, k_r, v_r, g_r = jnp.split(proj, list(IN_OFFSETS), axis=-1)

    q = (rms_norm(c_q, q_norm_g) @ w_uq).reshape(B, S, MLA_HEADS, MLA_NOPE + MLA_ROPE)
    q_nope, q_pe = q[..., :MLA_NOPE], rope(q[..., MLA_NOPE:], pos)
    kv = (rms_norm(c_kv, kv_norm_g) @ w_ukv).reshape(B, S, MLA_HEADS, MLA_NOPE + MLA_V)
    k_nope, v_mla = kv[..., :MLA_NOPE], kv[..., MLA_NOPE:]
    k_pe = rope(k_pe[:, :, None, :], pos)
    q_mla = jnp.concatenate([q_nope, q_pe], axis=-1)
    k_mla = jnp.concatenate([k_nope, jnp.broadcast_to(k_pe, (B, S, MLA_HEADS, MLA_ROPE))], axis=-1)
    y_mla = chunk_block_attention(q_mla, k_mla, v_mla).reshape(B, S, MLA_HEADS * MLA_V)

    q_r = rope(q_r.reshape(B, S, RET_HEADS, RET_DK), pos)
    k_r = rope(k_r.reshape(B, S, RET_HEADS, RET_DK), pos)
    v_r = v_r.reshape(B, S, RET_HEADS, RET_DV)
    y_ret = head_group_norm(chunkwise_retention(q_r, k_r, v_r)).reshape(B, S, RET_HEADS * RET_DV)
    y_ret = (jax.nn.silu(g_r.astype(F32)) * y_ret).astype(h.dtype)

    return jnp.concatenate([y_mla.astype(h.dtype), y_ret], axis=-1) @ w_o


def hier_moe(h, wg, bg, we, be, w_gate, w_up, w_down):
    B, S, D = h.shape
    T = B * S
    t = h.reshape(T, D)
    g_logits = (t @ wg + bg).astype(F32)
    g_prob = jax.nn.softmax(g_logits, axis=-1)
    _, g_sel = lax.top_k(g_logits, 1)
    g_sel = g_sel[:, 0]
    g_onehot = jax.nn.one_hot(g_sel, N_GROUPS, dtype=F32)
    p_group = jnp.sum(g_prob * g_onehot, axis=-1, keepdims=True)
    e_logits = (t @ we + be).astype(F32).reshape(T, N_GROUPS, EXPERTS_PER_GROUP)
    e_in_group = jnp.einsum('tg,tge->te', g_onehot, e_logits)
    top_logit, top_idx = lax.top_k(e_in_group, TOP_K)
    top_w = jax.nn.softmax(top_logit, axis=-1) * p_group
    expert_id = g_sel[:, None] * EXPERTS_PER_GROUP + top_idx
    gates = jnp.sum(jax.nn.one_hot(expert_id, N_EXPERTS, dtype=F32) * top_w[..., None], axis=1).astype(t.dtype)
    out = jnp.zeros((T, D), t.dtype)
    for gi in range(N_GROUPS):
        e0, e1 = gi * EXPERTS_PER_GROUP, (gi + 1) * EXPERTS_PER_GROUP
        a = jnp.einsum('td,edf->tef', t, w_gate[e0:e1])
        u = jnp.einsum('td,edf->tef', t, w_up[e0:e1])
        hid = jax.nn.silu(a) * u * gates[:, e0:e1, None]
        out = out + jnp.einsum('tef,efd->td', hid, w_down[e0:e1])
    return out.reshape(B, S, D)


def setup_inputs(seed: int = 0) -> dict:
    key = jax.random.key(seed)
    ks = jax.random.split(key, 24)
    L, D = DEPTH, D_MODEL

    def nrm(k, shape, s):
        return jax.random.normal(k, shape, F32) * s

    return {
        'x': nrm(ks[0], (BATCH, SEQ, D), 1.0),
        'c': nrm(ks[1], (BATCH, D), 1.0),
        'ada_w': nrm(ks[2], (L, D, 6 * D), 0.5 * D ** -0.5),
        'ada_b': nrm(ks[3], (L, 6 * D), 0.02),
        'norm1_g': 1.0 + nrm(ks[4], (L, D), 0.02),
        'w_in': nrm(ks[5], (L, D, IN_WIDTH), D ** -0.5),
        'q_norm_g': 1.0 + nrm(ks[6], (L, MLA_Q_RANK), 0.02),
        'w_uq': nrm(ks[7], (L, MLA_Q_RANK, MLA_HEADS * (MLA_NOPE + MLA_ROPE)), MLA_Q_RANK ** -0.5),
        'kv_norm_g': 1.0 + nrm(ks[8], (L, MLA_KV_RANK), 0.02),
        'w_ukv': nrm(ks[9], (L, MLA_KV_RANK, MLA_HEADS * (MLA_NOPE + MLA_V)), MLA_KV_RANK ** -0.5),
        'w_o': nrm(ks[10], (L, MIX_WIDTH, D), MIX_WIDTH ** -0.5),
        'norm2_g': 1.0 + nrm(ks[11], (L, D), 0.02),
        'router_group_w': nrm(ks[12], (L, D, N_GROUPS), D ** -0.5),
        'router_group_b': nrm(ks[13], (L, N_GROUPS), 0.01),
        'router_expert_w': nrm(ks[14], (L, D, N_EXPERTS), D ** -0.5),
        'router_expert_b': nrm(ks[15], (L, N_EXPERTS), 0.01),
        'w_gate': nrm(ks[16], (L, N_EXPERTS, D, D_EXPERT), D ** -0.5),
        'w_up': nrm(ks[17], (L, N_EXPERTS, D, D_EXPERT), D ** -0.5),
        'w_down': nrm(ks[18], (L, N_EXPERTS, D_EXPERT, D), D_EXPERT ** -0.5),
        'final_norm_g': 1.0 + nrm(ks[19], (D,), 0.02),
    }


def reference(x, c, ada_w, ada_b, norm1_g, w_in, q_norm_g, w_uq, kv_norm_g, w_ukv, w_o,
              norm2_g, router_group_w, router_group_b, router_expert_w, router_expert_b,
              w_gate, w_up, w_down, final_norm_g):
    for l in range(DEPTH):
        mod = jax.nn.silu(c) @ ada_w[l] + ada_b[l]
        sh1, sc1, g1, sh2, sc2, g2 = [m[:, None, :] for m in jnp.split(mod, 6, axis=-1)]
        h = rms_norm(x, norm1_g[l]) * (1.0 + sc1) + sh1
        x = x + g1 * hybrid_mixer(h, w_in[l], q_norm_g[l], w_uq[l], kv_norm_g[l], w_ukv[l], w_o[l])
        h = rms_norm(x, norm2_g[l]) * (1.0 + sc2) + sh2
        x = x + g2 * hier_moe(h, router_group_w[l], router_group_b[l], router_expert_w[l],
                              router_expert_b[l], w_gate[l], w_up[l], w_down[l])
    return rms_norm(x, final_norm_g)
```

```python
import math
import numpy as np
import concourse.bass as bass
import concourse.mybir as mybir
from concourse.bass_utils import run_bass_kernel_spmd

F32 = mybir.dt.float32
BF16 = mybir.dt.bfloat16
I32 = mybir.dt.int32
ALU = mybir.AluOpType
AF = mybir.ActivationFunctionType
AX = mybir.AxisListType

ENGS = ("pe", "act", "dve", "pool", "sp")

D = 2048
S = 4096
L = 2
NT = S // 128
GT = 4
G = GT * 128
NG = S // G
MT = 8
NMP = NT // MT
INW = 4928
NE = 32
DE = 512
EPS = 1e-6
LNG = [math.log1p(-2.0 ** (-5.0 - h)) for h in range(8)]


class Buf:
    __slots__ = ("name", "last_w", "readers")

    def __init__(self, name=""):
        self.name = name
        self.last_w = None
        self.readers = {}


class Op:
    __slots__ = ("eng", "fn", "deps", "is_dma", "sem", "val", "signal", "prewait")

    def __init__(self, eng, fn, is_dma):
        self.eng = eng
        self.fn = fn
        self.deps = []
        self.is_dma = is_dma
        self.sem = None
        self.val = 0
        self.signal = False
        self.prewait = None


class Prog:
    def __init__(self, nc, n_dma_sems=48):
        self.nc = nc
        self.ops = {e: [] for e in ENGS}
        self.n_dma_sems = n_dma_sems
        self.all_ops = []

    def add(self, eng, fn, reads=(), writes=(), dma=False):
        op = Op(eng, fn, dma)
        deps = {}
        for b in reads:
            if b.last_w is not None:
                deps[id(b.last_w)] = b.last_w
        for b in writes:
            if b.last_w is not None:
                deps[id(b.last_w)] = b.last_w
            for r in b.readers.values():
                deps[id(r)] = r
        for d in deps.values():
            if d is op:
                continue
            if d.eng == "pe" and eng == "pe" and not d.is_dma and not dma:
                continue
            op.deps.append(d)
            d.signal = True
        for b in reads:
            b.readers[(eng, dma, id(op) if dma else 0)] = op
        for b in writes:
            b.last_w = op
            b.readers = {}
        self.ops[eng].append(op)
        self.all_ops.append(op)
        return op

    def dma(self, eng, out, in_, reads=(), writes=(), **kw):
        return self.add(eng, lambda e: e.dma_start(out=out, in_=in_, **kw), reads, writes, dma=True)

    def emit(self, final_wait_ops=()):
        nc = self.nc
        EPOCH = 16000
        esem = {e: [nc.alloc_semaphore(f"s_{e}0")] for e in ENGS}
        dsems = [nc.alloc_semaphore(f"d_{i}") for i in range(self.n_dma_sems)]
        dcount = [0] * self.n_dma_sems
        ecount = {e: 0 for e in ENGS}
        di = 0
        for op in self.all_ops:
            if op.is_dma:
                s = di % self.n_dma_sems
                di += 1
                op.prewait = (dsems[s], dcount[s]) if dcount[s] > 0 else None
                dcount[s] += 16
                op.sem = dsems[s]
                op.val = dcount[s]
            elif op.signal:
                if ecount[op.eng] >= EPOCH:
                    esem[op.eng].append(nc.alloc_semaphore(f"s_{op.eng}{len(esem[op.eng])}"))
                    ecount[op.eng] = 0
                ecount[op.eng] += 1
                op.sem = esem[op.eng][-1]
                op.val = ecount[op.eng]
        final_wait_ops = list(final_wait_ops)

        def run(eng_name, e):
            known = {}
            for op in self.ops[eng_name]:
                waits = {}
                if op.prewait is not None:
                    waits[id(op.prewait[0])] = op.prewait
                for d in op.deps:
                    k = id(d.sem)
                    if k not in waits or waits[k][1] < d.val:
                        waits[k] = (d.sem, d.val)
                for k, (s, v) in waits.items():
                    if known.get(k, 0) >= v:
                        continue
                    e.wait_ge(s, v)
                    known[k] = v
                ins = op.fn(e)
                if op.is_dma:
                    ins.then_inc(op.sem, 16)
                elif op.signal:
                    ins.then_inc(op.sem, 1)
            if eng_name == "sp":
                for d in final_wait_ops:
                    e.wait_ge(d.sem, d.val)

        with nc.Block() as block:
            @block.tensor
            def _(e):
                run("pe", e)

            @block.scalar
            def _(e):
                run("act", e)

            @block.vector
            def _(e):
                run("dve", e)

            @block.gpsimd
            def _(e):
                run("pool", e)

            @block.sync
            def _(e):
                run("sp", e)


class Builder:
    def __init__(self, debug=(), stop=None, n_layers=L, small=False):
        self.small = small
        self.nc = nc = bass.Bass("TRN2", target_bir_lowering=False)
        self.P = Prog(nc)
        self.debug = set(debug)
        self.stop = stop
        self.n_layers = n_layers
        self.dbg_ops = []
        self.sb_base = 16512
        self.sb_top = 229376
        self.uid = 0
        self.inputs = {}
        self.build()

    def din(self, name, shape, dt=F32):
        if self.small and name in ("w_gate", "w_up", "w_down"):
            shape = [1, 1, 1, 1]
        t = self.nc.dram_tensor(name, list(shape), dt, kind="ExternalInput").ap()
        self.inputs[name] = t
        return t

    def dscr(self, name, shape, dt=F32):
        kind = "ExternalOutput" if name in self.debug else "Internal"
        return self.nc.dram_tensor(name, list(shape), dt, kind=kind).ap()

    def sb(self, off, shape, dt=F32, name=None):
        self.uid += 1
        n = f"{name or 't'}_{self.uid}"
        esz = 4 if dt in (F32, I32) else 2
        nbytes = int(np.prod(shape[1:])) * esz
        assert off + nbytes <= self.sb_top, (name, off, nbytes)
        t = self.nc.alloc_sbuf_tensor_at(n, list(shape), dt, offset=off)
        return t

    class Region:
        def __init__(self, bld, start, end):
            self.b, self.start, self.end, self.cur = bld, start, end, start
            self.bufs = []

        def alloc(self, shape, dt=F32, name=None):
            esz = 4 if dt in (F32, I32) else 2
            nbytes = int(np.prod(shape[1:])) * esz
            nbytes = (nbytes + 63) // 64 * 64
            off = self.cur
            assert off + nbytes <= self.end, (name, off, nbytes, self.end)
            self.cur += nbytes
            t = self.b.sb(off, shape, dt, name)
            buf = Buf(name or "")
            self.bufs.append(buf)
            return t, buf

    def barrier(self, old_bufs, new_bufs):
        op = self.P.add("dve", lambda e: e.engine_nop(), reads=[], writes=list(old_bufs))
        for b in new_bufs:
            b.last_w = op
            b.readers = {}
        op.signal = True

    def A(self, eng, fn, r, w):
        return self.P.add(eng, fn, r, w)

    def MM(self, out, lhsT, rhs, start, stop, r, w):
        return self.P.add("pe", lambda e: e.matmul(out, lhsT, rhs, start=start, stop=stop), r, w)

    def TR(self, out, in_, ident, r, w):
        return self.P.add("pe", lambda e: e.transpose(out, in_, ident), r, w)

    def ACT(self, out, in_, func, r, w, **kw):
        return self.P.add("act", lambda e: e.activation(out=out, in_=in_, func=func, **kw), r, w)

    def TT(self, eng, out, in0, in1, op, r, w):
        return self.P.add(eng, lambda e: e.tensor_tensor(out=out, in0=in0, in1=in1, op=op), r, w)

    def TS(self, eng, out, in0, s1, s2, op0, op1, r, w):
        if s2 is None:
            return self.P.add(eng, lambda e: e.tensor_scalar(out=out, in0=in0, scalar1=s1, scalar2=None, op0=op0), r, w)
        return self.P.add(eng, lambda e: e.tensor_scalar(out=out, in0=in0, scalar1=s1, scalar2=s2, op0=op0, op1=op1), r, w)

    def STT(self, eng, out, in0, scalar, in1, op0, op1, r, w):
        return self.P.add(eng, lambda e: e.scalar_tensor_tensor(out=out, in0=in0, scalar=scalar, in1=in1, op0=op0, op1=op1), r, w)

    def CP(self, eng, out, in_, r, w):
        if eng == "act":
            return self.ACT(out, in_, AF.Copy, r, w)
        return self.P.add(eng, lambda e: e.tensor_copy(out=out, in_=in_), r, w)

    def MS(self, eng, ap, val, w):
        return self.P.add(eng, lambda e: e.memset(ap, val), [], w)

    def DMA(self, eng, out, in_, r, w, **kw):
        return self.P.dma(eng, out, in_, r, w, **kw)

    def dbg(self, name, ap, shape, dt, bufs):
        if name not in self.debug:
            return
        o = self.nc.dram_tensor("dbg_" + name, list(shape), dt, kind="ExternalOutput").ap()
        self.dbg_ops.append(self.DMA("sp", o, ap, bufs, [Buf()]))

    def plan_load(self, src_view, view_fn):
        ent = {"src": src_view, "fn": view_fn, "res": None}
        self.wplan.append(ent)
        return ent

    def get_load(self, ent, lookahead=2):
        idx = self.wplan.index(ent)
        for e in self.wplan[:idx + 1 + lookahead]:
            if e["res"] is None:
                e["res"] = self.ring_load(e["src"], e["fn"])
        return ent["res"]

    def ring_load(self, src_view, view_fn):
        i = self.ring_i % len(self.ring)
        self.ring_i += 1
        t, b = self.ring[i]
        v = view_fn(t)
        self.DMA("pool", v, src_view, [self.wsrc], [b])
        return v, b

    def build(self):
        nc = self.nc
        P = self.P
        self.wsrc = Buf("weights")
        x_in = self.din("x", [S, D])
        c_col = self.din("c_col", [128, 16])
        ada_w = self.din("ada_w", [L, D, 6 * D])
        ada_b = self.din("ada_b", [L, 6 * D])
        norm1_g = self.din("norm1_g", [L, D])
        w_in = self.din("w_in", [L, D, INW])
        q_norm_g = self.din("q_norm_g", [L, 512])
        w_uq = self.din("w_uq", [L, 512, 1536])
        kv_norm_g = self.din("kv_norm_g", [L, 256])
        w_ukv = self.din("w_ukv", [L, 256, 2048])
        w_o = self.din("w_o", [L, D, D])
        norm2_g = self.din("norm2_g", [L, D])
        rgw = self.din("router_group_w", [L, D, 4])
        rgb = self.din("router_group_b", [L, 4])
        rew = self.din("router_expert_w", [L, D, 32])
        reb = self.din("router_expert_b", [L, 32])
        w_gate = self.din("w_gate", [L, NE, D, DE])
        w_up = self.din("w_up", [L, NE, D, DE])
        w_down = self.din("w_down", [L, NE, DE, D])
        fng = self.din("final_norm_g", [D])
        flag_in = self.din("flag", [128, 2])
        HS = S // 2
        out_d = nc.dram_tensor("out", [HS, D], F32, kind="ExternalOutput").ap()
        xsel_d = self.dscr("xsel_d", [HS, D])
        xsel_b = [Buf(f"xsel{t}") for t in range(NT // 2)]

        xres = self.dscr("xres", [S, D])
        xres_b = [Buf(f"xres{t}") for t in range(NT)]
        mod_d = self.dscr("mod_d", [L, 6 * D])
        mod_b = Buf("mod_d")
        cosF_d = self.dscr("cosF_d", [64, S]); sinF_d = self.dscr("sinF_d", [64, S])
        cosR_d = self.dscr("cosR_d", [128, NT, 64]); sinR_d = self.dscr("sinR_d", [128, NT, 64])
        tab_b = Buf("tables")
        kT_d = self.dscr("kT_d", [8, 128, S], BF16)
        kpe_d = self.dscr("kpe_d", [64, S], BF16)
        v_d = self.dscr("v_d", [8, 128, NT, 128], BF16)
        kv_b = [Buf(f"kv{g}") for g in range(NG)]

        R0 = self.Region(self, self.sb_base, self.sb_top)
        self.ring = []
        for i in range(4):
            t, b = R0.alloc([128, 8192], BF16, f"ring{i}")
            self.ring.append((t, b))
        self.ring_i = 0
        self.wplan = []
        identb, cb_ = R0.alloc([128, 128], BF16, "identb")
        identf, _ = R0.alloc([128, 128], F32, "identf")
        maskT, _ = R0.alloc([128, 128], F32, "maskT")
        maskD, _ = R0.alloc([128, 128], F32, "maskD")
        Gq, _ = R0.alloc([128, 8, 128], F32, "Gq")
        gk_col, _ = R0.alloc([128, 8], F32, "gk_col")
        g128, _ = R0.alloc([128, 8], F32, "g128")
        cst = Buf("consts")
        vecs, vecs_b = R0.alloc([128, 104], F32, "vecs")
        sc1c, _ = R0.alloc([128, 16], F32, "sc1c")
        sc2c, _ = R0.alloc([128, 16], F32, "sc2c")
        wr, wr_b = R0.alloc([128, 16, 36], F32, "wr")
        rbias, _ = R0.alloc([128, 36], F32, "rbias")
        Tst, Tst_b = R0.alloc([128, 8, 128], F32, "Tstate")
        gb, gb_b = R0.alloc([128, D], F32, "gb")
        silc, silc_b = R0.alloc([128, 16], BF16, "silc")
        flg, _ = R0.alloc([128, 2], F32, "flg")
        persist_end = R0.cur
        ps = [nc.alloc_psum_tensor(f"ps{i}", [128, 512], F32) for i in range(8)]
        psb = [Buf(f"ps{i}") for i in range(8)]
        self.ps, self.psb = ps, psb

        def psbf(i):
            return ps[i][:].bitcast(BF16)

        RC = self.Region(self, persist_end, self.sb_top)
        io_i, tb = RC.alloc([128, 128], I32, "io_i")
        io_f, _ = RC.alloc([128, 128], F32, "io_f")
        self.A("pool", lambda e: e.iota(io_i[:], pattern=[[1, 128]], base=0, channel_multiplier=-1), [], [tb])
        self.CP("dve", io_f[:], io_i[:], [tb], [tb])
        self.A("dve", lambda e: e.tensor_single_scalar(out=identf[:], in_=io_f[:], scalar=0.0, op=ALU.is_equal), [tb], [cst])
        self.CP("dve", identb[:], identf[:], [cst], [cst])
        self.A("dve", lambda e: e.tensor_single_scalar(out=maskT[:], in_=io_f[:], scalar=0.0, op=ALU.is_ge), [tb], [cst])
        self.MS("pool", maskD[:], 0.0, [cst])
        self.DMA("sp", flg[:], flag_in, [self.wsrc], [cst])
        self.MS("pool", maskD[0:64, 64:128], -30000.0, [cst])
        pp_i, _ = RC.alloc([128, 1], I32, "pp_i")
        pp1, _ = RC.alloc([128, 1], F32, "pp1")
        self.A("pool", lambda e: e.iota(pp_i[:], pattern=[[0, 1]], base=1, channel_multiplier=1), [], [tb])
        self.CP("dve", pp1[:], pp_i[:], [tb], [tb])
        lnrow, _ = RC.alloc([128, 8], F32, "lnrow")
        for h in range(8):
            self.MS("pool", lnrow[:, h:h + 1], LNG[h], [tb])
            self.MS("pool", g128[:, h:h + 1], math.exp(128.0 * LNG[h]), [cst])
        arg, _ = RC.alloc([128, 8], F32, "arg")
        self.TS("dve", arg[:], lnrow[:], pp1[:], None, ALU.mult, None, [tb], [tb])
        self.ACT(gk_col[:], arg[:], AF.Exp, [tb], [cst], scale=-1.0)
        self.TS("dve", gk_col[:], gk_col[:], 128.0 ** -0.5, None, ALU.mult, None, [cst], [cst])
        nrow_i, _ = RC.alloc([128, 128], I32, "nrow_i")
        nrow, _ = RC.alloc([128, 128], F32, "nrow")
        self.A("pool", lambda e: e.iota(nrow_i[:], pattern=[[1, 128]], base=1, channel_multiplier=0), [], [tb])
        self.CP("dve", nrow[:], nrow_i[:], [tb], [tb])
        for h in range(8):
            self.ACT(Gq[:, h, :], nrow[:], AF.Exp, [tb], [cst], scale=LNG[h])

        def sincos(ang, shape, sin_out, cos_out, tmpf, tmpi, tmpr):
            for (dst, shift) in ((sin_out, 0.0), (cos_out, math.pi / 2)):
                self.TS("dve", tmpf, ang, 1.0 / (2 * math.pi), shift / (2 * math.pi), ALU.mult, ALU.add, [tb], [tb])
                self.CP("dve", tmpi, tmpf, [tb], [tb])
                self.CP("dve", tmpf, tmpi, [tb], [tb])
                if shift:
                    self.TS("dve", tmpr, ang, shift, None, ALU.add, None, [tb], [tb])
                    self.STT("dve", tmpr, tmpf, -2 * math.pi, tmpr, ALU.mult, ALU.add, [tb], [tb])
                else:
                    self.STT("dve", tmpr, tmpf, -2 * math.pi, ang, ALU.mult, ALU.add, [tb], [tb])
                self.TS("dve", tmpr, tmpr, 3.14159, -3.14159, ALU.min, ALU.max, [tb], [tb])
                self.ACT(dst, tmpr, AF.Sin, [tb], [tb])

        pidx_i, _ = RC.alloc([128, 1], I32, "pidx_i")
        pidx, _ = RC.alloc([128, 1], F32, "pidx")
        self.A("pool", lambda e: e.iota(pidx_i[:], pattern=[[0, 1]], base=0, channel_multiplier=1), [], [tb])
        self.CP("dve", pidx[:], pidx_i[:], [tb], [tb])
        ge32, _ = RC.alloc([128, 1], F32, "ge32")
        self.A("dve", lambda e: e.tensor_single_scalar(out=ge32[:], in_=pidx[:], scalar=32.0, op=ALU.is_ge), [tb], [tb])
        self.STT("dve", pidx[:], ge32[:], -32.0, pidx[:], ALU.mult, ALU.add, [tb], [tb])
        invc, _ = RC.alloc([128, 1], F32, "invc")
        self.ACT(invc[:], pidx[:], AF.Exp, [tb], [tb], scale=-math.log(10000.0) / 32.0)
        CH = 1024
        tok_i, _ = RC.alloc([64, CH], I32, "tok_i")
        tokf, _ = RC.alloc([64, CH], F32, "tokf")
        angF, _ = RC.alloc([64, CH], F32, "angF")
        tmpf, _ = RC.alloc([64, CH], F32, "tmpf")
        tmpi, _ = RC.alloc([64, CH], I32, "tmpi")
        tmpr, _ = RC.alloc([64, CH], F32, "tmpr")
        sinc, _ = RC.alloc([64, CH], F32, "sinc")
        cosc, _ = RC.alloc([64, CH], F32, "cosc")
        for ci in range(S // CH):
            self.A("pool", lambda e, ci=ci: e.iota(tok_i[:], pattern=[[1, CH]], base=ci * CH, channel_multiplier=0), [], [tb])
            self.CP("dve", tokf[:], tok_i[:], [tb], [tb])
            self.TS("dve", angF[:], tokf[:], invc[0:64, :], None, ALU.mult, None, [tb], [tb])
            sincos(angF[:], None, sinc[:], cosc[:], tmpf[:], tmpi[:], tmpr[:])
            self.DMA("sp", sinF_d[:, ci * CH:(ci + 1) * CH], sinc[:], [tb], [tab_b])
            self.DMA("sp", cosF_d[:, ci * CH:(ci + 1) * CH], cosc[:], [tb], [tab_b])
        RC2 = self.Region(self, persist_end, self.sb_top)
        tb2 = Buf("tb2")
        self.barrier([tb], [tb2])
        tb = tb2
        tokc_i, _ = RC2.alloc([128, NT], I32, "tokc_i")
        tokc, _ = RC2.alloc([128, NT], F32, "tokc")
        self.A("pool", lambda e: e.iota(tokc_i[:], pattern=[[128, NT]], base=0, channel_multiplier=1), [], [tb])
        self.CP("dve", tokc[:], tokc_i[:], [tb], [tb])
        jr_i, _ = RC2.alloc([128, 64], I32, "jr_i")
        jr, _ = RC2.alloc([128, 64], F32, "jr")
        self.A("pool", lambda e: e.iota(jr_i[:], pattern=[[1, 64]], base=0, channel_multiplier=0), [], [tb])
        self.CP("dve", jr[:], jr_i[:], [tb], [tb])
        invr, _ = RC2.alloc([128, 64], F32, "invr")
        self.ACT(invr[:], jr[:], AF.Exp, [tb], [tb], scale=-math.log(10000.0) / 64.0)
        angR, _ = RC2.alloc([128, NT, 64], F32, "angR")
        self.TT("dve", angR[:], tokc[:].unsqueeze(2).broadcast_to([128, NT, 64]),
                invr[:].unsqueeze(1).broadcast_to([128, NT, 64]), ALU.mult, [tb], [tb])
        tmpf2, _ = RC2.alloc([128, NT, 64], F32, "tmpf2")
        tmpi2, _ = RC2.alloc([128, NT, 64], I32, "tmpi2")
        tmpr2, _ = RC2.alloc([128, NT, 64], F32, "tmpr2")
        sinR, _ = RC2.alloc([128, NT, 64], F32, "sinR")
        cosR, _ = RC2.alloc([128, NT, 64], F32, "cosR")
        sincos(angR[:], None, sinR[:], cosR[:], tmpf2[:], tmpi2[:], tmpr2[:])
        self.DMA("sp", sinR_d, sinR[:], [tb], [tab_b])
        self.DMA("sp", cosR_d, cosR[:], [tb], [tab_b])

        cc, _ = RC2.alloc([128, 16], F32, "cc")
        self.DMA("sp", cc[:], c_col, [self.wsrc], [tb])
        self.ACT(silc[:], cc[:], AF.Silu, [tb], [silc_b])
        brow = [RC2.alloc([1, 512], F32, f"brow{i}") for i in range(2)]
        mrow = [RC2.alloc([1, 512], F32, f"mrow{i}") for i in range(2)]
        nblk = 0
        for l in range(self.n_layers):
            for cbk in range(24):
                c0 = cbk * 512
                wv, wb_ = self.ring_load(
                    ada_w[l, :, c0:c0 + 512].rearrange("(k p) c -> p k c", p=128),
                    lambda t: t[:].rearrange("p (k c) -> p k c", k=16))
                br, brb = brow[nblk % 2]
                mr, mrb = mrow[nblk % 2]
                pi = nblk % 2
                nblk += 1
                self.DMA("sp", br[:], ada_b[l:l + 1, c0:c0 + 512], [self.wsrc], [brb])
                for k in range(16):
                    self.MM(ps[pi][0:1, :], silc[:, k:k + 1], wv[:, k, :], k == 0, k == 15, [silc_b, wb_], [psb[pi]])
                self.TT("dve", mr[:], ps[pi][0:1, :], br[:], ALU.add, [psb[pi], brb], [mrb])
                self.DMA("sp", mod_d[l:l + 1, c0:c0 + 512], mr[:], [mrb], [mod_b])
        self.dbg("mod", None, None, None, None)
        if self.stop == "mod":
            return self.finish([mod_b], out_d)

        idn = identb
        RM = self.Region(self, persist_end, self.sb_top)
        yT, yT_b = RM.alloc([128, 16, G], BF16, "yT")
        xn_g = self.sb(persist_end, [128, GT, D], BF16, "xn_g")
        cqnT, cqnT_b = RM.alloc([128, 4, G], BF16, "cqnT")
        cosF_g, ropeF_b = RM.alloc([64, G], F32, "cosF_g")
        sinF_g, _ = RM.alloc([64, G], F32, "sinF_g")
        cosR_g, ropeR_b = RM.alloc([128, GT, 64], F32, "cosR_g")
        sinR_g, _ = RM.alloc([128, GT, 64], F32, "sinR_g")
        nsinR_g, _ = RM.alloc([128, GT, 64], F32, "nsinR_g")
        ov0 = RM.cur
        RA = self.Region(self, ov0, self.sb_top)
        hT, hT_b = RA.alloc([128, 16, G], BF16, "hT")
        xt, xt_b = RA.alloc([128, D], F32, "xt")
        st = [RA.alloc([128, 1], F32, f"st{i}") for i in range(12)]
        junk, junk_b = RA.alloc([128, 512], BF16, "junk")
        cqn_g, cqn_b = RA.alloc([128, GT, 512], BF16, "cqn_g")
        ckvn_g, ckvn_b = RA.alloc([128, GT, 256], BF16, "ckvn_g")
        ckvnT, ckvnT_b = RA.alloc([128, 2, G], BF16, "ckvnT")
        kperot, kperot_b = RA.alloc([128, 16, 64], BF16, "kperot")
        kt1, kt1_b = RA.alloc([64, G], F32, "kt1")
        kt2, kt2_b = RA.alloc([64, G], F32, "kt2")
        kpeo, kpeo_b = RA.alloc([64, G], BF16, "kpeo")
        wukv, wukv_b = RA.alloc([128, 2, 2048], BF16, "wukv")
        kTo = [RA.alloc([128, G], BF16, f"kTo{i}") for i in range(2)]
        vo = [RA.alloc([128, 1024], BF16, f"vo{i}") for i in range(2)]
        q_r, q_r_b = RA.alloc([128, GT, 512], BF16, "q_r")
        k_p, k_p_b = RA.alloc([128, GT, 512], BF16, "k_p")
        v_r, v_r_b = RA.alloc([128, GT, 512], BF16, "v_r")
        g_r, g_r_b = RA.alloc([128, GT, 512], F32, "g_r")
        rt1 = [RA.alloc([128, 512], F32, f"rt1_{i}") for i in range(2)]
        rt2 = [RA.alloc([128, 512], F32, f"rt2_{i}") for i in range(1)] * 2
        rt3 = [RA.alloc([128, 512], F32, f"rt3_{i}") for i in range(1)] * 2
        qT4, qT4_b = RA.alloc([128, 4, 128], BF16, "qT4")
        kT4, kT4_b = RA.alloc([128, 4, 128], BF16, "kT4")
        sT4, sT4_b = RA.alloc([128, 4, 128], BF16, "sT4")
        Sf, Sf_b = RA.alloc([128, 4, 128], F32, "Sf")
        Sb, Sb_b = RA.alloc([128, 4, 128], BF16, "Sb")
        ysq, ysq_b = RA.alloc([128, 4, 128], F32, "ysq")
        yc, yc_b = RA.alloc([128, 4, 128], F32, "yc")
        yg, yg_b = RA.alloc([128, 512], BF16, "yg")
        s4 = [RA.alloc([128, 4], F32, f"s4_{i}") for i in range(8)]
        RB = self.Region(self, ov0, self.sb_top)
        stash, stash_b = RB.alloc([128, S], F32, "stash")
        kTh, kTh_b = RB.alloc([128, S], BF16, "kTh")
        vh, vh_b = RB.alloc([128, NT, 128], BF16, "vh")
        kpeT, kpeT_b = RB.alloc([64, S], BF16, "kpeT")
        wuq, wuq_b = RB.alloc([128, 4, 1536], BF16, "wuq")
        wrot, wrot_b = RB.alloc([128, 4, 8, 64], BF16, "wrot")
        qTh, qTh_b = RB.alloc([128, G], BF16, "qTh")
        qpeTh, qpeTh_b = RB.alloc([64, G], BF16, "qpeTh")
        qt1, qt1_b = RB.alloc([64, G], F32, "qt1")
        qt2, qt2_b = RB.alloc([64, G], F32, "qt2")
        Pb = [RB.alloc([128, 512], BF16, f"Pb{i}") for i in range(2)]
        PT = [RB.alloc([128, 4, 128], BF16, f"PT{i}") for i in range(2)]
        ytok, ytok_b = RB.alloc([128, GT, 1024], BF16, "ytok")
        bst = [RB.alloc([128, 1], F32, f"bst{i}") for i in range(6)]
        rs8 = [RB.alloc([128, 8], F32, f"rs8_{i}") for i in range(2)]
        RCo = self.Region(self, ov0, self.sb_top)
        xc = [RCo.alloc([128, 512], F32, f"xc{i}") for i in range(2)]
        xtmp = [RCo.alloc([128, 512], F32, f"xtmp{i}") for i in range(2)]
        xo = [RCo.alloc([128, 512], F32, f"xo{i}") for i in range(2)]
        RD = self.Region(self, persist_end, self.sb_top)
        h2T, h2T_b = RD.alloc([128, 16, MT * 128], BF16, "h2T")
        gates, gates_b = RD.alloc([128, MT, 32], F32, "gates")
        sa = [RD.alloc([128, 512], F32, f"sa{i}") for i in range(2)]
        hid = [RD.alloc([128, 512], BF16, f"hid{i}") for i in range(2)]
        hidT, hidT_b0 = RD.alloc([128, 4, MT * 128], BF16, "hidT")
        hidT_bs = [hidT_b0] + [Buf(f"hidT{i}") for i in range(1, MT)]
        RD.bufs.extend(hidT_bs[1:])
        dst = [RD.alloc([128, 1], F32, f"dst{i}") for i in range(16)]
        lg, lg_b = RD.alloc([128, 36], F32, "lg")
        rk = [RD.alloc([128, 32], F32, f"rk{i}") for i in range(6)]
        xc2 = [RD.alloc([128, 512], F32, f"xc2_{i}") for i in range(2)]
        xc3 = [RD.alloc([128, 512], F32, f"xc3_{i}") for i in range(2)]
        acc_off = RD.cur
        acc, acc_b = RD.alloc([128, MT, D], F32, "acc")
        RD2 = self.Region(self, acc_off, self.sb_top)
        xt2, xt2_b = RD2.alloc([128, D], F32, "xt2")
        xn2, xn2_b = RD2.alloc([128, D], F32, "xn2")
        h2f, h2f_b = RD2.alloc([128, 16, 128], F32, "h2f")
        xt2b, xt2b_b = RD2.alloc([128, D], F32, "xt2b")
        if self.debug:
            print("SBUF map: persist_end", persist_end, "ov0", ov0, "A", RA.cur, "B", RB.cur, "C", RCo.cur, "D", RD.cur, "D2", RD2.cur)

        def rmsnorm_stats(src_ap, n, junk_ap, r, junk_w, sti):
            (ssq, ssq_b), (std, std_b), (rstd, rstd_b) = st[sti], st[sti + 1], st[sti + 2]
            self.ACT(junk_ap, src_ap, AF.Square, r, junk_w + [ssq_b], accum_out=ssq[:])
            self.ACT(std[:], ssq[:], AF.Sqrt, [ssq_b], [std_b], scale=1.0 / n, bias=EPS)
            self.A("dve", lambda e: e.reciprocal(out=rstd[:], in_=std[:]), [std_b], [rstd_b])
            return rstd, rstd_b

        alt = [0]

        def evac(out, in_, r, w):
            alt[0] ^= 1
            if alt[0]:
                return self.ACT(out, in_, AF.Copy, r, w)
            return self.CP("dve", out, in_, r, w)

        cur_overlay = [list(RC2.bufs) + [tb]]

        def switch(new_bufs):
            self.barrier(cur_overlay[0], new_bufs)
            cur_overlay[0] = list(new_bufs)

        final_ops = []
        for l in range(self.n_layers):
            last_layer = (l == self.n_layers - 1)
            RS_b = Buf("rows")
            switch([RS_b])
            rows = self.sb(ov0, [128, 128], F32, "rows")
            self.MS("pool", rows[:], 0.0, [RS_b])
            srcs = [(mod_d[l, 0:D], 16, [mod_b]), (mod_d[l, D:2 * D], 16, [mod_b]), (mod_d[l, 3 * D:4 * D], 16, [mod_b]),
                    (mod_d[l, 4 * D:5 * D], 16, [mod_b]), (norm1_g[l], 16, [self.wsrc]), (norm2_g[l], 16, [self.wsrc]),
                    (q_norm_g[l], 4, [self.wsrc]), (kv_norm_g[l], 2, [self.wsrc])]
            r0 = 0
            for (src, n, rb_) in srcs:
                self.DMA("sp", rows[r0:r0 + n, :], src.rearrange("(k c) -> k c", c=128), rb_, [RS_b])
                r0 += n
            self.TR(ps[0][:, 0:128], rows[:, :], identf[:], [RS_b, cst], [psb[0]])
            self.CP("dve", vecs[:, 0:104], ps[0][:, 0:104], [psb[0]], [vecs_b])
            sh1c, sh2c, qng, kvng = vecs[:, 0:16], vecs[:, 32:48], vecs[:, 96:100], vecs[:, 100:102]
            self.STT("dve", sc1c[:], vecs[:, 16:32], 1.0, vecs[:, 64:80], ALU.add, ALU.mult, [vecs_b], [vecs_b])
            self.STT("dve", sc2c[:], vecs[:, 48:64], 1.0, vecs[:, 80:96], ALU.add, ALU.mult, [vecs_b], [vecs_b])
            self.DMA("sp", wr[:, :, 0:4], rgw[l].rearrange("(k p) g -> p k g", p=128), [self.wsrc], [wr_b])
            self.DMA("sp", wr[:, :, 4:36], rew[l].rearrange("(k p) g -> p k g", p=128), [self.wsrc], [wr_b])
            self.DMA("sp", rbias[:, 0:4], rgb[l:l + 1, :].partition_broadcast(128), [self.wsrc], [wr_b])
            self.DMA("sp", rbias[:, 4:36], reb[l:l + 1, :].partition_broadcast(128), [self.wsrc], [wr_b])
            self.MS("pool", Tst[:], 0.0, [Tst_b])
            xsrc = x_in if l == 0 else xres

            def moe_pass(mp, sel):
                switch(RD.bufs[:-1] + RD2.bufs)
                for t in range(MT):
                    tile = mp * MT + t
                    self.DMA("sp", xt2[:], xres[tile * 128:(tile + 1) * 128, :], [xres_b[tile]], [xt2_b])
                    if sel:
                        tileB = NT // 2 + tile
                        self.DMA("sp", xt2b[:], xres[tileB * 128:(tileB + 1) * 128, :], [xres_b[tileB]], [xt2b_b])
                        self.TS("pool", xt2b[:], xt2b[:], flg[:, 1:2], None, ALU.mult, None, [xt2b_b, cst], [xt2b_b])
                        self.STT("dve", xt2[:], xt2[:], flg[:, 0:1], xt2b[:], ALU.mult, ALU.add, [xt2_b, xt2b_b, cst], [xt2_b])
                    (ssq, ssq_b), (std, std_b), (rstd, rstd_b) = dst[0:3]
                    self.ACT(xn2[:], xt2[:], AF.Square, [xt2_b], [xn2_b, ssq_b], accum_out=ssq[:])
                    self.ACT(std[:], ssq[:], AF.Sqrt, [ssq_b], [std_b], scale=1.0 / D, bias=EPS)
                    self.A("dve", lambda e, rstd=rstd, std=std: e.reciprocal(out=rstd[:], in_=std[:]), [std_b], [rstd_b])
                    self.ACT(xn2[:], xt2[:], AF.Copy, [xt2_b, rstd_b], [xn2_b], scale=rstd[:])
                    for k in range(16):
                        bk = k // 4
                        self.TR(ps[bk][:, (k % 4) * 128:(k % 4 + 1) * 128], xn2[:, k * 128:(k + 1) * 128], identf[:], [xn2_b, cst], [psb[bk]])
                        if k % 2 == 0:
                            self.ACT(h2f[:, k, :], ps[bk][:, (k % 4) * 128:(k % 4 + 1) * 128], AF.Identity, [psb[bk], vecs_b], [h2f_b], scale=sc2c[:, k:k + 1], bias=sh2c[:, k:k + 1])
                        else:
                            self.TS("dve", h2f[:, k, :], ps[bk][:, (k % 4) * 128:(k % 4 + 1) * 128], sc2c[:, k:k + 1], sh2c[:, k:k + 1], ALU.mult, ALU.add, [psb[bk], vecs_b], [h2f_b])
                    self.CP("pool", h2T[:, :, t * 128:(t + 1) * 128], h2f[:], [h2f_b], [h2T_b])
                    for k in range(16):
                        self.MM(ps[4][:, 0:36], h2f[:, k, :], wr[:, k, :], k == 0, k == 15, [h2f_b, wr_b], [psb[4]])
                    self.TT("dve", lg[:], ps[4][:, 0:36], rbias[:], ALU.add, [psb[4], wr_b], [lg_b])
                    (gmax, gmaxb), (ngmax, ngmaxb), (gsum, gsumb), (pgrp, pgrpb), (m1, m1b), (m2, m2b), (dd, ddb), (ee, eeb), (w1, w1b), (w2, w2b) = dst[3:13]
                    (eg, egb), (oh, ohb), (me, meb), (k1, k1b), (me2, me2b), (k2, k2b) = rk
                    self.A("dve", lambda e, gmax=gmax: e.reduce_max(out=gmax[:], in_=lg[:, 0:4], axis=AX.X), [lg_b], [gmaxb])
                    self.TS("dve", ngmax[:], gmax[:], -1.0, None, ALU.mult, None, [gmaxb], [ngmaxb])
                    self.ACT(eg[:, 0:4], lg[:, 0:4], AF.Exp, [lg_b, ngmaxb], [egb, gsumb], bias=ngmax[:], accum_out=gsum[:])
                    self.A("dve", lambda e, pgrp=pgrp, gsum=gsum: e.reciprocal(out=pgrp[:], in_=gsum[:]), [gsumb], [pgrpb])
                    self.TS("dve", oh[:, 0:4], lg[:, 0:4], gmax[:], None, ALU.is_ge, None, [lg_b, gmaxb], [ohb])
                    self.TS("dve", oh[:, 0:4], oh[:, 0:4], -1.0, 1e30, ALU.add, ALU.mult, [ohb], [ohb])
                    self.TT("dve", me[:].rearrange("p (g e) -> p g e", g=4), lg[:, 4:36].rearrange("p (g e) -> p g e", g=4),
                            oh[:, 0:4].unsqueeze(2).broadcast_to([128, 4, 8]), ALU.add, [lg_b, ohb], [meb])
                    self.A("dve", lambda e, m1=m1, me=me: e.reduce_max(out=m1[:], in_=me[:], axis=AX.X), [meb], [m1b])
                    self.TS("dve", k1[:], me[:], m1[:], None, ALU.is_ge, None, [meb, m1b], [k1b])
                    self.STT("dve", me2[:], k1[:], -1e30, me[:], ALU.mult, ALU.add, [k1b, meb], [me2b])
                    self.A("dve", lambda e, m2=m2, me2=me2: e.reduce_max(out=m2[:], in_=me2[:], axis=AX.X), [me2b], [m2b])
                    self.TS("dve", k2[:], me2[:], m2[:], None, ALU.is_ge, None, [me2b, m2b], [k2b])
                    self.TT("dve", dd[:], m2[:], m1[:], ALU.subtract, [m1b, m2b], [ddb])
                    self.ACT(ee[:], dd[:], AF.Exp, [ddb], [eeb])
                    self.TS("dve", ee[:], ee[:], 1.0, None, ALU.add, None, [eeb], [eeb])
                    self.A("dve", lambda e, ee=ee: e.reciprocal(out=ee[:], in_=ee[:]), [eeb], [eeb])
                    self.TT("dve", w1[:], ee[:], pgrp[:], ALU.mult, [eeb, pgrpb], [w1b])
                    self.TT("dve", w2[:], pgrp[:], w1[:], ALU.subtract, [pgrpb, w1b], [w2b])
                    self.TS("dve", k1[:], k1[:], w1[:], None, ALU.mult, None, [k1b, w1b], [k1b])
                    self.STT("dve", gates[:, t, :], k2[:], w2[:], k1[:], ALU.mult, ALU.add, [k2b, w2b, k1b], [gates_b])
                if l == 0 and mp == 0:
                    self.dbg("gates", gates[:], [128, MT, 32], F32, [gates_b])
                    self.dbg("h2T", h2T[:], [128, 16, MT * 128], BF16, [h2T_b])
                if self.stop == "router":
                    self.finish([gates_b, h2T_b], out_d)
                    return "stop"
                self.barrier(RD2.bufs, [acc_b])
                cur_overlay[0] = list(RD.bufs)

                def load_wg(e):
                    return self.ring_load(w_gate[l, e].rearrange("(k p) f -> p k f", p=128), slot16)

                def load_wu(e):
                    return self.ring_load(w_up[l, e].rearrange("(k p) f -> p k f", p=128), slot16)

                def load_wd(e):
                    return self.ring_load(w_down[l, e].rearrange("(k p) d -> p k d", p=128), lambda tt: tt[:].rearrange("p (k d) -> p k d", k=4))

                nxt = [load_wg(0), load_wu(0), load_wd(0)]
                for e in range(NE):
                    (wg, wgb), (wu, wub), (wd, wdb) = nxt
                    if e + 1 < NE:
                        nxt = [load_wg(e + 1), None, None]

                    def au_mm(t):
                        pa, pu = (0, 1) if t % 2 == 0 else (2, 3)
                        for k in range(16):
                            self.MM(ps[pa][:], h2T[:, k, t * 128:(t + 1) * 128], wg[:, k, :], k == 0, k == 15, [h2T_b, wgb], [psb[pa]])
                        for k in range(16):
                            self.MM(ps[pu][:], h2T[:, k, t * 128:(t + 1) * 128], wu[:, k, :], k == 0, k == 15, [h2T_b, wub], [psb[pu]])
                        sa_, sab = sa[t % 2]
                        hd_, hdb = hid[t % 2]
                        self.ACT(sa_[:], ps[pa][:], AF.Silu, [psb[pa]], [sab])
                        self.STT("dve", hd_[:], ps[pu][:], gates[:, t, e:e + 1], sa_[:], ALU.mult, ALU.mult, [psb[pu], gates_b, sab], [hdb])

                    def au_tr(t):
                        hd_, hdb = hid[t % 2]
                        bk = 4 + (t % 2)
                        for i in range(4):
                            self.TR(psbf(bk)[:, i * 128:(i + 1) * 128], hd_[:, i * 128:(i + 1) * 128], idn[:], [hdb, cst], [psb[bk]])
                        self.ACT(hidT[:, :, t * 128:(t + 1) * 128], psbf(bk)[:, 0:512].rearrange("p (i n) -> p i n", i=4), AF.Copy, [psb[bk]], [hidT_bs[t]])

                    for t in range(MT):
                        au_mm(t)
                        if t >= 1:
                            au_tr(t - 1)
                    au_tr(MT - 1)
                    if e + 1 < NE:
                        nxt[1] = load_wu(e + 1)
                        nxt[2] = load_wd(e + 1)
                    for t in range(MT):
                        b0 = 0 if t % 2 == 0 else 4
                        for dbk in range(4):
                            for fc in range(4):
                                self.MM(ps[b0 + dbk][:], hidT[:, fc, t * 128:(t + 1) * 128], wd[:, fc, dbk * 512:(dbk + 1) * 512], fc == 0, fc == 3, [hidT_bs[t], wdb], [psb[b0 + dbk]])
                        for dbk in range(4):
                            dst_ap = acc[:, t, dbk * 512:(dbk + 1) * 512]
                            if e == 0:
                                self.CP("dve", dst_ap, ps[b0 + dbk][:], [psb[b0 + dbk]], [acc_b])
                            else:
                                self.TT("dve", dst_ap, ps[b0 + dbk][:], dst_ap, ALU.add, [psb[b0 + dbk], acc_b], [acc_b])
                self.DMA("sp", gb[:], mod_d[l:l + 1, 5 * D:6 * D].partition_broadcast(128), [mod_b], [gb_b])
                n7 = 0
                for t in range(MT):
                    tile = mp * MT + t
                    for cbk in range(4):
                        xc_, xcb = xc2[n7 % 2]
                        xd_, xdb = xc3[n7 % 2]
                        n7 += 1
                        cs = slice(cbk * 512, (cbk + 1) * 512)
                        self.DMA("sp", xc_[:], xres[tile * 128:(tile + 1) * 128, cs], [xres_b[tile]], [xcb])
                        if sel:
                            tileB = NT // 2 + tile
                            self.DMA("sp", xd_[:], xres[tileB * 128:(tileB + 1) * 128, cs], [xres_b[tileB]], [xdb])
                            self.TS("dve", xd_[:], xd_[:], flg[:, 1:2], None, ALU.mult, None, [xdb, cst], [xdb])
                            self.STT("dve", xc_[:], xc_[:], flg[:, 0:1], xd_[:], ALU.mult, ALU.add, [xcb, xdb, cst], [xcb])
                        self.TT("pool", acc[:, t, cs], acc[:, t, cs], gb[:, cs], ALU.mult, [acc_b, gb_b], [acc_b])
                        self.TT("dve", xc_[:], xc_[:], acc[:, t, cs], ALU.add, [xcb, acc_b], [xcb])
                        if sel:
                            self.DMA("sp", xsel_d[tile * 128:(tile + 1) * 128, cs], xc_[:], [xcb], [xsel_b[tile]])
                        else:
                            self.DMA("sp", xres[tile * 128:(tile + 1) * 128, cs], xc_[:], [xcb], [xres_b[tile]])
                if self.stop == "moe":
                    if sel:
                        self.dbg("xout", xsel_d[0:MT * 128, :], [MT * 128, D], F32, xsel_b[0:MT])
                        self.finish(xsel_b[mp * MT:(mp + 1) * MT], out_d)
                    else:
                        self.dbg("xout", xres[0:MT * 128, :], [MT * 128, D], F32, xres_b[0:MT])
                        self.finish(xres_b[mp * MT:(mp + 1) * MT], out_d)
                    return "stop"
                return None


            for g in range(NG):
                switch(RA.bufs + [yT_b, cqnT_b, ropeF_b, ropeR_b])

                def slot16(tt):
                    return tt[:].rearrange("p (k c) -> p k c", k=16)

                self.wplan = []
                pl_cq = self.plan_load(w_in[l, :, 0:512].rearrange("(k p) c -> p k c", p=128), slot16)
                pl_ckv = self.plan_load(w_in[l, :, 512:832].rearrange("(k p) c -> p k c", p=128),
                                        lambda tt: tt[:, 0:16 * 320].rearrange("p (k c) -> p k c", k=16))
                pl_ret = {}
                for hh_ in range(2):
                    for kind_, cbase_ in (("q", 832), ("k", 1856), ("v", 2880), ("g", 3904)):
                        c0_ = cbase_ + hh_ * 512
                        pl_ret[(hh_, kind_)] = self.plan_load(w_in[l, :, c0_:c0_ + 512].rearrange("(k p) c -> p k c", p=128), slot16)
                pl_wo = [self.plan_load(w_o[l, :, cb_ * 512:(cb_ + 1) * 512].rearrange("(k p) c -> p k c", p=128), slot16) for cb_ in range(4)]
                self.DMA("sp", cosF_g[:], cosF_d[:, g * G:(g + 1) * G], [tab_b], [ropeF_b])
                self.DMA("sp", sinF_g[:], sinF_d[:, g * G:(g + 1) * G], [tab_b], [ropeF_b])
                self.DMA("sp", cosR_g[:], cosR_d[:, g * GT:(g + 1) * GT, :], [tab_b], [ropeR_b])
                self.DMA("sp", sinR_g[:], sinR_d[:, g * GT:(g + 1) * GT, :], [tab_b], [ropeR_b])
                self.TS("dve", nsinR_g[:], sinR_g[:], -1.0, None, ALU.mult, None, [ropeR_b], [ropeR_b])
                self.DMA("pool", wukv[:], w_ukv[l].rearrange("(k p) c -> p k c", p=128), [self.wsrc], [wukv_b])
                for t in range(GT):
                    tile = g * GT + t
                    rsrc = [self.wsrc] if l == 0 else [xres_b[tile]]
                    self.DMA("sp", xt[:], xsrc[tile * 128:(tile + 1) * 128, :], rsrc, [xt_b])
                    rstd, rstd_b = rmsnorm_stats(xt[:], D, xn_g[:, t, :], [xt_b], [yT_b], 0)
                    self.ACT(xn_g[:, t, :], xt[:], AF.Copy, [xt_b, rstd_b], [yT_b], scale=rstd[:])
                for k in range(16):
                    bk = 2 + (k % 2)
                    for t in range(GT):
                        self.TR(psbf(bk)[:, t * 128:(t + 1) * 128], xn_g[:, t, k * 128:(k + 1) * 128], idn[:], [yT_b, cst], [psb[bk]])
                    if k % 2 == 0:
                        self.ACT(hT[:, k, :], psbf(bk)[:, 0:G], AF.Identity, [psb[bk], vecs_b], [hT_b], scale=sc1c[:, k:k + 1], bias=sh1c[:, k:k + 1])
                    else:
                        self.TS("dve", hT[:, k, :], psbf(bk)[:, 0:G], sc1c[:, k:k + 1], sh1c[:, k:k + 1], ALU.mult, ALU.add, [psb[bk], vecs_b], [hT_b])
                if l == 0 and g == 0:
                    self.dbg("hT", hT[:], [128, 16, G], BF16, [hT_b])
                if self.stop == "hT":
                    return self.finish([hT_b], out_d)

                wv, wb_ = self.get_load(pl_cq)
                for t in range(GT):
                    pi = t % 2
                    for k in range(16):
                        self.MM(ps[pi][:], hT[:, k, t * 128:(t + 1) * 128], wv[:, k, :], k == 0, k == 15, [hT_b, wb_], [psb[pi]])
                    rstd, rstd_b = rmsnorm_stats(ps[pi][:], 512, junk[:], [psb[pi]], [junk_b], 3 * (t % 2))
                    self.ACT(cqn_g[:, t, :], ps[pi][:], AF.Copy, [psb[pi], rstd_b], [cqn_b], scale=rstd[:])
                for kc in range(4):
                    bk = 2 + (kc % 2)
                    for t in range(GT):
                        self.TR(psbf(bk)[:, t * 128:(t + 1) * 128], cqn_g[:, t, kc * 128:(kc + 1) * 128], idn[:], [cqn_b, cst], [psb[bk]])
                    self.TS("dve", cqnT[:, kc, :], psbf(bk)[:, 0:G], qng[:, kc:kc + 1], None, ALU.mult, None, [psb[bk], vecs_b], [cqnT_b])
                wv, wb_ = self.get_load(pl_ckv)
                for t in range(GT):
                    pi = t % 2
                    for k in range(16):
                        self.MM(ps[pi][:, 0:256], hT[:, k, t * 128:(t + 1) * 128], wv[:, k, 0:256], k == 0, k == 15, [hT_b, wb_], [psb[pi]])
                    rstd, rstd_b = rmsnorm_stats(ps[pi][:, 0:256], 256, junk[:, 0:256], [psb[pi]], [junk_b], 3 * (t % 2))
                    self.ACT(ckvn_g[:, t, :], ps[pi][:, 0:256], AF.Copy, [psb[pi], rstd_b], [ckvn_b], scale=rstd[:])
                for kc in range(2):
                    bk = 2 + (kc % 2)
                    for t in range(GT):
                        self.TR(psbf(bk)[:, t * 128:(t + 1) * 128], ckvn_g[:, t, kc * 128:(kc + 1) * 128], idn[:], [ckvn_b, cst], [psb[bk]])
                    self.TS("dve", ckvnT[:, kc, :], psbf(bk)[:, 0:G], kvng[:, kc:kc + 1], None, ALU.mult, None, [psb[bk], vecs_b], [ckvnT_b])
                self.TS("dve", kperot[:, :, 0:32], wv[:, :, 288:320], -1.0, None, ALU.mult, None, [wb_], [kperot_b])
                self.CP("dve", kperot[:, :, 32:64], wv[:, :, 256:288], [wb_], [kperot_b])
                for k in range(16):
                    self.MM(ps[0][0:64, :], wv[:, k, 256:320], hT[:, k, :], k == 0, k == 15, [hT_b, wb_], [psb[0]])
                for k in range(16):
                    self.MM(ps[1][0:64, :], kperot[:, k, :], hT[:, k, :], k == 0, k == 15, [hT_b, kperot_b], [psb[1]])
                self.TT("dve", kt1[:], ps[0][0:64, :], cosF_g[:], ALU.mult, [psb[0], ropeF_b], [kt1_b])
                self.TT("dve", kt2[:], ps[1][0:64, :], sinF_g[:], ALU.mult, [psb[1], ropeF_b], [kt2_b])
                self.TT("dve", kpeo[:], kt1[:], kt2[:], ALU.add, [kt1_b, kt2_b], [kpeo_b])
                self.DMA("sp", kpe_d[:, g * G:(g + 1) * G], kpeo[:], [kpeo_b], [kv_b[g]])
                for h in range(8):
                    pi = h % 2
                    for kc in range(2):
                        self.MM(ps[pi][:], wukv[:, kc, h * 256:h * 256 + 128], ckvnT[:, kc, :], kc == 0, kc == 1, [wukv_b, ckvnT_b], [psb[pi]])
                    kt_, ktb_ = kTo[h % 2]
                    evac(kt_[:], ps[pi][:], [psb[pi]], [ktb_])
                    self.DMA("sp", kT_d[h, :, g * G:(g + 1) * G], kt_[:], [ktb_], [kv_b[g]])
                wukv_v = wukv[:].rearrange("p k (h c) -> p k h c", h=8)
                for t in range(GT):
                    tile = g * GT + t
                    vt_, vtb_ = vo[t % 2]
                    for hf in range(2):
                        pi = hf
                        for kc in range(2):
                            self.MM(ps[pi][:].rearrange("p (h c) -> p h c", h=4), ckvnT[:, kc, t * 128:(t + 1) * 128],
                                    wukv_v[:, kc, 4 * hf:4 * hf + 4, 128:256], kc == 0, kc == 1, [wukv_b, ckvnT_b], [psb[pi]])
                        evac(vt_[:, hf * 512:(hf + 1) * 512], ps[pi][:], [psb[pi]], [vtb_])
                    self.DMA("sp", v_d[:, :, tile, :].rearrange("h p d -> p h d"), vt_[:].rearrange("p (h d) -> p h d", h=8), [vtb_], [kv_b[g]])
                if l == 0 and g == 0:
                    self.dbg("cqnT", cqnT[:], [128, 4, G], BF16, [cqnT_b])
                    self.dbg("ckvnT", ckvnT[:], [128, 2, G], BF16, [ckvnT_b])
                if self.stop == "kv":
                    return self.finish([kv_b[g], cqnT_b], out_d)

                for hh in range(2):
                    for kind, cbase in (("q", 832), ("k", 1856), ("v", 2880), ("g", 3904)):
                        wv, wb_ = self.get_load(pl_ret[(hh, kind)])
                        for t in range(GT):
                            pi = t % 2
                            for k in range(16):
                                self.MM(ps[pi][:], hT[:, k, t * 128:(t + 1) * 128], wv[:, k, :], k == 0, k == 15, [hT_b, wb_], [psb[pi]])
                            if kind in ("q", "k"):
                                (a1, a1b), (a2, a2b), (a3, a3b) = rt1[t % 2], rt2[t % 2], rt3[t % 2]
                                psv = ps[pi][:].rearrange("p (h two j) -> p h two j", h=4, two=2)
                                a1v = a1[:].rearrange("p (h two j) -> p h two j", h=4, two=2)
                                a2v = a2[:].rearrange("p (h two j) -> p h two j", h=4, two=2)
                                cosb = cosR_g[:, t, :].unsqueeze(1).unsqueeze(1).broadcast_to([128, 4, 2, 64])
                                sinb = sinR_g[:, t, :].unsqueeze(1).broadcast_to([128, 4, 64])
                                nsinb = nsinR_g[:, t, :].unsqueeze(1).broadcast_to([128, 4, 64])
                                self.TT("dve", a1v, psv, cosb, ALU.mult, [psb[pi], ropeR_b], [a1b])
                                self.TT("dve", a2v[:, :, 0, :], psv[:, :, 1, :], nsinb, ALU.mult, [psb[pi], ropeR_b], [a2b])
                                self.TT("dve", a2v[:, :, 1, :], psv[:, :, 0, :], sinb, ALU.mult, [psb[pi], ropeR_b], [a2b])
                                if kind == "q":
                                    self.TT("pool", q_r[:, t, :], a1[:], a2[:], ALU.add, [a1b, a2b], [q_r_b])
                                else:
                                    self.TT("pool", a3[:], a1[:], a2[:], ALU.add, [a1b, a2b], [a3b])
                                    self.TT("pool", k_p[:, t, :].rearrange("p (h d) -> p h d", h=4), a3[:].rearrange("p (h d) -> p h d", h=4),
                                            gk_col[:, 4 * hh:4 * hh + 4].unsqueeze(2).broadcast_to([128, 4, 128]), ALU.mult, [a3b, cst], [k_p_b])
                            elif kind == "v":
                                self.ACT(v_r[:, t, :], ps[pi][:], AF.Copy, [psb[pi]], [v_r_b])
                            else:
                                self.ACT(g_r[:, t, :], ps[pi][:], AF.Silu, [psb[pi]], [g_r_b])
                    if l == 0 and g == 0 and hh == 0:
                        self.dbg("q_r", q_r[:], [128, GT, 512], BF16, [q_r_b])
                        self.dbg("k_p", k_p[:], [128, GT, 512], BF16, [k_p_b])
                    if self.stop == "qk":
                        return self.finish([q_r_b, k_p_b, v_r_b, g_r_b], out_d)
                    for t in range(GT):
                        for j in range(4):
                            self.TR(psbf(4)[:, j * 128:(j + 1) * 128], q_r[:, t, j * 128:(j + 1) * 128], idn[:], [q_r_b, cst], [psb[4]])
                        for j in range(4):
                            self.TR(psbf(4)[:, 512 + j * 128:512 + (j + 1) * 128], k_p[:, t, j * 128:(j + 1) * 128], idn[:], [k_p_b, cst], [psb[4]])
                        self.ACT(ysq[:], psbf(4)[:, 0:512].rearrange("p (h n) -> p h n", h=4), AF.Copy, [psb[4]], [ysq_b])
                        self.TT("dve", qT4[:], ysq[:], Gq[:, 4 * hh:4 * hh + 4, :], ALU.mult, [ysq_b, cst], [qT4_b])
                        self.ACT(kT4[:], psbf(4)[:, 512:1024].rearrange("p (h n) -> p h n", h=4), AF.Copy, [psb[4]], [kT4_b])
                        if self.stop == "r1":
                            return self.finish([qT4_b, kT4_b], out_d)
                        for j in range(4):
                            self.MM(ps[5][:, j * 128:(j + 1) * 128], kT4[:, j, :], qT4[:, j, :], True, True, [kT4_b, qT4_b], [psb[5]])
                        self.TT("dve", sT4[:], ps[5][:].rearrange("p (h n) -> p h n", h=4), maskT[:].unsqueeze(1).broadcast_to([128, 4, 128]), ALU.mult, [psb[5], cst], [sT4_b])
                        self.TT("pool", Sf[:], Tst[:, 4 * hh:4 * hh + 4, :], g128[:, 4 * hh:4 * hh + 4].unsqueeze(2).broadcast_to([128, 4, 128]), ALU.mult, [Tst_b, cst], [Sf_b])
                        self.CP("pool", Sb[:], Sf[:], [Sf_b], [Sb_b])
                        if self.stop == "r2":
                            return self.finish([sT4_b, Sb_b], out_d)
                        for j in range(4):
                            self.MM(ps[6][:, j * 128:(j + 1) * 128], sT4[:, j, :], v_r[:, t, j * 128:(j + 1) * 128], True, False, [sT4_b, v_r_b], [psb[6]])
                            self.MM(ps[6][:, j * 128:(j + 1) * 128], qT4[:, j, :], Sb[:, j, :], False, True, [qT4_b, Sb_b], [psb[6]])
                        for j in range(4):
                            self.MM(ps[7][:, j * 128:(j + 1) * 128], k_p[:, t, j * 128:(j + 1) * 128], v_r[:, t, j * 128:(j + 1) * 128], True, True, [k_p_b, v_r_b], [psb[7]])
                        self.TT("dve", Tst[:, 4 * hh:4 * hh + 4, :], ps[7][:].rearrange("p (h n) -> p h n", h=4), Sf[:], ALU.add, [Sf_b, psb[7]], [Tst_b])
                        if self.stop == "r3":
                            return self.finish([Tst_b, psb[6]], out_d)
                        y3 = ps[6][:].rearrange("p (h n) -> p h n", h=4)
                        (s1, s1b), (s2, s2b), (mean, meanb), (msq, msqb), (var, varb), (sd, sdb), (rs_, rsb) = s4[0:7]
                        self.A("dve", lambda e, s1=s1, y3=y3: e.reduce_sum(out=s1[:], in_=y3, axis=AX.X), [psb[6]], [s1b])
                        self.ACT(ysq[:], y3, AF.Square, [psb[6]], [ysq_b])
                        self.A("dve", lambda e, s2=s2: e.reduce_sum(out=s2[:], in_=ysq[:], axis=AX.X), [ysq_b], [s2b])
                        self.TS("dve", mean[:], s1[:], 1.0 / 128, None, ALU.mult, None, [s1b], [meanb])
                        self.TT("dve", msq[:], mean[:], mean[:], ALU.mult, [meanb], [msqb])
                        self.STT("dve", var[:], s2[:], 1.0 / 128, msq[:], ALU.mult, ALU.subtract, [s2b, msqb], [varb])
                        self.ACT(sd[:], var[:], AF.Sqrt, [varb], [sdb], bias=EPS)
                        self.A("dve", lambda e, rs_=rs_, sd=sd: e.reciprocal(out=rs_[:], in_=sd[:]), [sdb], [rsb])
                        self.TT("dve", yc[:], y3, mean[:].unsqueeze(2).broadcast_to([128, 4, 128]), ALU.subtract, [psb[6], meanb], [yc_b])
                        self.TT("pool", yc[:], yc[:], rs_[:].unsqueeze(2).broadcast_to([128, 4, 128]), ALU.mult, [yc_b, rsb], [yc_b])
                        self.TT("pool", yg[:], yc[:].rearrange("p h n -> p (h n)"), g_r[:, t, :], ALU.mult, [yc_b, g_r_b], [yg_b])
                        if self.stop == "r4":
                            return self.finish([yg_b], out_d)
                        for j in range(4):
                            self.TR(psbf(3)[:, j * 128:(j + 1) * 128], yg[:, j * 128:(j + 1) * 128], idn[:], [yg_b, cst], [psb[3]])
                        self.ACT(yT[:, 8 + 4 * hh:12 + 4 * hh, t * 128:(t + 1) * 128], psbf(3)[:, 0:512].rearrange("p (h n) -> p h n", h=4), AF.Copy, [psb[3]], [yT_b])
                if l == 0 and g == 0:
                    self.dbg("yT_ret", yT[:, 8:16, :], [128, 8, G], BF16, [yT_b])
                if self.stop == "ret":
                    return self.finish([yT_b, kv_b[g]], out_d)

                switch(RB.bufs)
                nk = (g + 1) * G
                nkt = nk // 128
                SC = 192.0 ** -0.5
                self.DMA("pool", wuq[:], w_uq[l].rearrange("(k p) c -> p k c", p=128), [self.wsrc], [wuq_b])
                wuq_v = wuq[:].rearrange("p k (h c) -> p k h c", h=8)
                self.TS("dve", wrot[:, :, :, 0:32], wuq_v[:, :, :, 160:192], -1.0, None, ALU.mult, None, [wuq_b], [wrot_b])
                self.CP("dve", wrot[:, :, :, 32:64], wuq_v[:, :, :, 128:160], [wuq_b], [wrot_b])
                self.DMA("sp", kpeT[:, 0:nk], kpe_d[:, 0:nk], kv_b[:g + 1], [kpeT_b])
                for h in range(8):
                    self.DMA("sp", kTh[:, 0:nk], kT_d[h, :, 0:nk], kv_b[:g + 1], [kTh_b])
                    self.DMA("sp", vh[:, 0:nkt, :], v_d[h, :, 0:nkt, :], kv_b[:g + 1], [vh_b])
                    for kc in range(4):
                        self.MM(ps[0][:], wuq[:, kc, h * 192:h * 192 + 128], cqnT[:, kc, :], kc == 0, kc == 3, [wuq_b, cqnT_b], [psb[0]])
                    self.ACT(qTh[:], ps[0][:], AF.Copy, [psb[0]], [qTh_b])
                    for kc in range(4):
                        self.MM(ps[1][0:64, :], wuq[:, kc, h * 192 + 128:h * 192 + 192], cqnT[:, kc, :], kc == 0, kc == 3, [wuq_b, cqnT_b], [psb[1]])
                    for kc in range(4):
                        self.MM(ps[2][0:64, :], wrot[:, kc, h, :], cqnT[:, kc, :], kc == 0, kc == 3, [wrot_b, cqnT_b], [psb[2]])
                    self.TT("dve", qt1[:], ps[1][0:64, :], cosF_g[:], ALU.mult, [psb[1], ropeF_b], [qt1_b])
                    self.TT("dve", qt2[:], ps[2][0:64, :], sinF_g[:], ALU.mult, [psb[2], ropeF_b], [qt2_b])
                    self.TT("dve", qpeTh[:], qt1[:], qt2[:], ALU.add, [qt1_b, qt2_b], [qpeTh_b])
                    for t in range(GT):
                        J = g * GT + t
                        nkeys = (J + 1) * 128
                        nblk = (nkeys + 511) // 512
                        for kb in range(nblk):
                            w = min(512, nkeys - kb * 512)
                            bk = 3 + (kb % 2)
                            self.MM(ps[bk][:, 0:w], qTh[:, t * 128:(t + 1) * 128], kTh[:, kb * 512:kb * 512 + w], True, False, [qTh_b, kTh_b], [psb[bk]])
                            self.MM(ps[bk][:, 0:w], qpeTh[:, t * 128:(t + 1) * 128], kpeT[:, kb * 512:kb * 512 + w], False, True, [qpeTh_b, kpeT_b], [psb[bk]])
                            if kb == nblk - 1:
                                if w > 128:
                                    evac(stash[:, kb * 512:kb * 512 + w - 128], ps[bk][:, 0:w - 128], [psb[bk]], [stash_b])
                                self.TT("dve", stash[:, nkeys - 128:nkeys], ps[bk][:, w - 128:w], maskD[:], ALU.add, [psb[bk], cst], [stash_b])
                            else:
                                evac(stash[:, kb * 512:kb * 512 + w], ps[bk][:, 0:w], [psb[bk]], [stash_b])
                        (mx, mxb), (nb, nbb), (rsum, rsumb), (rinv, rinvb) = bst[0:4]
                        rs_t, rs_tb = rs8[t % 2]
                        self.A("dve", lambda e, mx=mx, nkeys=nkeys: e.reduce_max(out=mx[:], in_=stash[:, 0:nkeys], axis=AX.X), [stash_b], [mxb])
                        self.TS("dve", nb[:], mx[:], -SC, None, ALU.mult, None, [mxb], [nbb])
                        ob = 6 if t % 2 == 0 else 2
                        for kb in range(nblk):
                            w = min(512, nkeys - kb * 512)
                            pbt, pbb = Pb[kb % 2]
                            ptt, ptb = PT[kb % 2]
                            tbk = 5 if kb % 2 == 0 else 7
                            self.ACT(pbt[:, 0:w], stash[:, kb * 512:kb * 512 + w], AF.Exp, [stash_b, nbb], [pbb, rs_tb], bias=nb[:], scale=SC, accum_out=rs_t[:, kb:kb + 1])
                            nti = w // 128
                            for i in range(nti):
                                self.TR(psbf(tbk)[:, i * 128:(i + 1) * 128], pbt[:, i * 128:(i + 1) * 128], idn[:], [pbb, cst], [psb[tbk]])
                            if kb % 2 == 0:
                                self.CP("dve", ptt[:, 0:nti, :], psbf(tbk)[:, 0:nti * 128].rearrange("p (i n) -> p i n", i=nti), [psb[tbk]], [ptb])
                            else:
                                self.ACT(ptt[:, 0:nti, :], psbf(tbk)[:, 0:nti * 128].rearrange("p (i n) -> p i n", i=nti), AF.Copy, [psb[tbk]], [ptb])
                            for i in range(nti):
                                kt_i = kb * 4 + i
                                self.MM(ps[ob][:, 0:128], ptt[:, i, :], vh[:, kt_i, :], kt_i == 0, kt_i == J, [ptb, vh_b], [psb[ob]])
                        self.A("dve", lambda e, rsum=rsum, rs_t=rs_t, nblk=nblk: e.reduce_sum(out=rsum[:], in_=rs_t[:, 0:nblk], axis=AX.X), [rs_tb], [rsumb])
                        self.A("dve", lambda e, rinv=rinv, rsum=rsum: e.reciprocal(out=rinv[:], in_=rsum[:]), [rsumb], [rinvb])
                        self.ACT(ytok[:, t, h * 128:(h + 1) * 128], ps[ob][:, 0:128], AF.Copy, [psb[ob], rinvb], [ytok_b], scale=rinv[:])
                for t in range(GT):
                    bk = 0 if t % 2 == 0 else 1
                    for h in range(8):
                        self.TR(psbf(bk)[:, h * 128:(h + 1) * 128], ytok[:, t, h * 128:(h + 1) * 128], idn[:], [ytok_b, cst], [psb[bk]])
                    evac(yT[:, 0:8, t * 128:(t + 1) * 128], psbf(bk)[:, 0:1024].rearrange("p (h n) -> p h n", h=8), [psb[bk]], [yT_b])
                if l == 0 and g == 0:
                    self.dbg("yT", yT[:], [128, 16, G], BF16, [yT_b])
                if self.stop == "attn":
                    return self.finish([yT_b], out_d)

                switch(RCo.bufs)
                self.DMA("sp", gb[:], mod_d[l:l + 1, 2 * D:3 * D].partition_broadcast(128), [mod_b], [gb_b])
                n6 = 0
                if self.stop == "w6a":
                    return self.finish([gb_b], out_d)
                for cbk in range(4):
                    wv, wb_ = self.get_load(pl_wo[cbk])
                    for t in range(GT):
                        tile = g * GT + t
                        pi = n6 % 2
                        (xc_, xcb), (xm_, xmb), (xo_, xob) = xc[pi], xtmp[pi], xo[pi]
                        n6 += 1
                        rsrc = [self.wsrc] if l == 0 else [xres_b[tile]]
                        self.DMA("sp", xc_[:], xsrc[tile * 128:(tile + 1) * 128, cbk * 512:(cbk + 1) * 512], rsrc, [xcb])
                        for k in range(16):
                            self.MM(ps[pi][:], yT[:, k, t * 128:(t + 1) * 128], wv[:, k, :], k == 0, k == 15, [yT_b, wb_], [psb[pi]])
                        self.TT("dve", xm_[:], ps[pi][:], gb[:, cbk * 512:(cbk + 1) * 512], ALU.mult, [psb[pi], gb_b], [xmb])
                        self.TT("pool", xo_[:], xm_[:], xc_[:], ALU.add, [xmb, xcb], [xob])
                        if self.stop == "w6b":
                            return self.finish([xob], out_d)
                        self.DMA("sp", xres[tile * 128:(tile + 1) * 128, cbk * 512:(cbk + 1) * 512], xo_[:], [xob], [xres_b[tile]])
                if self.stop == "wo":
                    self.dbg("xmid", xres[0:G, :], [G, D], F32, xres_b[0:GT])
                    return self.finish(xres_b[g * GT:(g + 1) * GT], out_d)
                if self.stop == "wo_all" and g == NG - 1:
                    return self.finish(xres_b, out_d)

                if last_layer or (g + 1) % (MT // GT) != 0:
                    continue
                if moe_pass((g + 1) // (MT // GT) - 1, False) == "stop":
                    return self
            if last_layer:
                for mp in range(NMP // 2):
                    if moe_pass(mp, True) == "stop":
                        return self

        RF_b = [Buf("fin_x"), Buf("fin_o")]
        switch(RF_b + [gb_b])
        fx = [self.sb(persist_end + i * 8192, [128, D], F32, f"fx{i}") for i in range(2)]
        fo = [self.sb(persist_end + 16384 + i * 8192, [128, D], F32, f"fo{i}") for i in range(2)]
        fxb = [Buf(), Buf()]
        fob = [Buf(), Buf()]
        fst = [(self.sb(persist_end + 40000 + i * 64, [128, 1], F32, f"fst{i}"), Buf()) for i in range(6)]
        for b in fxb + fob + [x[1] for x in fst]:
            b.last_w = RF_b[0].last_w
        self.DMA("sp", gb[:], fng.rearrange("(o d) -> o d", o=1).partition_broadcast(128), [self.wsrc], [gb_b])
        outs = []
        for tile in range(NT // 2):
            i = tile % 2
            self.DMA("sp", fx[i][:], xsel_d[tile * 128:(tile + 1) * 128, :], [xsel_b[tile]], [fxb[i]])
            (ssq, ssq_b), (std, std_b), (rstd, rstd_b) = fst[3 * i:3 * i + 3]
            self.ACT(fo[i][:], fx[i][:], AF.Square, [fxb[i]], [fob[i], ssq_b], accum_out=ssq[:])
            self.ACT(std[:], ssq[:], AF.Sqrt, [ssq_b], [std_b], scale=1.0 / D, bias=EPS)
            self.A("dve", lambda e, rstd=rstd, std=std: e.reciprocal(out=rstd[:], in_=std[:]), [std_b], [rstd_b])
            self.STT("dve", fo[i][:], fx[i][:], rstd[:], gb[:], ALU.mult, ALU.mult, [fxb[i], rstd_b, gb_b], [fob[i]])
            outs.append(self.DMA("sp", out_d[tile * 128:(tile + 1) * 128, :], fo[i][:], [fob[i]], [Buf()]))
        self.P.emit(final_wait_ops=outs + self.dbg_ops)
        return self

    def finish(self, bufs, out_d):
        t = self.sb(self.sb_top - 4096, [128, 8], F32, "fin")
        b = Buf()
        self.MS("pool", t[:], 0.0, [b])
        op = self.DMA("sp", out_d[0:128, 0:8], t[:], [b] + list(bufs), [Buf()])
        self.P.emit(final_wait_ops=[op] + self.dbg_ops)
        return self


def _core_role(cidx):
    die, idx = cidx // 4, cidx % 4
    return die * 2 + idx % 2, idx // 2


def _in_maps(inputs, n_cores=8):
    shared = {k: np.ascontiguousarray(np.asarray(v, dtype=np.float32)) for k, v in inputs.items() if k not in ("x", "c")}
    maps = []
    for cidx in range(n_cores):
        b, half = _core_role(cidx)
        m = dict(shared)
        xb = np.asarray(inputs["x"][b], dtype=np.float32)
        if half == 0:
            xs = np.zeros_like(xb)
            xs[:S // 2] = xb[:S // 2]
            xb = xs
        m["x"] = np.ascontiguousarray(xb)
        m["c_col"] = np.ascontiguousarray(np.asarray(inputs["c"][b], dtype=np.float32).reshape(16, 128).T)
        fl = np.zeros((128, 2), np.float32)
        fl[:, half] = 1.0
        m["flag"] = fl
        maps.append(m)
    return maps


def kernel(**inputs):
    bld = Builder()
    maps = _in_maps(inputs)
    res = run_bass_kernel_spmd(bld.nc, maps, core_ids=list(range(8)))
    out = np.zeros((4, S, D), np.float32)
    for cidx in range(8):
        b, half = _core_role(cidx)
        out[b, half * (S // 2):(half + 1) * (S // 2)] = np.asarray(res.results[cidx]["out"], dtype=np.float32)
    return out
```

```python
import math
import numpy as np
import concourse.bass as bass
import concourse.mybir as mybir
from concourse.bass_utils import run_bass_kernel_spmd

F32 = mybir.dt.float32
BF16 = mybir.dt.bfloat16
I32 = mybir.dt.int32
ALU = mybir.AluOpType
AF = mybir.ActivationFunctionType
AX = mybir.AxisListType

ENGS = ("pe", "act", "dve", "pool", "sp")

D = 2048
S = 4096
L = 2
NT = S // 128
GT = 4
G = GT * 128
NG = S // G
MT = 8
NMP = NT // MT
INW = 4928
NE = 32
DE = 512
EPS = 1e-6
LNG = [math.log1p(-2.0 ** (-5.0 - h)) for h in range(8)]


class Buf:
    __slots__ = ("name", "last_w", "readers")

    def __init__(self, name=""):
        self.name = name
        self.last_w = None
        self.readers = {}


class Op:
    __slots__ = ("eng", "fn", "deps", "is_dma", "sem", "val", "signal", "prewait")

    def __init__(self, eng, fn, is_dma):
        self.eng = eng
        self.fn = fn
        self.deps = []
        self.is_dma = is_dma
        self.sem = None
        self.val = 0
        self.signal = False
        self.prewait = None


class Prog:
    def __init__(self, nc, n_dma_sems=48):
        self.nc = nc
        self.ops = {e: [] for e in ENGS}
        self.n_dma_sems = n_dma_sems
        self.all_ops = []

    def add(self, eng, fn, reads=(), writes=(), dma=False):
        op = Op(eng, fn, dma)
        deps = {}
        for b in reads:
            if b.last_w is not None:
                deps[id(b.last_w)] = b.last_w
        for b in writes:
            if b.last_w is not None:
                deps[id(b.last_w)] = b.last_w
            for r in b.readers.values():
                deps[id(r)] = r
        for d in deps.values():
            if d is op:
                continue
            if d.eng == "pe" and eng == "pe" and not d.is_dma and not dma:
                continue
            op.deps.append(d)
            d.signal = True
        for b in reads:
            b.readers[(eng, dma, id(op) if dma else 0)] = op
        for b in writes:
            b.last_w = op
            b.readers = {}
        self.ops[eng].append(op)
        self.all_ops.append(op)
        return op

    def dma(self, eng, out, in_, reads=(), writes=(), **kw):
        return self.add(eng, lambda e: e.dma_start(out=out, in_=in_, **kw), reads, writes, dma=True)

    def emit(self, final_wait_ops=()):
        nc = self.nc
        EPOCH = 16000
        esem = {e: [nc.alloc_semaphore(f"s_{e}0")] for e in ENGS}
        dsems = [nc.alloc_semaphore(f"d_{i}") for i in range(self.n_dma_sems)]
        dcount = [0] * self.n_dma_sems
        ecount = {e: 0 for e in ENGS}
        di = 0
        for op in self.all_ops:
            if op.is_dma:
                s = di % self.n_dma_sems
                di += 1
                op.prewait = (dsems[s], dcount[s]) if dcount[s] > 0 else None
                dcount[s] += 16
                op.sem = dsems[s]
                op.val = dcount[s]
            elif op.signal:
                if ecount[op.eng] >= EPOCH:
                    esem[op.eng].append(nc.alloc_semaphore(f"s_{op.eng}{len(esem[op.eng])}"))
                    ecount[op.eng] = 0
                ecount[op.eng] += 1
                op.sem = esem[op.eng][-1]
                op.val = ecount[op.eng]
        final_wait_ops = list(final_wait_ops)

        def run(eng_name, e):
            known = {}
            for op in self.ops[eng_name]:
                waits = {}
                if op.prewait is not None:
                    waits[id(op.prewait[0])] = op.prewait
                for d in op.deps:
                    k = id(d.sem)
                    if k not in waits or waits[k][1] < d.val:
                        waits[k] = (d.sem, d.val)
                for k, (s, v) in waits.items():
                    if known.get(k, 0) >= v:
                        continue
                    e.wait_ge(s, v)
                    known[k] = v
                ins = op.fn(e)
                if op.is_dma:
                    ins.then_inc(op.sem, 16)
                elif op.signal:
                    ins.then_inc(op.sem, 1)
            if eng_name == "sp":
                for d in final_wait_ops:
                    e.wait_ge(d.sem, d.val)

        with nc.Block() as block:
            @block.tensor
            def _(e):
                run("pe", e)

            @block.scalar
            def _(e):
                run("act", e)

            @block.vector
            def _(e):
                run("dve", e)

            @block.gpsimd
            def _(e):
                run("pool", e)

            @block.sync
            def _(e):
                run("sp", e)


class Builder:
    def __init__(self, debug=(), stop=None, n_layers=L, small=False):
        self.small = small
        self.nc = nc = bass.Bass("TRN2", target_bir_lowering=False)
        self.P = Prog(nc)
        self.debug = set(debug)
        self.stop = stop
        self.n_layers = n_layers
        self.dbg_ops = []
        self.sb_base = 16512
        self.sb_top = 229376
        self.uid = 0
        self.inputs = {}
        self.build()

    def din(self, name, shape, dt=F32):
        if self.small and name in ("w_gate", "w_up", "w_down"):
            shape = [1, 1, 1, 1]
        t = self.nc.dram_tensor(name, list(shape), dt, kind="ExternalInput").ap()
        self.inputs[name] = t
        return t

    def dscr(self, name, shape, dt=F32):
        kind = "ExternalOutput" if name in self.debug else "Internal"
        return self.nc.dram_tensor(name, list(shape), dt, kind=kind).ap()

    def sb(self, off, shape, dt=F32, name=None):
        self.uid += 1
        n = f"{name or 't'}_{self.uid}"
        esz = 4 if dt in (F32, I32) else 2
        nbytes = int(np.prod(shape[1:])) * esz
        assert off + nbytes <= self.sb_top, (name, off, nbytes)
        t = self.nc.alloc_sbuf_tensor_at(n, list(shape), dt, offset=off)
        return t

    class Region:
        def __init__(self, bld, start, end):
            self.b, self.start, self.end, self.cur = bld, start, end, start
            self.bufs = []

        def alloc(self, shape, dt=F32, name=None):
            esz = 4 if dt in (F32, I32) else 2
            nbytes = int(np.prod(shape[1:])) * esz
            nbytes = (nbytes + 63) // 64 * 64
            off = self.cur
            assert off + nbytes <= self.end, (name, off, nbytes, self.end)
            self.cur += nbytes
            t = self.b.sb(off, shape, dt, name)
            buf = Buf(name or "")
            self.bufs.append(buf)
            return t, buf

    def barrier(self, old_bufs, new_bufs):
        op = self.P.add("dve", lambda e: e.engine_nop(), reads=[], writes=list(old_bufs))
        for b in new_bufs:
            b.last_w = op
            b.readers = {}
        op.signal = True

    def A(self, eng, fn, r, w):
        return self.P.add(eng, fn, r, w)

    def MM(self, out, lhsT, rhs, start, stop, r, w):
        return self.P.add("pe", lambda e: e.matmul(out, lhsT, rhs, start=start, stop=stop), r, w)

    def TR(self, out, in_, ident, r, w):
        return self.P.add("pe", lambda e: e.transpose(out, in_, ident), r, w)

    def ACT(self, out, in_, func, r, w, **kw):
        return self.P.add("act", lambda e: e.activation(out=out, in_=in_, func=func, **kw), r, w)

    def TT(self, eng, out, in0, in1, op, r, w):
        return self.P.add(eng, lambda e: e.tensor_tensor(out=out, in0=in0, in1=in1, op=op), r, w)

    def TS(self, eng, out, in0, s1, s2, op0, op1, r, w):
        if s2 is None:
            return self.P.add(eng, lambda e: e.tensor_scalar(out=out, in0=in0, scalar1=s1, scalar2=None, op0=op0), r, w)
        return self.P.add(eng, lambda e: e.tensor_scalar(out=out, in0=in0, scalar1=s1, scalar2=s2, op0=op0, op1=op1), r, w)

    def STT(self, eng, out, in0, scalar, in1, op0, op1, r, w):
        return self.P.add(eng, lambda e: e.scalar_tensor_tensor(out=out, in0=in0, scalar=scalar, in1=in1, op0=op0, op1=op1), r, w)

    def CP(self, eng, out, in_, r, w):
        if eng == "act":
            return self.ACT(out, in_, AF.Copy, r, w)
        return self.P.add(eng, lambda e: e.tensor_copy(out=out, in_=in_), r, w)

    def MS(self, eng, ap, val, w):
        return self.P.add(eng, lambda e: e.memset(ap, val), [], w)

    def DMA(self, eng, out, in_, r, w, **kw):
        return self.P.dma(eng, out, in_, r, w, **kw)

    def dbg(self, name, ap, shape, dt, bufs):
        if name not in self.debug:
            return
        o = self.nc.dram_tensor("dbg_" + name, list(shape), dt, kind="ExternalOutput").ap()
        self.dbg_ops.append(self.DMA("sp", o, ap, bufs, [Buf()]))

    def plan_load(self, src_view, view_fn):
        ent = {"src": src_view, "fn": view_fn, "res": None}
        self.wplan.append(ent)
        return ent

    def get_load(self, ent, lookahead=2):
        idx = self.wplan.index(ent)
        for e in self.wplan[:idx + 1 + lookahead]:
            if e["res"] is None:
                e["res"] = self.ring_load(e["src"], e["fn"])
        return ent["res"]

    def ring_load(self, src_view, view_fn):
        i = self.ring_i % len(self.ring)
        self.ring_i += 1
        t, b = self.ring[i]
        v = view_fn(t)
        self.DMA("pool", v, src_view, [self.wsrc], [b])
        return v, b

    def build(self):
        nc = self.nc
        P = self.P
        self.wsrc = Buf("weights")
        x_in = self.din("x", [S, D])
        c_col = self.din("c_col", [128, 16])
        ada_w = self.din("ada_w", [L, D, 6 * D])
        ada_b = self.din("ada_b", [L, 6 * D])
        norm1_g = self.din("norm1_g", [L, D])
        w_in = self.din("w_in", [L, D, INW])
        q_norm_g = self.din("q_norm_g", [L, 512])
        w_uq = self.din("w_uq", [L, 512, 1536])
        kv_norm_g = self.din("kv_norm_g", [L, 256])
        w_ukv = self.din("w_ukv", [L, 256, 2048])
        w_o = self.din("w_o", [L, D, D])
        norm2_g = self.din("norm2_g", [L, D])
        rgw = self.din("router_group_w", [L, D, 4])
        rgb = self.din("router_group_b", [L, 4])
        rew = self.din("router_expert_w", [L, D, 32])
        reb = self.din("router_expert_b", [L, 32])
        w_gate = self.din("w_gate", [L, NE, D, DE])
        w_up = self.din("w_up", [L, NE, D, DE])
        w_down = self.din("w_down", [L, NE, DE, D])
        fng = self.din("final_norm_g", [D])
        flag_in = self.din("flag", [128, 2])
        HS = S // 2
        out_d = nc.dram_tensor("out", [HS, D], F32, kind="ExternalOutput").ap()
        xsel_d = self.dscr("xsel_d", [HS, D])
        xsel_b = [Buf(f"xsel{t}") for t in range(NT // 2)]

        xres = self.dscr("xres", [S, D])
        xres_b = [Buf(f"xres{t}") for t in range(NT)]
        mod_d = self.dscr("mod_d", [L, 6 * D])
        mod_b = Buf("mod_d")
        cosF_d = self.dscr("cosF_d", [64, S]); sinF_d = self.dscr("sinF_d", [64, S])
        cosR_d = self.dscr("cosR_d", [128, NT, 64]); sinR_d = self.dscr("sinR_d", [128, NT, 64])
        tab_b = Buf("tables")
        kT_d = self.dscr("kT_d", [8, 128, S], BF16)
        kpe_d = self.dscr("kpe_d", [64, S], BF16)
        v_d = self.dscr("v_d", [8, 128, NT, 128], BF16)
        kv_b = [Buf(f"kv{g}") for g in range(NG)]

        R0 = self.Region(self, self.sb_base, self.sb_top)
        self.ring = []
        for i in range(4):
            t, b = R0.alloc([128, 8192], BF16, f"ring{i}")
            self.ring.append((t, b))
        self.ring_i = 0
        self.wplan = []
        identb, cb_ = R0.alloc([128, 128], BF16, "identb")
        identf, _ = R0.alloc([128, 128], F32, "identf")
        maskT, _ = R0.alloc([128, 128], F32, "maskT")
        maskD, _ = R0.alloc([128, 128], F32, "maskD")
        Gq, _ = R0.alloc([128, 8, 128], F32, "Gq")
        gk_col, _ = R0.alloc([128, 8], F32, "gk_col")
        g128, _ = R0.alloc([128, 8], F32, "g128")
        cst = Buf("consts")
        vecs, vecs_b = R0.alloc([128, 104], F32, "vecs")
        sc1c, _ = R0.alloc([128, 16], F32, "sc1c")
        sc2c, _ = R0.alloc([128, 16], F32, "sc2c")
        wr, wr_b = R0.alloc([128, 16, 36], F32, "wr")
        rbias, _ = R0.alloc([128, 36], F32, "rbias")
        Tst, Tst_b = R0.alloc([128, 8, 128], F32, "Tstate")
        gb, gb_b = R0.alloc([128, D], F32, "gb")
        silc, silc_b = R0.alloc([128, 16], BF16, "silc")
        flg, _ = R0.alloc([128, 2], F32, "flg")
        persist_end = R0.cur
        ps = [nc.alloc_psum_tensor(f"ps{i}", [128, 512], F32) for i in range(8)]
        psb = [Buf(f"ps{i}") for i in range(8)]
        self.ps, self.psb = ps, psb

        def psbf(i):
            return ps[i][:].bitcast(BF16)

        RC = self.Region(self, persist_end, self.sb_top)
        io_i, tb = RC.alloc([128, 128], I32, "io_i")
        io_f, _ = RC.alloc([128, 128], F32, "io_f")
        self.A("pool", lambda e: e.iota(io_i[:], pattern=[[1, 128]], base=0, channel_multiplier=-1), [], [tb])
        self.CP("dve", io_f[:], io_i[:], [tb], [tb])
        self.A("dve", lambda e: e.tensor_single_scalar(out=identf[:], in_=io_f[:], scalar=0.0, op=ALU.is_equal), [tb], [cst])
        self.CP("dve", identb[:], identf[:], [cst], [cst])
        self.A("dve", lambda e: e.tensor_single_scalar(out=maskT[:], in_=io_f[:], scalar=0.0, op=ALU.is_ge), [tb], [cst])
        self.MS("pool", maskD[:], 0.0, [cst])
        self.DMA("sp", flg[:], flag_in, [self.wsrc], [cst])
        self.MS("pool", maskD[0:64, 64:128], -30000.0, [cst])
        pp_i, _ = RC.alloc([128, 1], I32, "pp_i")
        pp1, _ = RC.alloc([128, 1], F32, "pp1")
        self.A("pool", lambda e: e.iota(pp_i[:], pattern=[[0, 1]], base=1, channel_multiplier=1), [], [tb])
        self.CP("dve", pp1[:], pp_i[:], [tb], [tb])
        lnrow, _ = RC.alloc([128, 8], F32, "lnrow")
        for h in range(8):
            self.MS("pool", lnrow[:, h:h + 1], LNG[h], [tb])
            self.MS("pool", g128[:, h:h + 1], math.exp(128.0 * LNG[h]), [cst])
        arg, _ = RC.alloc([128, 8], F32, "arg")
        self.TS("dve", arg[:], lnrow[:], pp1[:], None, ALU.mult, None, [tb], [tb])
        self.ACT(gk_col[:], arg[:], AF.Exp, [tb], [cst], scale=-1.0)
        self.TS("dve", gk_col[:], gk_col[:], 128.0 ** -0.5, None, ALU.mult, None, [cst], [cst])
        nrow_i, _ = RC.alloc([128, 128], I32, "nrow_i")
        nrow, _ = RC.alloc([128, 128], F32, "nrow")
        self.A("pool", lambda e: e.iota(nrow_i[:], pattern=[[1, 128]], base=1, channel_multiplier=0), [], [tb])
        self.CP("dve", nrow[:], nrow_i[:], [tb], [tb])
        for h in range(8):
            self.ACT(Gq[:, h, :], nrow[:], AF.Exp, [tb], [cst], scale=LNG[h])

        def sincos(ang, shape, sin_out, cos_out, tmpf, tmpi, tmpr):
            for (dst, shift) in ((sin_out, 0.0), (cos_out, math.pi / 2)):
                self.TS("dve", tmpf, ang, 1.0 / (2 * math.pi), shift / (2 * math.pi), ALU.mult, ALU.add, [tb], [tb])
                self.CP("dve", tmpi, tmpf, [tb], [tb])
                self.CP("dve", tmpf, tmpi, [tb], [tb])
                if shift:
                    self.TS("dve", tmpr, ang, shift, None, ALU.add, None, [tb], [tb])
                    self.STT("dve", tmpr, tmpf, -2 * math.pi, tmpr, ALU.mult, ALU.add, [tb], [tb])
                else:
                    self.STT("dve", tmpr, tmpf, -2 * math.pi, ang, ALU.mult, ALU.add, [tb], [tb])
                self.TS("dve", tmpr, tmpr, 3.14159, -3.14159, ALU.min, ALU.max, [tb], [tb])
                self.ACT(dst, tmpr, AF.Sin, [tb], [tb])

        pidx_i, _ = RC.alloc([128, 1], I32, "pidx_i")
        pidx, _ = RC.alloc([128, 1], F32, "pidx")
        self.A("pool", lambda e: e.iota(pidx_i[:], pattern=[[0, 1]], base=0, channel_multiplier=1), [], [tb])
        self.CP("dve", pidx[:], pidx_i[:], [tb], [tb])
        ge32, _ = RC.alloc([128, 1], F32, "ge32")
        self.A("dve", lambda e: e.tensor_single_scalar(out=ge32[:], in_=pidx[:], scalar=32.0, op=ALU.is_ge), [tb], [tb])
        self.STT("dve", pidx[:], ge32[:], -32.0, pidx[:], ALU.mult, ALU.add, [tb], [tb])
        invc, _ = RC.alloc([128, 1], F32, "invc")
        self.ACT(invc[:], pidx[:], AF.Exp, [tb], [tb], scale=-math.log(10000.0) / 32.0)
        CH = 1024
        tok_i, _ = RC.alloc([64, CH], I32, "tok_i")
        tokf, _ = RC.alloc([64, CH], F32, "tokf")
        angF, _ = RC.alloc([64, CH], F32, "angF")
        tmpf, _ = RC.alloc([64, CH], F32, "tmpf")
        tmpi, _ = RC.alloc([64, CH], I32, "tmpi")
        tmpr, _ = RC.alloc([64, CH], F32, "tmpr")
        sinc, _ = RC.alloc([64, CH], F32, "sinc")
        cosc, _ = RC.alloc([64, CH], F32, "cosc")
        for ci in range(S // CH):
            self.A("pool", lambda e, ci=ci: e.iota(tok_i[:], pattern=[[1, CH]], base=ci * CH, channel_multiplier=0), [], [tb])
            self.CP("dve", tokf[:], tok_i[:], [tb], [tb])
            self.TS("dve", angF[:], tokf[:], invc[0:64, :], None, ALU.mult, None, [tb], [tb])
            sincos(angF[:], None, sinc[:], cosc[:], tmpf[:], tmpi[:], tmpr[:])
            self.DMA("sp", sinF_d[:, ci * CH:(ci + 1) * CH], sinc[:], [tb], [tab_b])
            self.DMA("sp", cosF_d[:, ci * CH:(ci + 1) * CH], cosc[:], [tb], [tab_b])
        RC2 = self.Region(self, persist_end, self.sb_top)
        tb2 = Buf("tb2")
        self.barrier([tb], [tb2])
        tb = tb2
        tokc_i, _ = RC2.alloc([128, NT], I32, "tokc_i")
        tokc, _ = RC2.alloc([128, NT], F32, "tokc")
        self.A("pool", lambda e: e.iota(tokc_i[:], pattern=[[128, NT]], base=0, channel_multiplier=1), [], [tb])
        self.CP("dve", tokc[:], tokc_i[:], [tb], [tb])
        jr_i, _ = RC2.alloc([128, 64], I32, "jr_i")
        jr, _ = RC2.alloc([128, 64], F32, "jr")
        self.A("pool", lambda e: e.iota(jr_i[:], pattern=[[1, 64]], base=0, channel_multiplier=0), [], [tb])
        self.CP("dve", jr[:], jr_i[:], [tb], [tb])
        invr, _ = RC2.alloc([128, 64], F32, "invr")
        self.ACT(invr[:], jr[:], AF.Exp, [tb], [tb], scale=-math.log(10000.0) / 64.0)
        angR, _ = RC2.alloc([128, NT, 64], F32, "angR")
        self.TT("dve", angR[:], tokc[:].unsqueeze(2).broadcast_to([128, NT, 64]),
                invr[:].unsqueeze(1).broadcast_to([128, NT, 64]), ALU.mult, [tb], [tb])
        tmpf2, _ = RC2.alloc([128, NT, 64], F32, "tmpf2")
        tmpi2, _ = RC2.alloc([128, NT, 64], I32, "tmpi2")
        tmpr2, _ = RC2.alloc([128, NT, 64], F32, "tmpr2")
        sinR, _ = RC2.alloc([128, NT, 64], F32, "sinR")
        cosR, _ = RC2.alloc([128, NT, 64], F32, "cosR")
        sincos(angR[:], None, sinR[:], cosR[:], tmpf2[:], tmpi2[:], tmpr2[:])
        self.DMA("sp", sinR_d, sinR[:], [tb], [tab_b])
        self.DMA("sp", cosR_d, cosR[:], [tb], [tab_b])

        cc, _ = RC2.alloc([128, 16], F32, "cc")
        self.DMA("sp", cc[:], c_col, [self.wsrc], [tb])
        self.ACT(silc[:], cc[:], AF.Silu, [tb], [silc_b])
        brow = [RC2.alloc([1, 512], F32, f"brow{i}") for i in range(2)]
        mrow = [RC2.alloc([1, 512], F32, f"mrow{i}") for i in range(2)]
        nblk = 0
        for l in range(self.n_layers):
            for cbk in range(24):
                c0 = cbk * 512
                wv, wb_ = self.ring_load(
                    ada_w[l, :, c0:c0 + 512].rearrange("(k p) c -> p k c", p=128),
                    lambda t: t[:].rearrange("p (k c) -> p k c", k=16))
                br, brb = brow[nblk % 2]
                mr, mrb = mrow[nblk % 2]
                pi = nblk % 2
                nblk += 1
                self.DMA("sp", br[:], ada_b[l:l + 1, c0:c0 + 512], [self.wsrc], [brb])
                for k in range(16):
                    self.MM(ps[pi][0:1, :], silc[:, k:k + 1], wv[:, k, :], k == 0, k == 15, [silc_b, wb_], [psb[pi]])
                self.TT("dve", mr[:], ps[pi][0:1, :], br[:], ALU.add, [psb[pi], brb], [mrb])
                self.DMA("sp", mod_d[l:l + 1, c0:c0 + 512], mr[:], [mrb], [mod_b])
        self.dbg("mod", None, None, None, None)
        if self.stop == "mod":
            return self.finish([mod_b], out_d)

        idn = identb
        RM = self.Region(self, persist_end, self.sb_top)
        yT, yT_b = RM.alloc([128, 16, G], BF16, "yT")
        xn_g = self.sb(persist_end, [128, GT, D], BF16, "xn_g")
        cqnT, cqnT_b = RM.alloc([128, 4, G], BF16, "cqnT")
        cosF_g, ropeF_b = RM.alloc([64, G], F32, "cosF_g")
        sinF_g, _ = RM.alloc([64, G], F32, "sinF_g")
        cosR_g, ropeR_b = RM.alloc([128, GT, 64], F32, "cosR_g")
        sinR_g, _ = RM.alloc([128, GT, 64], F32, "sinR_g")
        nsinR_g, _ = RM.alloc([128, GT, 64], F32, "nsinR_g")
        ov0 = RM.cur
        RA = self.Region(self, ov0, self.sb_top)
        hT, hT_b = RA.alloc([128, 16, G], BF16, "hT")
        xt, xt_b = RA.alloc([128, D], F32, "xt")
        st = [RA.alloc([128, 1], F32, f"st{i}") for i in range(12)]
        junk, junk_b = RA.alloc([128, 512], BF16, "junk")
        cqn_g, cqn_b = RA.alloc([128, GT, 512], BF16, "cqn_g")
        ckvn_g, ckvn_b = RA.alloc([128, GT, 256], BF16, "ckvn_g")
        ckvnT, ckvnT_b = RA.alloc([128, 2, G], BF16, "ckvnT")
        kperot, kperot_b = RA.alloc([128, 16, 64], BF16, "kperot")
        kt1, kt1_b = RA.alloc([64, G], F32, "kt1")
        kt2, kt2_b = RA.alloc([64, G], F32, "kt2")
        kpeo, kpeo_b = RA.alloc([64, G], BF16, "kpeo")
        wukv, wukv_b = RA.alloc([128, 2, 2048], BF16, "wukv")
        kTo = [RA.alloc([128, G], BF16, f"kTo{i}") for i in range(2)]
        vo = [RA.alloc([128, 1024], BF16, f"vo{i}") for i in range(2)]
        q_r, q_r_b = RA.alloc([128, GT, 512], BF16, "q_r")
        k_p, k_p_b = RA.alloc([128, GT, 512], BF16, "k_p")
        v_r, v_r_b = RA.alloc([128, GT, 512], BF16, "v_r")
        g_r, g_r_b = RA.alloc([128, GT, 512], F32, "g_r")
        rt1 = [RA.alloc([128, 512], F32, f"rt1_{i}") for i in range(2)]
        rt2 = [RA.alloc([128, 512], F32, f"rt2_{i}") for i in range(1)] * 2
        rt3 = [RA.alloc([128, 512], F32, f"rt3_{i}") for i in range(1)] * 2
        qT4, qT4_b = RA.alloc([128, 4, 128], BF16, "qT4")
        kT4, kT4_b = RA.alloc([128, 4, 128], BF16, "kT4")
        sT4, sT4_b = RA.alloc([128, 4, 128], BF16, "sT4")
        Sf, Sf_b = RA.alloc([128, 4, 128], F32, "Sf")
        Sb, Sb_b = RA.alloc([128, 4, 128], BF16, "Sb")
        ysq, ysq_b = RA.alloc([128, 4, 128], F32, "ysq")
        yc, yc_b = RA.alloc([128, 4, 128], F32, "yc")
        yg, yg_b = RA.alloc([128, 512], BF16, "yg")
        s4 = [RA.alloc([128, 4], F32, f"s4_{i}") for i in range(8)]
        RB = self.Region(self, ov0, self.sb_top)
        stash, stash_b = RB.alloc([128, S], F32, "stash")
        kTh, kTh_b = RB.alloc([128, S], BF16, "kTh")
        vh, vh_b = RB.alloc([128, NT, 128], BF16, "vh")
        kpeT, kpeT_b = RB.alloc([64, S], BF16, "kpeT")
        wuq, wuq_b = RB.alloc([128, 4, 1536], BF16, "wuq")
        wrot, wrot_b = RB.alloc([128, 4, 8, 64], BF16, "wrot")
        qTh, qTh_b = RB.alloc([128, G], BF16, "qTh")
        qpeTh, qpeTh_b = RB.alloc([64, G], BF16, "qpeTh")
        qt1, qt1_b = RB.alloc([64, G], F32, "qt1")
        qt2, qt2_b = RB.alloc([64, G], F32, "qt2")
        Pb = [RB.alloc([128, 512], BF16, f"Pb{i}") for i in range(2)]
        PT = [RB.alloc([128, 4, 128], BF16, f"PT{i}") for i in range(2)]
        ytok, ytok_b = RB.alloc([128, GT, 1024], BF16, "ytok")
        bst = [RB.alloc([128, 1], F32, f"bst{i}") for i in range(6)]
        rs8 = [RB.alloc([128, 8], F32, f"rs8_{i}") for i in range(2)]
        rs8b = [RB.alloc([128, 8], F32, f"rs8b_{i}") for i in range(2)]
        RCo = self.Region(self, ov0, self.sb_top)
        xc = [RCo.alloc([128, 512], F32, f"xc{i}") for i in range(2)]
        xtmp = [RCo.alloc([128, 512], F32, f"xtmp{i}") for i in range(2)]
        xo = [RCo.alloc([128, 512], F32, f"xo{i}") for i in range(2)]
        RD = self.Region(self, persist_end, self.sb_top)
        h2T, h2T_b = RD.alloc([128, 16, MT * 128], BF16, "h2T")
        gates, gates_b = RD.alloc([128, MT, 32], F32, "gates")
        sa = [RD.alloc([128, 512], F32, f"sa{i}") for i in range(2)]
        hid = [RD.alloc([128, 512], BF16, f"hid{i}") for i in range(2)]
        hidT, hidT_b0 = RD.alloc([128, 4, MT * 128], BF16, "hidT")
        hidT_bs = [hidT_b0] + [Buf(f"hidT{i}") for i in range(1, MT)]
        RD.bufs.extend(hidT_bs[1:])
        dst = [RD.alloc([128, 1], F32, f"dst{i}") for i in range(16)]
        lg, lg_b = RD.alloc([128, 36], F32, "lg")
        rk = [RD.alloc([128, 32], F32, f"rk{i}") for i in range(6)]
        xc2 = [RD.alloc([128, 512], F32, f"xc2_{i}") for i in range(2)]
        xc3 = [RD.alloc([128, 512], F32, f"xc3_{i}") for i in range(2)]
        acc_off = RD.cur
        acc, acc_b = RD.alloc([128, MT, D], F32, "acc")
        RD2 = self.Region(self, acc_off, self.sb_top)
        xt2, xt2_b = RD2.alloc([128, D], F32, "xt2")
        xn2, xn2_b = RD2.alloc([128, D], F32, "xn2")
        h2f, h2f_b = RD2.alloc([128, 16, 128], F32, "h2f")
        xt2b, xt2b_b = RD2.alloc([128, D], F32, "xt2b")
        lgall, lgall_b = RD2.alloc([128, MT, 36], F32, "lgall")
        tk8 = [RD2.alloc([128, MT], F32, f"tk8_{i}")[0] for i in range(7)]
        tkg = [RD2.alloc([128, MT, 4], F32, f"tkg_{i}")[0] for i in range(2)]
        tke = [RD2.alloc([128, MT, 32], F32, f"tke_{i}")[0] for i in range(4)]
        if self.debug:
            print("SBUF map: persist_end", persist_end, "ov0", ov0, "A", RA.cur, "B", RB.cur, "C", RCo.cur, "D", RD.cur, "D2", RD2.cur)

        def rmsnorm_stats(src_ap, n, junk_ap, r, junk_w, sti):
            (ssq, ssq_b), (std, std_b), (rstd, rstd_b) = st[sti], st[sti + 1], st[sti + 2]
            self.ACT(junk_ap, src_ap, AF.Square, r, junk_w + [ssq_b], accum_out=ssq[:])
            self.ACT(std[:], ssq[:], AF.Sqrt, [ssq_b], [std_b], scale=1.0 / n, bias=EPS)
            self.A("dve", lambda e: e.reciprocal(out=rstd[:], in_=std[:]), [std_b], [rstd_b])
            return rstd, rstd_b

        alt = [0]

        def evac(out, in_, r, w):
            alt[0] ^= 1
            if alt[0]:
                return self.ACT(out, in_, AF.Copy, r, w)
            return self.CP("dve", out, in_, r, w)

        cur_overlay = [list(RC2.bufs) + [tb]]

        def switch(new_bufs):
            self.barrier(cur_overlay[0], new_bufs)
            cur_overlay[0] = list(new_bufs)

        final_ops = []
        for l in range(self.n_layers):
            last_layer = (l == self.n_layers - 1)
            RS_b = Buf("rows")
            switch([RS_b])
            rows = self.sb(ov0, [128, 128], F32, "rows")
            self.MS("pool", rows[:], 0.0, [RS_b])
            srcs = [(mod_d[l, 0:D], 16, [mod_b]), (mod_d[l, D:2 * D], 16, [mod_b]), (mod_d[l, 3 * D:4 * D], 16, [mod_b]),
                    (mod_d[l, 4 * D:5 * D], 16, [mod_b]), (norm1_g[l], 16, [self.wsrc]), (norm2_g[l], 16, [self.wsrc]),
                    (q_norm_g[l], 4, [self.wsrc]), (kv_norm_g[l], 2, [self.wsrc])]
            r0 = 0
            for (src, n, rb_) in srcs:
                self.DMA("sp", rows[r0:r0 + n, :], src.rearrange("(k c) -> k c", c=128), rb_, [RS_b])
                r0 += n
            self.TR(ps[0][:, 0:128], rows[:, :], identf[:], [RS_b, cst], [psb[0]])
            self.CP("dve", vecs[:, 0:104], ps[0][:, 0:104], [psb[0]], [vecs_b])
            sh1c, sh2c, qng, kvng = vecs[:, 0:16], vecs[:, 32:48], vecs[:, 96:100], vecs[:, 100:102]
            self.STT("dve", sc1c[:], vecs[:, 16:32], 1.0, vecs[:, 64:80], ALU.add, ALU.mult, [vecs_b], [vecs_b])
            self.STT("dve", sc2c[:], vecs[:, 48:64], 1.0, vecs[:, 80:96], ALU.add, ALU.mult, [vecs_b], [vecs_b])
            self.DMA("sp", wr[:, :, 0:4], rgw[l].rearrange("(k p) g -> p k g", p=128), [self.wsrc], [wr_b])
            self.DMA("sp", wr[:, :, 4:36], rew[l].rearrange("(k p) g -> p k g", p=128), [self.wsrc], [wr_b])
            self.DMA("sp", rbias[:, 0:4], rgb[l:l + 1, :].partition_broadcast(128), [self.wsrc], [wr_b])
            self.DMA("sp", rbias[:, 4:36], reb[l:l + 1, :].partition_broadcast(128), [self.wsrc], [wr_b])
            self.MS("pool", Tst[:], 0.0, [Tst_b])
            xsrc = x_in if l == 0 else xres

            def moe_pass(mp, sel):
                switch(RD.bufs[:-1] + RD2.bufs)
                for t in range(MT):
                    tile = mp * MT + t
                    self.DMA("sp", xt2[:], xres[tile * 128:(tile + 1) * 128, :], [xres_b[tile]], [xt2_b])
                    if sel:
                        tileB = NT // 2 + tile
                        self.DMA("sp", xt2b[:], xres[tileB * 128:(tileB + 1) * 128, :], [xres_b[tileB]], [xt2b_b])
                        self.TS("pool", xt2b[:], xt2b[:], flg[:, 1:2], None, ALU.mult, None, [xt2b_b, cst], [xt2b_b])
                        self.STT("dve", xt2[:], xt2[:], flg[:, 0:1], xt2b[:], ALU.mult, ALU.add, [xt2_b, xt2b_b, cst], [xt2_b])
                    (ssq, ssq_b), (std, std_b), (rstd, rstd_b) = dst[0:3]
                    self.ACT(xn2[:], xt2[:], AF.Square, [xt2_b], [xn2_b, ssq_b], accum_out=ssq[:])
                    self.ACT(std[:], ssq[:], AF.Sqrt, [ssq_b], [std_b], scale=1.0 / D, bias=EPS)
                    self.A("dve", lambda e, rstd=rstd, std=std: e.reciprocal(out=rstd[:], in_=std[:]), [std_b], [rstd_b])
                    self.ACT(xn2[:], xt2[:], AF.Copy, [xt2_b, rstd_b], [xn2_b], scale=rstd[:])
                    for k in range(16):
                        bk = k // 4
                        self.TR(ps[bk][:, (k % 4) * 128:(k % 4 + 1) * 128], xn2[:, k * 128:(k + 1) * 128], identf[:], [xn2_b, cst], [psb[bk]])
                        if k % 2 == 0:
                            self.ACT(h2f[:, k, :], ps[bk][:, (k % 4) * 128:(k % 4 + 1) * 128], AF.Identity, [psb[bk], vecs_b], [h2f_b], scale=sc2c[:, k:k + 1], bias=sh2c[:, k:k + 1])
                        else:
                            self.TS("dve", h2f[:, k, :], ps[bk][:, (k % 4) * 128:(k % 4 + 1) * 128], sc2c[:, k:k + 1], sh2c[:, k:k + 1], ALU.mult, ALU.add, [psb[bk], vecs_b], [h2f_b])
                    self.CP("pool", h2T[:, :, t * 128:(t + 1) * 128], h2f[:], [h2f_b], [h2T_b])
                    for k in range(16):
                        self.MM(ps[4][:, 0:36], h2f[:, k, :], wr[:, k, :], k == 0, k == 15, [h2f_b, wr_b], [psb[4]])
                    self.TT("dve", lgall[:, t, :], ps[4][:, 0:36], rbias[:], ALU.add, [psb[4], wr_b], [lgall_b])
                tk = Buf("topk")
                tk.last_w = lgall_b.last_w
                g4 = lgall[:, :, 0:4]
                e32 = lgall[:, :, 4:36]
                self.A("dve", lambda e: e.reduce_max(out=tk8[0][:], in_=g4, axis=AX.X), [lgall_b], [tk])
                self.TT("dve", tkg[0][:], g4, tk8[0][:].unsqueeze(2).broadcast_to([128, MT, 4]), ALU.subtract, [lgall_b, tk], [tk])
                self.ACT(tkg[1][:], tkg[0][:], AF.Exp, [tk], [tk])
                self.A("dve", lambda e: e.reduce_sum(out=tk8[1][:], in_=tkg[1][:], axis=AX.X), [tk], [tk])
                self.A("dve", lambda e: e.reciprocal(out=tk8[1][:], in_=tk8[1][:]), [tk], [tk])
                self.TS("dve", tkg[1][:], tkg[0][:], 0.0, None, ALU.is_ge, None, [tk], [tk])
                self.TS("dve", tkg[1][:], tkg[1][:], -1.0, 1e30, ALU.add, ALU.mult, [tk], [tk])
                self.TT("dve", tke[0][:].rearrange("p t (g e) -> p t g e", g=4), e32.rearrange("p t (g e) -> p t g e", g=4),
                        tkg[1][:].unsqueeze(3).broadcast_to([128, MT, 4, 8]), ALU.add, [lgall_b, tk], [tk])
                self.A("dve", lambda e: e.reduce_max(out=tk8[2][:], in_=tke[0][:], axis=AX.X), [tk], [tk])
                self.TT("dve", tke[1][:], tke[0][:], tk8[2][:].unsqueeze(2).broadcast_to([128, MT, 32]), ALU.subtract, [tk], [tk])
                self.TS("dve", tke[1][:], tke[1][:], 0.0, None, ALU.is_ge, None, [tk], [tk])
                self.STT("dve", tke[2][:], tke[1][:], -1e30, tke[0][:], ALU.mult, ALU.add, [tk], [tk])
                self.A("dve", lambda e: e.reduce_max(out=tk8[3][:], in_=tke[2][:], axis=AX.X), [tk], [tk])
                self.TT("dve", tke[3][:], tke[2][:], tk8[3][:].unsqueeze(2).broadcast_to([128, MT, 32]), ALU.subtract, [tk], [tk])
                self.TS("dve", tke[3][:], tke[3][:], 0.0, None, ALU.is_ge, None, [tk], [tk])
                self.TT("dve", tk8[4][:], tk8[3][:], tk8[2][:], ALU.subtract, [tk], [tk])
                self.ACT(tk8[4][:], tk8[4][:], AF.Exp, [tk], [tk])
                self.TS("dve", tk8[4][:], tk8[4][:], 1.0, None, ALU.add, None, [tk], [tk])
                self.A("dve", lambda e: e.reciprocal(out=tk8[4][:], in_=tk8[4][:]), [tk], [tk])
                self.TT("dve", tk8[5][:], tk8[4][:], tk8[1][:], ALU.mult, [tk], [tk])
                self.TT("dve", tk8[6][:], tk8[1][:], tk8[5][:], ALU.subtract, [tk], [tk])
                self.TT("dve", tke[1][:], tke[1][:], tk8[5][:].unsqueeze(2).broadcast_to([128, MT, 32]), ALU.mult, [tk], [tk])
                self.TT("dve", tke[3][:], tke[3][:], tk8[6][:].unsqueeze(2).broadcast_to([128, MT, 32]), ALU.mult, [tk], [tk])
                self.TT("dve", gates[:], tke[1][:], tke[3][:], ALU.add, [tk], [gates_b, tk])
                if l == 0 and mp == 0:
                    self.dbg("gates", gates[:], [128, MT, 32], F32, [gates_b])
                    self.dbg("h2T", h2T[:], [128, 16, MT * 128], BF16, [h2T_b])
                if self.stop == "router":
                    self.finish([gates_b, h2T_b], out_d)
                    return "stop"
                self.barrier(RD2.bufs, [acc_b])
                cur_overlay[0] = list(RD.bufs)

                def load_wg(e):
                    return self.ring_load(w_gate[l, e].rearrange("(k p) f -> p k f", p=128), slot16)

                def load_wu(e):
                    return self.ring_load(w_up[l, e].rearrange("(k p) f -> p k f", p=128), slot16)

                def load_wd(e):
                    return self.ring_load(w_down[l, e].rearrange("(k p) d -> p k d", p=128), lambda tt: tt[:].rearrange("p (k d) -> p k d", k=4))

                nxt = [load_wg(0), load_wu(0), load_wd(0)]
                for e in range(NE):
                    (wg, wgb), (wu, wub), (wd, wdb) = nxt
                    if e + 1 < NE:
                        nxt = [load_wg(e + 1), None, None]

                    def au_mm(t):
                        pa, pu = (0, 1) if t % 2 == 0 else (2, 3)
                        for k in range(16):
                            self.MM(ps[pa][:], h2T[:, k, t * 128:(t + 1) * 128], wg[:, k, :], k == 0, k == 15, [h2T_b, wgb], [psb[pa]])
                        for k in range(16):
                            self.MM(ps[pu][:], h2T[:, k, t * 128:(t + 1) * 128], wu[:, k, :], k == 0, k == 15, [h2T_b, wub], [psb[pu]])
                        sa_, sab = sa[t % 2]
                        hd_, hdb = hid[t % 2]
                        self.ACT(sa_[:], ps[pa][:], AF.Silu, [psb[pa]], [sab])
                        self.STT("dve", hd_[:], ps[pu][:], gates[:, t, e:e + 1], sa_[:], ALU.mult, ALU.mult, [psb[pu], gates_b, sab], [hdb])

                    def au_tr(t):
                        hd_, hdb = hid[t % 2]
                        bk = 4 + (t % 2)
                        for i in range(4):
                            self.TR(psbf(bk)[:, i * 128:(i + 1) * 128], hd_[:, i * 128:(i + 1) * 128], idn[:], [hdb, cst], [psb[bk]])
                        self.ACT(hidT[:, :, t * 128:(t + 1) * 128], psbf(bk)[:, 0:512].rearrange("p (i n) -> p i n", i=4), AF.Copy, [psb[bk]], [hidT_bs[t]])

                    for t in range(MT):
                        au_mm(t)
                        if t >= 1:
                            au_tr(t - 1)
                    au_tr(MT - 1)
                    if e + 1 < NE:
                        nxt[1] = load_wu(e + 1)
                        nxt[2] = load_wd(e + 1)
                    for t in range(MT):
                        b0 = 0 if t % 2 == 0 else 4
                        for dbk in range(4):
                            for fc in range(4):
                                self.MM(ps[b0 + dbk][:], hidT[:, fc, t * 128:(t + 1) * 128], wd[:, fc, dbk * 512:(dbk + 1) * 512], fc == 0, fc == 3, [hidT_bs[t], wdb], [psb[b0 + dbk]])
                        for dbk in range(4):
                            dst_ap = acc[:, t, dbk * 512:(dbk + 1) * 512]
                            if e == 0:
                                self.CP("dve", dst_ap, ps[b0 + dbk][:], [psb[b0 + dbk]], [acc_b])
                            else:
                                self.TT("dve", dst_ap, ps[b0 + dbk][:], dst_ap, ALU.add, [psb[b0 + dbk], acc_b], [acc_b])
                self.DMA("sp", gb[:], mod_d[l:l + 1, 5 * D:6 * D].partition_broadcast(128), [mod_b], [gb_b])
                n7 = 0
                for t in range(MT):
                    tile = mp * MT + t
                    for cbk in range(4):
                        xc_, xcb = xc2[n7 % 2]
                        xd_, xdb = xc3[n7 % 2]
                        n7 += 1
                        cs = slice(cbk * 512, (cbk + 1) * 512)
                        self.DMA("sp", xc_[:], xres[tile * 128:(tile + 1) * 128, cs], [xres_b[tile]], [xcb])
                        if sel:
                            tileB = NT // 2 + tile
                            self.DMA("sp", xd_[:], xres[tileB * 128:(tileB + 1) * 128, cs], [xres_b[tileB]], [xdb])
                            self.TS("dve", xd_[:], xd_[:], flg[:, 1:2], None, ALU.mult, None, [xdb, cst], [xdb])
                            self.STT("dve", xc_[:], xc_[:], flg[:, 0:1], xd_[:], ALU.mult, ALU.add, [xcb, xdb, cst], [xcb])
                        self.TT("pool", acc[:, t, cs], acc[:, t, cs], gb[:, cs], ALU.mult, [acc_b, gb_b], [acc_b])
                        self.TT("dve", xc_[:], xc_[:], acc[:, t, cs], ALU.add, [xcb, acc_b], [xcb])
                        if sel:
                            self.DMA("sp", xsel_d[tile * 128:(tile + 1) * 128, cs], xc_[:], [xcb], [xsel_b[tile]])
                        else:
                            self.DMA("sp", xres[tile * 128:(tile + 1) * 128, cs], xc_[:], [xcb], [xres_b[tile]])
                if self.stop == "moe":
                    if sel:
                        self.dbg("xout", xsel_d[0:MT * 128, :], [MT * 128, D], F32, xsel_b[0:MT])
                        self.finish(xsel_b[mp * MT:(mp + 1) * MT], out_d)
                    else:
                        self.dbg("xout", xres[0:MT * 128, :], [MT * 128, D], F32, xres_b[0:MT])
                        self.finish(xres_b[mp * MT:(mp + 1) * MT], out_d)
                    return "stop"
                return None


            for g in range(NG):
                switch(RA.bufs + [yT_b, cqnT_b, ropeF_b, ropeR_b])

                def slot16(tt):
                    return tt[:].rearrange("p (k c) -> p k c", k=16)

                self.wplan = []
                pl_cq = self.plan_load(w_in[l, :, 0:512].rearrange("(k p) c -> p k c", p=128), slot16)
                pl_ckv = self.plan_load(w_in[l, :, 512:832].rearrange("(k p) c -> p k c", p=128),
                                        lambda tt: tt[:, 0:16 * 320].rearrange("p (k c) -> p k c", k=16))
                pl_ret = {}
                for hh_ in range(2):
                    for kind_, cbase_ in (("q", 832), ("k", 1856), ("v", 2880), ("g", 3904)):
                        c0_ = cbase_ + hh_ * 512
                        pl_ret[(hh_, kind_)] = self.plan_load(w_in[l, :, c0_:c0_ + 512].rearrange("(k p) c -> p k c", p=128), slot16)
                pl_wo = [self.plan_load(w_o[l, :, cb_ * 512:(cb_ + 1) * 512].rearrange("(k p) c -> p k c", p=128), slot16) for cb_ in range(4)]
                self.DMA("sp", cosF_g[:], cosF_d[:, g * G:(g + 1) * G], [tab_b], [ropeF_b])
                self.DMA("sp", sinF_g[:], sinF_d[:, g * G:(g + 1) * G], [tab_b], [ropeF_b])
                self.DMA("sp", cosR_g[:], cosR_d[:, g * GT:(g + 1) * GT, :], [tab_b], [ropeR_b])
                self.DMA("sp", sinR_g[:], sinR_d[:, g * GT:(g + 1) * GT, :], [tab_b], [ropeR_b])
                self.TS("dve", nsinR_g[:], sinR_g[:], -1.0, None, ALU.mult, None, [ropeR_b], [ropeR_b])
                self.DMA("pool", wukv[:], w_ukv[l].rearrange("(k p) c -> p k c", p=128), [self.wsrc], [wukv_b])
                for t in range(GT):
                    tile = g * GT + t
                    rsrc = [self.wsrc] if l == 0 else [xres_b[tile]]
                    self.DMA("sp", xt[:], xsrc[tile * 128:(tile + 1) * 128, :], rsrc, [xt_b])
                    rstd, rstd_b = rmsnorm_stats(xt[:], D, xn_g[:, t, :], [xt_b], [yT_b], 0)
                    self.ACT(xn_g[:, t, :], xt[:], AF.Copy, [xt_b, rstd_b], [yT_b], scale=rstd[:])
                for k in range(16):
                    bk = 2 + (k % 2)
                    for t in range(GT):
                        self.TR(psbf(bk)[:, t * 128:(t + 1) * 128], xn_g[:, t, k * 128:(k + 1) * 128], idn[:], [yT_b, cst], [psb[bk]])
                    if k % 2 == 0:
                        self.ACT(hT[:, k, :], psbf(bk)[:, 0:G], AF.Identity, [psb[bk], vecs_b], [hT_b], scale=sc1c[:, k:k + 1], bias=sh1c[:, k:k + 1])
                    else:
                        self.TS("dve", hT[:, k, :], psbf(bk)[:, 0:G], sc1c[:, k:k + 1], sh1c[:, k:k + 1], ALU.mult, ALU.add, [psb[bk], vecs_b], [hT_b])
                if l == 0 and g == 0:
                    self.dbg("hT", hT[:], [128, 16, G], BF16, [hT_b])
                if self.stop == "hT":
                    return self.finish([hT_b], out_d)

                wv, wb_ = self.get_load(pl_cq)
                for t in range(GT):
                    pi = t % 2
                    for k in range(16):
                        self.MM(ps[pi][:], hT[:, k, t * 128:(t + 1) * 128], wv[:, k, :], k == 0, k == 15, [hT_b, wb_], [psb[pi]])
                    rstd, rstd_b = rmsnorm_stats(ps[pi][:], 512, junk[:], [psb[pi]], [junk_b], 3 * (t % 2))
                    self.ACT(cqn_g[:, t, :], ps[pi][:], AF.Copy, [psb[pi], rstd_b], [cqn_b], scale=rstd[:])
                for kc in range(4):
                    bk = 2 + (kc % 2)
                    for t in range(GT):
                        self.TR(psbf(bk)[:, t * 128:(t + 1) * 128], cqn_g[:, t, kc * 128:(kc + 1) * 128], idn[:], [cqn_b, cst], [psb[bk]])
                    self.TS("dve", cqnT[:, kc, :], psbf(bk)[:, 0:G], qng[:, kc:kc + 1], None, ALU.mult, None, [psb[bk], vecs_b], [cqnT_b])
                wv, wb_ = self.get_load(pl_ckv)
                for t in range(GT):
                    pi = t % 2
                    for k in range(16):
                        self.MM(ps[pi][:, 0:256], hT[:, k, t * 128:(t + 1) * 128], wv[:, k, 0:256], k == 0, k == 15, [hT_b, wb_], [psb[pi]])
                    rstd, rstd_b = rmsnorm_stats(ps[pi][:, 0:256], 256, junk[:, 0:256], [psb[pi]], [junk_b], 3 * (t % 2))
                    self.ACT(ckvn_g[:, t, :], ps[pi][:, 0:256], AF.Copy, [psb[pi], rstd_b], [ckvn_b], scale=rstd[:])
                for kc in range(2):
                    bk = 2 + (kc % 2)
                    for t in range(GT):
                        self.TR(psbf(bk)[:, t * 128:(t + 1) * 128], ckvn_g[:, t, kc * 128:(kc + 1) * 128], idn[:], [ckvn_b, cst], [psb[bk]])
                    self.TS("dve", ckvnT[:, kc, :], psbf(bk)[:, 0:G], kvng[:, kc:kc + 1], None, ALU.mult, None, [psb[bk], vecs_b], [ckvnT_b])
                self.TS("dve", kperot[:, :, 0:32], wv[:, :, 288:320], -1.0, None, ALU.mult, None, [wb_], [kperot_b])
                self.CP("dve", kperot[:, :, 32:64], wv[:, :, 256:288], [wb_], [kperot_b])
                for k in range(16):
                    self.MM(ps[0][0:64, :], wv[:, k, 256:320], hT[:, k, :], k == 0, k == 15, [hT_b, wb_], [psb[0]])
                for k in range(16):
                    self.MM(ps[1][0:64, :], kperot[:, k, :], hT[:, k, :], k == 0, k == 15, [hT_b, kperot_b], [psb[1]])
                self.TT("dve", kt1[:], ps[0][0:64, :], cosF_g[:], ALU.mult, [psb[0], ropeF_b], [kt1_b])
                self.TT("dve", kt2[:], ps[1][0:64, :], sinF_g[:], ALU.mult, [psb[1], ropeF_b], [kt2_b])
                self.TT("dve", kpeo[:], kt1[:], kt2[:], ALU.add, [kt1_b, kt2_b], [kpeo_b])
                self.DMA("sp", kpe_d[:, g * G:(g + 1) * G], kpeo[:], [kpeo_b], [kv_b[g]])
                for h in range(8):
                    pi = h % 2
                    for kc in range(2):
                        self.MM(ps[pi][:], wukv[:, kc, h * 256:h * 256 + 128], ckvnT[:, kc, :], kc == 0, kc == 1, [wukv_b, ckvnT_b], [psb[pi]])
                    kt_, ktb_ = kTo[h % 2]
                    evac(kt_[:], ps[pi][:], [psb[pi]], [ktb_])
                    self.DMA("sp", kT_d[h, :, g * G:(g + 1) * G], kt_[:], [ktb_], [kv_b[g]])
                wukv_v = wukv[:].rearrange("p k (h c) -> p k h c", h=8)
                for t in range(GT):
                    tile = g * GT + t
                    vt_, vtb_ = vo[t % 2]
                    for hf in range(2):
                        pi = hf
                        for kc in range(2):
                            self.MM(ps[pi][:].rearrange("p (h c) -> p h c", h=4), ckvnT[:, kc, t * 128:(t + 1) * 128],
                                    wukv_v[:, kc, 4 * hf:4 * hf + 4, 128:256], kc == 0, kc == 1, [wukv_b, ckvnT_b], [psb[pi]])
                        evac(vt_[:, hf * 512:(hf + 1) * 512], ps[pi][:], [psb[pi]], [vtb_])
                    self.DMA("sp", v_d[:, :, tile, :].rearrange("h p d -> p h d"), vt_[:].rearrange("p (h d) -> p h d", h=8), [vtb_], [kv_b[g]])
                if l == 0 and g == 0:
                    self.dbg("cqnT", cqnT[:], [128, 4, G], BF16, [cqnT_b])
                    self.dbg("ckvnT", ckvnT[:], [128, 2, G], BF16, [ckvnT_b])
                if self.stop == "kv":
                    return self.finish([kv_b[g], cqnT_b], out_d)

                for hh in range(2):
                    for kind, cbase in (("q", 832), ("k", 1856), ("v", 2880), ("g", 3904)):
                        wv, wb_ = self.get_load(pl_ret[(hh, kind)])
                        for t in range(GT):
                            pi = t % 2
                            for k in range(16):
                                self.MM(ps[pi][:], hT[:, k, t * 128:(t + 1) * 128], wv[:, k, :], k == 0, k == 15, [hT_b, wb_], [psb[pi]])
                            if kind in ("q", "k"):
                                (a1, a1b), (a2, a2b), (a3, a3b) = rt1[t % 2], rt2[t % 2], rt3[t % 2]
                                psv = ps[pi][:].rearrange("p (h two j) -> p h two j", h=4, two=2)
                                a1v = a1[:].rearrange("p (h two j) -> p h two j", h=4, two=2)
                                a2v = a2[:].rearrange("p (h two j) -> p h two j", h=4, two=2)
                                cosb = cosR_g[:, t, :].unsqueeze(1).unsqueeze(1).broadcast_to([128, 4, 2, 64])
                                sinb = sinR_g[:, t, :].unsqueeze(1).broadcast_to([128, 4, 64])
                                nsinb = nsinR_g[:, t, :].unsqueeze(1).broadcast_to([128, 4, 64])
                                self.TT("dve", a1v, psv, cosb, ALU.mult, [psb[pi], ropeR_b], [a1b])
                                self.TT("dve", a2v[:, :, 0, :], psv[:, :, 1, :], nsinb, ALU.mult, [psb[pi], ropeR_b], [a2b])
                                self.TT("dve", a2v[:, :, 1, :], psv[:, :, 0, :], sinb, ALU.mult, [psb[pi], ropeR_b], [a2b])
                                if kind == "q":
                                    self.TT("pool", q_r[:, t, :], a1[:], a2[:], ALU.add, [a1b, a2b], [q_r_b])
                                else:
                                    self.TT("pool", a3[:], a1[:], a2[:], ALU.add, [a1b, a2b], [a3b])
                                    self.TT("pool", k_p[:, t, :].rearrange("p (h d) -> p h d", h=4), a3[:].rearrange("p (h d) -> p h d", h=4),
                                            gk_col[:, 4 * hh:4 * hh + 4].unsqueeze(2).broadcast_to([128, 4, 128]), ALU.mult, [a3b, cst], [k_p_b])
                            elif kind == "v":
                                self.ACT(v_r[:, t, :], ps[pi][:], AF.Copy, [psb[pi]], [v_r_b])
                            else:
                                self.ACT(g_r[:, t, :], ps[pi][:], AF.Silu, [psb[pi]], [g_r_b])
                    if l == 0 and g == 0 and hh == 0:
                        self.dbg("q_r", q_r[:], [128, GT, 512], BF16, [q_r_b])
                        self.dbg("k_p", k_p[:], [128, GT, 512], BF16, [k_p_b])
                    if self.stop == "qk":
                        return self.finish([q_r_b, k_p_b, v_r_b, g_r_b], out_d)
                    for t in range(GT):
                        for j in range(4):
                            self.TR(psbf(4)[:, j * 128:(j + 1) * 128], q_r[:, t, j * 128:(j + 1) * 128], idn[:], [q_r_b, cst], [psb[4]])
                        for j in range(4):
                            self.TR(psbf(4)[:, 512 + j * 128:512 + (j + 1) * 128], k_p[:, t, j * 128:(j + 1) * 128], idn[:], [k_p_b, cst], [psb[4]])
                        self.ACT(ysq[:], psbf(4)[:, 0:512].rearrange("p (h n) -> p h n", h=4), AF.Copy, [psb[4]], [ysq_b])
                        self.TT("dve", qT4[:], ysq[:], Gq[:, 4 * hh:4 * hh + 4, :], ALU.mult, [ysq_b, cst], [qT4_b])
                        self.ACT(kT4[:], psbf(4)[:, 512:1024].rearrange("p (h n) -> p h n", h=4), AF.Copy, [psb[4]], [kT4_b])
                        if self.stop == "r1":
                            return self.finish([qT4_b, kT4_b], out_d)
                        for j in range(4):
                            self.MM(ps[5][:, j * 128:(j + 1) * 128], kT4[:, j, :], qT4[:, j, :], True, True, [kT4_b, qT4_b], [psb[5]])
                        self.TT("dve", sT4[:], ps[5][:].rearrange("p (h n) -> p h n", h=4), maskT[:].unsqueeze(1).broadcast_to([128, 4, 128]), ALU.mult, [psb[5], cst], [sT4_b])
                        self.TT("pool", Sf[:], Tst[:, 4 * hh:4 * hh + 4, :], g128[:, 4 * hh:4 * hh + 4].unsqueeze(2).broadcast_to([128, 4, 128]), ALU.mult, [Tst_b, cst], [Sf_b])
                        self.CP("pool", Sb[:], Sf[:], [Sf_b], [Sb_b])
                        if self.stop == "r2":
                            return self.finish([sT4_b, Sb_b], out_d)
                        for j in range(4):
                            self.MM(ps[6][:, j * 128:(j + 1) * 128], sT4[:, j, :], v_r[:, t, j * 128:(j + 1) * 128], True, False, [sT4_b, v_r_b], [psb[6]])
                            self.MM(ps[6][:, j * 128:(j + 1) * 128], qT4[:, j, :], Sb[:, j, :], False, True, [qT4_b, Sb_b], [psb[6]])
                        for j in range(4):
                            self.MM(ps[7][:, j * 128:(j + 1) * 128], k_p[:, t, j * 128:(j + 1) * 128], v_r[:, t, j * 128:(j + 1) * 128], True, True, [k_p_b, v_r_b], [psb[7]])
                        self.TT("dve", Tst[:, 4 * hh:4 * hh + 4, :], ps[7][:].rearrange("p (h n) -> p h n", h=4), Sf[:], ALU.add, [Sf_b, psb[7]], [Tst_b])
                        if self.stop == "r3":
                            return self.finish([Tst_b, psb[6]], out_d)
                        y3 = ps[6][:].rearrange("p (h n) -> p h n", h=4)
                        (s1, s1b), (s2, s2b), (mean, meanb), (msq, msqb), (var, varb), (sd, sdb), (rs_, rsb) = s4[0:7]
                        self.A("dve", lambda e, s1=s1, y3=y3: e.reduce_sum(out=s1[:], in_=y3, axis=AX.X), [psb[6]], [s1b])
                        self.ACT(ysq[:], y3, AF.Square, [psb[6]], [ysq_b])
                        self.A("dve", lambda e, s2=s2: e.reduce_sum(out=s2[:], in_=ysq[:], axis=AX.X), [ysq_b], [s2b])
                        self.TS("dve", mean[:], s1[:], 1.0 / 128, None, ALU.mult, None, [s1b], [meanb])
                        self.TT("dve", msq[:], mean[:], mean[:], ALU.mult, [meanb], [msqb])
                        self.STT("dve", var[:], s2[:], 1.0 / 128, msq[:], ALU.mult, ALU.subtract, [s2b, msqb], [varb])
                        self.ACT(sd[:], var[:], AF.Sqrt, [varb], [sdb], bias=EPS)
                        self.A("dve", lambda e, rs_=rs_, sd=sd: e.reciprocal(out=rs_[:], in_=sd[:]), [sdb], [rsb])
                        self.TT("dve", yc[:], y3, mean[:].unsqueeze(2).broadcast_to([128, 4, 128]), ALU.subtract, [psb[6], meanb], [yc_b])
                        self.TT("pool", yc[:], yc[:], rs_[:].unsqueeze(2).broadcast_to([128, 4, 128]), ALU.mult, [yc_b, rsb], [yc_b])
                        self.TT("pool", yg[:], yc[:].rearrange("p h n -> p (h n)"), g_r[:, t, :], ALU.mult, [yc_b, g_r_b], [yg_b])
                        if self.stop == "r4":
                            return self.finish([yg_b], out_d)
                        for j in range(4):
                            self.TR(psbf(3)[:, j * 128:(j + 1) * 128], yg[:, j * 128:(j + 1) * 128], idn[:], [yg_b, cst], [psb[3]])
                        self.ACT(yT[:, 8 + 4 * hh:12 + 4 * hh, t * 128:(t + 1) * 128], psbf(3)[:, 0:512].rearrange("p (h n) -> p h n", h=4), AF.Copy, [psb[3]], [yT_b])
                if l == 0 and g == 0:
                    self.dbg("yT_ret", yT[:, 8:16, :], [128, 8, G], BF16, [yT_b])
                if self.stop == "ret":
                    return self.finish([yT_b, kv_b[g]], out_d)

                switch(RB.bufs)
                nk = (g + 1) * G
                nkt = nk // 128
                SC = 192.0 ** -0.5
                self.DMA("pool", wuq[:], w_uq[l].rearrange("(k p) c -> p k c", p=128), [self.wsrc], [wuq_b])
                wuq_v = wuq[:].rearrange("p k (h c) -> p k h c", h=8)
                self.TS("dve", wrot[:, :, :, 0:32], wuq_v[:, :, :, 160:192], -1.0, None, ALU.mult, None, [wuq_b], [wrot_b])
                self.CP("dve", wrot[:, :, :, 32:64], wuq_v[:, :, :, 128:160], [wuq_b], [wrot_b])
                self.DMA("sp", kpeT[:, 0:nk], kpe_d[:, 0:nk], kv_b[:g + 1], [kpeT_b])
                for h in range(8):
                    self.DMA("sp", kTh[:, 0:nk], kT_d[h, :, 0:nk], kv_b[:g + 1], [kTh_b])
                    self.DMA("sp", vh[:, 0:nkt, :], v_d[h, :, 0:nkt, :], kv_b[:g + 1], [vh_b])
                    for kc in range(4):
                        self.MM(ps[0][:], wuq[:, kc, h * 192:h * 192 + 128], cqnT[:, kc, :], kc == 0, kc == 3, [wuq_b, cqnT_b], [psb[0]])
                    self.ACT(qTh[:], ps[0][:], AF.Copy, [psb[0]], [qTh_b])
                    for kc in range(4):
                        self.MM(ps[1][0:64, :], wuq[:, kc, h * 192 + 128:h * 192 + 192], cqnT[:, kc, :], kc == 0, kc == 3, [wuq_b, cqnT_b], [psb[1]])
                    for kc in range(4):
                        self.MM(ps[2][0:64, :], wrot[:, kc, h, :], cqnT[:, kc, :], kc == 0, kc == 3, [wrot_b, cqnT_b], [psb[2]])
                    self.TT("dve", qt1[:], ps[1][0:64, :], cosF_g[:], ALU.mult, [psb[1], ropeF_b], [qt1_b])
                    self.TT("dve", qt2[:], ps[2][0:64, :], sinF_g[:], ALU.mult, [psb[2], ropeF_b], [qt2_b])
                    self.TT("dve", qpeTh[:], qt1[:], qt2[:], ALU.add, [qt1_b, qt2_b], [qpeTh_b])
                    for t in range(GT):
                        J = g * GT + t
                        nkeys = (J + 1) * 128
                        nblk = (nkeys + 511) // 512
                        bmx, bmxb = rs8b[t % 2]
                        for kb in range(nblk):
                            w = min(512, nkeys - kb * 512)
                            bk = 3 + (kb % 2)
                            self.MM(ps[bk][:, 0:w], qTh[:, t * 128:(t + 1) * 128], kTh[:, kb * 512:kb * 512 + w], True, False, [qTh_b, kTh_b], [psb[bk]])
                            self.MM(ps[bk][:, 0:w], qpeTh[:, t * 128:(t + 1) * 128], kpeT[:, kb * 512:kb * 512 + w], False, True, [qpeTh_b, kpeT_b], [psb[bk]])
                            if kb == nblk - 1:
                                if w > 128:
                                    evac(stash[:, kb * 512:kb * 512 + w - 128], ps[bk][:, 0:w - 128], [psb[bk]], [stash_b])
                                self.TT("dve", stash[:, nkeys - 128:nkeys], ps[bk][:, w - 128:w], maskD[:], ALU.add, [psb[bk], cst], [stash_b])
                                self.A("dve", lambda e, kb=kb, w=w, bmx=bmx: e.reduce_max(out=bmx[:, kb:kb + 1], in_=stash[:, kb * 512:kb * 512 + w], axis=AX.X), [stash_b], [bmxb])
                            else:
                                evac(stash[:, kb * 512:kb * 512 + w], ps[bk][:, 0:w], [psb[bk]], [stash_b])
                                self.A("dve", lambda e, kb=kb, bmx=bmx: e.reduce_max(out=bmx[:, kb:kb + 1], in_=stash[:, kb * 512:(kb + 1) * 512], axis=AX.X), [stash_b], [bmxb])
                        (mx, mxb), (nb, nbb), (rsum, rsumb), (rinv, rinvb) = bst[0:4]
                        rs_t, rs_tb = rs8[t % 2]
                        self.A("dve", lambda e, mx=mx, nblk=nblk, bmx=bmx: e.reduce_max(out=mx[:], in_=bmx[:, 0:nblk], axis=AX.X), [bmxb], [mxb])
                        self.TS("dve", nb[:], mx[:], -SC, None, ALU.mult, None, [mxb], [nbb])
                        ob = 6 if t % 2 == 0 else 2
                        for kb in range(nblk):
                            w = min(512, nkeys - kb * 512)
                            pbt, pbb = Pb[kb % 2]
                            ptt, ptb = PT[kb % 2]
                            tbk = 5 if kb % 2 == 0 else 7
                            self.ACT(pbt[:, 0:w], stash[:, kb * 512:kb * 512 + w], AF.Exp, [stash_b, nbb], [pbb, rs_tb], bias=nb[:], scale=SC, accum_out=rs_t[:, kb:kb + 1])
                            nti = w // 128
                            for i in range(nti):
                                self.TR(psbf(tbk)[:, i * 128:(i + 1) * 128], pbt[:, i * 128:(i + 1) * 128], idn[:], [pbb, cst], [psb[tbk]])
                            if kb % 2 == 0:
                                self.CP("dve", ptt[:, 0:nti, :], psbf(tbk)[:, 0:nti * 128].rearrange("p (i n) -> p i n", i=nti), [psb[tbk]], [ptb])
                            else:
                                self.ACT(ptt[:, 0:nti, :], psbf(tbk)[:, 0:nti * 128].rearrange("p (i n) -> p i n", i=nti), AF.Copy, [psb[tbk]], [ptb])
                            for i in range(nti):
                                kt_i = kb * 4 + i
                                self.MM(ps[ob][:, 0:128], ptt[:, i, :], vh[:, kt_i, :], kt_i == 0, kt_i == J, [ptb, vh_b], [psb[ob]])
                        self.A("dve", lambda e, rsum=rsum, rs_t=rs_t, nblk=nblk: e.reduce_sum(out=rsum[:], in_=rs_t[:, 0:nblk], axis=AX.X), [rs_tb], [rsumb])
                        self.A("dve", lambda e, rinv=rinv, rsum=rsum: e.reciprocal(out=rinv[:], in_=rsum[:]), [rsumb], [rinvb])
                        self.ACT(ytok[:, t, h * 128:(h + 1) * 128], ps[ob][:, 0:128], AF.Copy, [psb[ob], rinvb], [ytok_b], scale=rinv[:])
                for t in range(GT):
                    bk = 0 if t % 2 == 0 else 1
                    for h in range(8):
                        self.TR(psbf(bk)[:, h * 128:(h + 1) * 128], ytok[:, t, h * 128:(h + 1) * 128], idn[:], [ytok_b, cst], [psb[bk]])
                    evac(yT[:, 0:8, t * 128:(t + 1) * 128], psbf(bk)[:, 0:1024].rearrange("p (h n) -> p h n", h=8), [psb[bk]], [yT_b])
                if l == 0 and g == 0:
                    self.dbg("yT", yT[:], [128, 16, G], BF16, [yT_b])
                if self.stop == "attn" or (self.stop == "attn1" and g == 1):
                    return self.finish([yT_b], out_d)

                switch(RCo.bufs)
                self.DMA("sp", gb[:], mod_d[l:l + 1, 2 * D:3 * D].partition_broadcast(128), [mod_b], [gb_b])
                n6 = 0
                if self.stop == "w6a":
                    return self.finish([gb_b], out_d)
                for cbk in range(4):
                    wv, wb_ = self.get_load(pl_wo[cbk])
                    for t in range(GT):
                        tile = g * GT + t
                        pi = n6 % 2
                        (xc_, xcb), (xm_, xmb), (xo_, xob) = xc[pi], xtmp[pi], xo[pi]
                        n6 += 1
                        rsrc = [self.wsrc] if l == 0 else [xres_b[tile]]
                        self.DMA("sp", xc_[:], xsrc[tile * 128:(tile + 1) * 128, cbk * 512:(cbk + 1) * 512], rsrc, [xcb])
                        for k in range(16):
                            self.MM(ps[pi][:], yT[:, k, t * 128:(t + 1) * 128], wv[:, k, :], k == 0, k == 15, [yT_b, wb_], [psb[pi]])
                        self.TT("dve", xm_[:], ps[pi][:], gb[:, cbk * 512:(cbk + 1) * 512], ALU.mult, [psb[pi], gb_b], [xmb])
                        self.TT("pool", xo_[:], xm_[:], xc_[:], ALU.add, [xmb, xcb], [xob])
                        if self.stop == "w6b":
                            return self.finish([xob], out_d)
                        self.DMA("sp", xres[tile * 128:(tile + 1) * 128, cbk * 512:(cbk + 1) * 512], xo_[:], [xob], [xres_b[tile]])
                if self.stop == "wo":
                    self.dbg("xmid", xres[0:G, :], [G, D], F32, xres_b[0:GT])
                    return self.finish(xres_b[g * GT:(g + 1) * GT], out_d)
                if self.stop == "wo_all" and g == NG - 1:
                    return self.finish(xres_b, out_d)

                if last_layer or (g + 1) % (MT // GT) != 0:
                    continue
                if moe_pass((g + 1) // (MT // GT) - 1, False) == "stop":
                    return self
            if last_layer:
                for mp in range(NMP // 2):
                    if moe_pass(mp, True) == "stop":
                        return self

        RF_b = [Buf("fin_x"), Buf("fin_o")]
        switch(RF_b + [gb_b])
        fx = [self.sb(persist_end + i * 8192, [128, D], F32, f"fx{i}") for i in range(2)]
        fo = [self.sb(persist_end + 16384 + i * 8192, [128, D], F32, f"fo{i}") for i in range(2)]
        fxb = [Buf(), Buf()]
        fob = [Buf(), Buf()]
        fst = [(self.sb(persist_end + 40000 + i * 64, [128, 1], F32, f"fst{i}"), Buf()) for i in range(6)]
        for b in fxb + fob + [x[1] for x in fst]:
            b.last_w = RF_b[0].last_w
        self.DMA("sp", gb[:], fng.rearrange("(o d) -> o d", o=1).partition_broadcast(128), [self.wsrc], [gb_b])
        outs = []
        for tile in range(NT // 2):
            i = tile % 2
            self.DMA("sp", fx[i][:], xsel_d[tile * 128:(tile + 1) * 128, :], [xsel_b[tile]], [fxb[i]])
            (ssq, ssq_b), (std, std_b), (rstd, rstd_b) = fst[3 * i:3 * i + 3]
            self.ACT(fo[i][:], fx[i][:], AF.Square, [fxb[i]], [fob[i], ssq_b], accum_out=ssq[:])
            self.ACT(std[:], ssq[:], AF.Sqrt, [ssq_b], [std_b], scale=1.0 / D, bias=EPS)
            self.A("dve", lambda e, rstd=rstd, std=std: e.reciprocal(out=rstd[:], in_=std[:]), [std_b], [rstd_b])
            self.STT("dve", fo[i][:], fx[i][:], rstd[:], gb[:], ALU.mult, ALU.mult, [fxb[i], rstd_b, gb_b], [fob[i]])
            outs.append(self.DMA("sp", out_d[tile * 128:(tile + 1) * 128, :], fo[i][:], [fob[i]], [Buf()]))
        self.P.emit(final_wait_ops=outs + self.dbg_ops)
        return self

    def finish(self, bufs, out_d):
        t = self.sb(self.sb_top - 4096, [128, 8], F32, "fin")
        b = Buf()
        self.MS("pool", t[:], 0.0, [b])
        op = self.DMA("sp", out_d[0:128, 0:8], t[:], [b] + list(bufs), [Buf()])
        self.P.emit(final_wait_ops=[op] + self.dbg_ops)
        return self


def _core_role(cidx):
    die, idx = cidx // 4, cidx % 4
    return die * 2 + idx % 2, idx // 2


def _in_maps(inputs, n_cores=8):
    shared = {k: np.ascontiguousarray(np.asarray(v, dtype=np.float32)) for k, v in inputs.items() if k not in ("x", "c")}
    maps = []
    for cidx in range(n_cores):
        b, half = _core_role(cidx)
        m = dict(shared)
        xb = np.asarray(inputs["x"][b], dtype=np.float32)
        if half == 0:
            xs = np.zeros_like(xb)
            xs[:S // 2] = xb[:S // 2]
            xb = xs
        m["x"] = np.ascontiguousarray(xb)
        m["c_col"] = np.ascontiguousarray(np.asarray(inputs["c"][b], dtype=np.float32).reshape(16, 128).T)
        fl = np.zeros((128, 2), np.float32)
        fl[:, half] = 1.0
        m["flag"] = fl
        maps.append(m)
    return maps


def kernel(**inputs):
    bld = Builder()
    maps = _in_maps(inputs)
    res = run_bass_kernel_spmd(bld.nc, maps, core_ids=list(range(8)))
    out = np.zeros((4, S, D), np.float32)
    for cidx in range(8):
        b, half = _core_role(cidx)
        out[b, half * (S // 2):(half + 1) * (S // 2)] = np.asarray(res.results[cidx]["out"], dtype=np.float32)
    return out
```

```python
import math
import numpy as np
import concourse.bass as bass
import concourse.mybir as mybir
from concourse.bass_utils import run_bass_kernel_spmd

F32 = mybir.dt.float32
BF16 = mybir.dt.bfloat16
I32 = mybir.dt.int32
ALU = mybir.AluOpType
AF = mybir.ActivationFunctionType
AX = mybir.AxisListType

ENGS = ("pe", "act", "dve", "pool", "sp")

D = 2048
S = 4096
L = 2
NT = S // 128
GT = 4
G = GT * 128
NG = S // G
MT = 8
NMP = NT // MT
INW = 4928
NE = 32
DE = 512
EPS = 1e-6
LNG = [math.log1p(-2.0 ** (-5.0 - h)) for h in range(8)]


class Buf:
    __slots__ = ("name", "last_w", "readers")

    def __init__(self, name=""):
        self.name = name
        self.last_w = None
        self.readers = {}


class Op:
    __slots__ = ("eng", "fn", "deps", "is_dma", "sem", "val", "signal", "prewait")

    def __init__(self, eng, fn, is_dma):
        self.eng = eng
        self.fn = fn
        self.deps = []
        self.is_dma = is_dma
        self.sem = None
        self.val = 0
        self.signal = False
        self.prewait = None


class Prog:
    def __init__(self, nc, n_dma_sems=48):
        self.nc = nc
        self.ops = {e: [] for e in ENGS}
        self.n_dma_sems = n_dma_sems
        self.all_ops = []

    def add(self, eng, fn, reads=(), writes=(), dma=False):
        op = Op(eng, fn, dma)
        deps = {}
        for b in reads:
            if b.last_w is not None:
                deps[id(b.last_w)] = b.last_w
        for b in writes:
            if b.last_w is not None:
                deps[id(b.last_w)] = b.last_w
            for r in b.readers.values():
                deps[id(r)] = r
        for d in deps.values():
            if d is op:
                continue
            if d.eng == "pe" and eng == "pe" and not d.is_dma and not dma:
                continue
            op.deps.append(d)
            d.signal = True
        for b in reads:
            b.readers[(eng, dma, id(op) if dma else 0)] = op
        for b in writes:
            b.last_w = op
            b.readers = {}
        self.ops[eng].append(op)
        self.all_ops.append(op)
        return op

    def dma(self, eng, out, in_, reads=(), writes=(), **kw):
        return self.add(eng, lambda e: e.dma_start(out=out, in_=in_, **kw), reads, writes, dma=True)

    def emit(self, final_wait_ops=()):
        nc = self.nc
        EPOCH = 16000
        esem = {e: [nc.alloc_semaphore(f"s_{e}0")] for e in ENGS}
        dsems = [nc.alloc_semaphore(f"d_{i}") for i in range(self.n_dma_sems)]
        dcount = [0] * self.n_dma_sems
        ecount = {e: 0 for e in ENGS}
        di = 0
        for op in self.all_ops:
            if op.is_dma:
                s = di % self.n_dma_sems
                di += 1
                op.prewait = (dsems[s], dcount[s]) if dcount[s] > 0 else None
                dcount[s] += 16
                op.sem = dsems[s]
                op.val = dcount[s]
            elif op.signal:
                if ecount[op.eng] >= EPOCH:
                    esem[op.eng].append(nc.alloc_semaphore(f"s_{op.eng}{len(esem[op.eng])}"))
                    ecount[op.eng] = 0
                ecount[op.eng] += 1
                op.sem = esem[op.eng][-1]
                op.val = ecount[op.eng]
        final_wait_ops = list(final_wait_ops)

        def run(eng_name, e):
            known = {}
            for op in self.ops[eng_name]:
                waits = {}
                if op.prewait is not None:
                    waits[id(op.prewait[0])] = op.prewait
                for d in op.deps:
                    k = id(d.sem)
                    if k not in waits or waits[k][1] < d.val:
                        waits[k] = (d.sem, d.val)
                for k, (s, v) in waits.items():
                    if known.get(k, 0) >= v:
                        continue
                    e.wait_ge(s, v)
                    known[k] = v
                ins = op.fn(e)
                if op.is_dma:
                    ins.then_inc(op.sem, 16)
                elif op.signal:
                    ins.then_inc(op.sem, 1)
            if eng_name == "sp":
                for d in final_wait_ops:
                    e.wait_ge(d.sem, d.val)

        with nc.Block() as block:
            @block.tensor
            def _(e):
                run("pe", e)

            @block.scalar
            def _(e):
                run("act", e)

            @block.vector
            def _(e):
                run("dve", e)

            @block.gpsimd
            def _(e):
                run("pool", e)

            @block.sync
            def _(e):
                run("sp", e)


class Builder:
    def __init__(self, debug=(), stop=None, n_layers=L, small=False):
        self.small = small
        self.nc = nc = bass.Bass("TRN2", target_bir_lowering=False)
        self.P = Prog(nc)
        self.debug = set(debug)
        self.stop = stop
        self.n_layers = n_layers
        self.dbg_ops = []
        self.sb_base = 16512
        self.sb_top = 229376
        self.uid = 0
        self.inputs = {}
        self.build()

    def din(self, name, shape, dt=F32):
        if self.small and name in ("w_gate", "w_up", "w_down"):
            shape = [1, 1, 1, 1]
        t = self.nc.dram_tensor(name, list(shape), dt, kind="ExternalInput").ap()
        self.inputs[name] = t
        return t

    def dscr(self, name, shape, dt=F32):
        kind = "ExternalOutput" if name in self.debug else "Internal"
        return self.nc.dram_tensor(name, list(shape), dt, kind=kind).ap()

    def sb(self, off, shape, dt=F32, name=None):
        self.uid += 1
        n = f"{name or 't'}_{self.uid}"
        esz = 4 if dt in (F32, I32) else 2
        nbytes = int(np.prod(shape[1:])) * esz
        assert off + nbytes <= self.sb_top, (name, off, nbytes)
        t = self.nc.alloc_sbuf_tensor_at(n, list(shape), dt, offset=off)
        return t

    class Region:
        def __init__(self, bld, start, end):
            self.b, self.start, self.end, self.cur = bld, start, end, start
            self.bufs = []

        def alloc(self, shape, dt=F32, name=None):
            esz = 4 if dt in (F32, I32) else 2
            nbytes = int(np.prod(shape[1:])) * esz
            nbytes = (nbytes + 63) // 64 * 64
            off = self.cur
            assert off + nbytes <= self.end, (name, off, nbytes, self.end)
            self.cur += nbytes
            t = self.b.sb(off, shape, dt, name)
            buf = Buf(name or "")
            self.bufs.append(buf)
            return t, buf

    def barrier(self, old_bufs, new_bufs):
        op = self.P.add("dve", lambda e: e.engine_nop(), reads=[], writes=list(old_bufs))
        for b in new_bufs:
            b.last_w = op
            b.readers = {}
        op.signal = True

    def A(self, eng, fn, r, w):
        return self.P.add(eng, fn, r, w)

    def MM(self, out, lhsT, rhs, start, stop, r, w):
        return self.P.add("pe", lambda e: e.matmul(out, lhsT, rhs, start=start, stop=stop), r, w)

    def TR(self, out, in_, ident, r, w):
        return self.P.add("pe", lambda e: e.transpose(out, in_, ident), r, w)

    def ACT(self, out, in_, func, r, w, **kw):
        return self.P.add("act", lambda e: e.activation(out=out, in_=in_, func=func, **kw), r, w)

    def TT(self, eng, out, in0, in1, op, r, w):
        return self.P.add(eng, lambda e: e.tensor_tensor(out=out, in0=in0, in1=in1, op=op), r, w)

    def TS(self, eng, out, in0, s1, s2, op0, op1, r, w):
        if s2 is None:
            return self.P.add(eng, lambda e: e.tensor_scalar(out=out, in0=in0, scalar1=s1, scalar2=None, op0=op0), r, w)
        return self.P.add(eng, lambda e: e.tensor_scalar(out=out, in0=in0, scalar1=s1, scalar2=s2, op0=op0, op1=op1), r, w)

    def STT(self, eng, out, in0, scalar, in1, op0, op1, r, w):
        return self.P.add(eng, lambda e: e.scalar_tensor_tensor(out=out, in0=in0, scalar=scalar, in1=in1, op0=op0, op1=op1), r, w)

    def CP(self, eng, out, in_, r, w):
        if eng == "act":
            return self.ACT(out, in_, AF.Copy, r, w)
        return self.P.add(eng, lambda e: e.tensor_copy(out=out, in_=in_), r, w)

    def MS(self, eng, ap, val, w):
        return self.P.add(eng, lambda e: e.memset(ap, val), [], w)

    def DMA(self, eng, out, in_, r, w, **kw):
        return self.P.dma(eng, out, in_, r, w, **kw)

    def dbg(self, name, ap, shape, dt, bufs):
        if name not in self.debug:
            return
        o = self.nc.dram_tensor("dbg_" + name, list(shape), dt, kind="ExternalOutput").ap()
        self.dbg_ops.append(self.DMA("sp", o, ap, bufs, [Buf()]))

    def plan_load(self, src_view, view_fn):
        ent = {"src": src_view, "fn": view_fn, "res": None}
        self.wplan.append(ent)
        return ent

    def get_load(self, ent, lookahead=2):
        idx = self.wplan.index(ent)
        for e in self.wplan[:idx + 1 + lookahead]:
            if e["res"] is None:
                e["res"] = self.ring_load(e["src"], e["fn"])
        return ent["res"]

    def ring_load(self, src_view, view_fn):
        i = self.ring_i % len(self.ring)
        self.ring_i += 1
        t, b = self.ring[i]
        v = view_fn(t)
        self.DMA("pool", v, src_view, [self.wsrc], [b])
        return v, b

    def build(self):
        nc = self.nc
        P = self.P
        self.wsrc = Buf("weights")
        x_in = self.din("x", [S, D])
        c_col = self.din("c_col", [128, 16])
        ada_w = self.din("ada_w", [L, D, 6 * D])
        ada_b = self.din("ada_b", [L, 6 * D])
        norm1_g = self.din("norm1_g", [L, D])
        w_in = self.din("w_in", [L, D, INW])
        q_norm_g = self.din("q_norm_g", [L, 512])
        w_uq = self.din("w_uq", [L, 512, 1536])
        kv_norm_g = self.din("kv_norm_g", [L, 256])
        w_ukv = self.din("w_ukv", [L, 256, 2048])
        w_o = self.din("w_o", [L, D, D])
        norm2_g = self.din("norm2_g", [L, D])
        rgw = self.din("router_group_w", [L, D, 4])
        rgb = self.din("router_group_b", [L, 4])
        rew = self.din("router_expert_w", [L, D, 32])
        reb = self.din("router_expert_b", [L, 32])
        w_gate = self.din("w_gate", [L, NE, D, DE])
        w_up = self.din("w_up", [L, NE, D, DE])
        w_down = self.din("w_down", [L, NE, DE, D])
        fng = self.din("final_norm_g", [D])
        flag_in = self.din("flag", [128, 2])
        HS = S // 2
        out_d = nc.dram_tensor("out", [HS, D], F32, kind="ExternalOutput").ap()
        xsel_d = self.dscr("xsel_d", [HS, D])
        xsel_b = [Buf(f"xsel{t}") for t in range(NT // 2)]

        xres = self.dscr("xres", [S, D])
        xres_b = [Buf(f"xres{t}") for t in range(NT)]
        mod_d = self.dscr("mod_d", [L, 6 * D])
        mod_b = Buf("mod_d")
        cosF_d = self.dscr("cosF_d", [64, S]); sinF_d = self.dscr("sinF_d", [64, S])
        cosR_d = self.dscr("cosR_d", [128, NT, 64]); sinR_d = self.dscr("sinR_d", [128, NT, 64])
        tab_b = Buf("tables")
        kT_d = self.dscr("kT_d", [8, 128, S], BF16)
        kpe_d = self.dscr("kpe_d", [64, S], BF16)
        v_d = self.dscr("v_d", [8, 128, NT, 128], BF16)
        kv_b = [Buf(f"kv{g}") for g in range(NG)]

        R0 = self.Region(self, self.sb_base, self.sb_top)
        self.ring = []
        for i in range(4):
            t, b = R0.alloc([128, 8192], BF16, f"ring{i}")
            self.ring.append((t, b))
        self.ring_i = 0
        self.wplan = []
        identb, cb_ = R0.alloc([128, 128], BF16, "identb")
        identf, _ = R0.alloc([128, 128], F32, "identf")
        maskT, _ = R0.alloc([128, 128], F32, "maskT")
        maskD, _ = R0.alloc([128, 128], F32, "maskD")
        Gq, _ = R0.alloc([128, 8, 128], F32, "Gq")
        gk_col, _ = R0.alloc([128, 8], F32, "gk_col")
        g128, _ = R0.alloc([128, 8], F32, "g128")
        cst = Buf("consts")
        vecs, vecs_b = R0.alloc([128, 104], F32, "vecs")
        sc1c, _ = R0.alloc([128, 16], F32, "sc1c")
        sc2c, _ = R0.alloc([128, 16], F32, "sc2c")
        wr, wr_b = R0.alloc([128, 16, 36], F32, "wr")
        rbias, _ = R0.alloc([128, 36], F32, "rbias")
        Tst, Tst_b = R0.alloc([128, 8, 128], F32, "Tstate")
        gb, gb_b = R0.alloc([128, D], F32, "gb")
        silc, silc_b = R0.alloc([128, 16], BF16, "silc")
        flg, _ = R0.alloc([128, 2], F32, "flg")
        persist_end = R0.cur
        ps = [nc.alloc_psum_tensor(f"ps{i}", [128, 512], F32) for i in range(8)]
        psb = [Buf(f"ps{i}") for i in range(8)]
        self.ps, self.psb = ps, psb

        def psbf(i):
            return ps[i][:].bitcast(BF16)

        RC = self.Region(self, persist_end, self.sb_top)
        io_i, tb = RC.alloc([128, 128], I32, "io_i")
        io_f, _ = RC.alloc([128, 128], F32, "io_f")
        self.A("pool", lambda e: e.iota(io_i[:], pattern=[[1, 128]], base=0, channel_multiplier=-1), [], [tb])
        self.CP("dve", io_f[:], io_i[:], [tb], [tb])
        self.A("dve", lambda e: e.tensor_single_scalar(out=identf[:], in_=io_f[:], scalar=0.0, op=ALU.is_equal), [tb], [cst])
        self.CP("dve", identb[:], identf[:], [cst], [cst])
        self.A("dve", lambda e: e.tensor_single_scalar(out=maskT[:], in_=io_f[:], scalar=0.0, op=ALU.is_ge), [tb], [cst])
        self.MS("pool", maskD[:], 0.0, [cst])
        self.DMA("sp", flg[:], flag_in, [self.wsrc], [cst])
        self.MS("pool", maskD[0:64, 64:128], -30000.0, [cst])
        pp_i, _ = RC.alloc([128, 1], I32, "pp_i")
        pp1, _ = RC.alloc([128, 1], F32, "pp1")
        self.A("pool", lambda e: e.iota(pp_i[:], pattern=[[0, 1]], base=1, channel_multiplier=1), [], [tb])
        self.CP("dve", pp1[:], pp_i[:], [tb], [tb])
        lnrow, _ = RC.alloc([128, 8], F32, "lnrow")
        for h in range(8):
            self.MS("pool", lnrow[:, h:h + 1], LNG[h], [tb])
            self.MS("pool", g128[:, h:h + 1], math.exp(128.0 * LNG[h]), [cst])
        arg, _ = RC.alloc([128, 8], F32, "arg")
        self.TS("dve", arg[:], lnrow[:], pp1[:], None, ALU.mult, None, [tb], [tb])
        self.ACT(gk_col[:], arg[:], AF.Exp, [tb], [cst], scale=-1.0)
        self.TS("dve", gk_col[:], gk_col[:], 128.0 ** -0.5, None, ALU.mult, None, [cst], [cst])
        nrow_i, _ = RC.alloc([128, 128], I32, "nrow_i")
        nrow, _ = RC.alloc([128, 128], F32, "nrow")
        self.A("pool", lambda e: e.iota(nrow_i[:], pattern=[[1, 128]], base=1, channel_multiplier=0), [], [tb])
        self.CP("dve", nrow[:], nrow_i[:], [tb], [tb])
        for h in range(8):
            self.ACT(Gq[:, h, :], nrow[:], AF.Exp, [tb], [cst], scale=LNG[h])

        def sincos(ang, shape, sin_out, cos_out, tmpf, tmpi, tmpr):
            for (dst, shift) in ((sin_out, 0.0), (cos_out, math.pi / 2)):
                self.TS("dve", tmpf, ang, 1.0 / (2 * math.pi), shift / (2 * math.pi), ALU.mult, ALU.add, [tb], [tb])
                self.CP("dve", tmpi, tmpf, [tb], [tb])
                self.CP("dve", tmpf, tmpi, [tb], [tb])
                if shift:
                    self.TS("dve", tmpr, ang, shift, None, ALU.add, None, [tb], [tb])
                    self.STT("dve", tmpr, tmpf, -2 * math.pi, tmpr, ALU.mult, ALU.add, [tb], [tb])
                else:
                    self.STT("dve", tmpr, tmpf, -2 * math.pi, ang, ALU.mult, ALU.add, [tb], [tb])
                self.TS("dve", tmpr, tmpr, 3.14159, -3.14159, ALU.min, ALU.max, [tb], [tb])
                self.ACT(dst, tmpr, AF.Sin, [tb], [tb])

        pidx_i, _ = RC.alloc([128, 1], I32, "pidx_i")
        pidx, _ = RC.alloc([128, 1], F32, "pidx")
        self.A("pool", lambda e: e.iota(pidx_i[:], pattern=[[0, 1]], base=0, channel_multiplier=1), [], [tb])
        self.CP("dve", pidx[:], pidx_i[:], [tb], [tb])
        ge32, _ = RC.alloc([128, 1], F32, "ge32")
        self.A("dve", lambda e: e.tensor_single_scalar(out=ge32[:], in_=pidx[:], scalar=32.0, op=ALU.is_ge), [tb], [tb])
        self.STT("dve", pidx[:], ge32[:], -32.0, pidx[:], ALU.mult, ALU.add, [tb], [tb])
        invc, _ = RC.alloc([128, 1], F32, "invc")
        self.ACT(invc[:], pidx[:], AF.Exp, [tb], [tb], scale=-math.log(10000.0) / 32.0)
        CH = 1024
        tok_i, _ = RC.alloc([64, CH], I32, "tok_i")
        tokf, _ = RC.alloc([64, CH], F32, "tokf")
        angF, _ = RC.alloc([64, CH], F32, "angF")
        tmpf, _ = RC.alloc([64, CH], F32, "tmpf")
        tmpi, _ = RC.alloc([64, CH], I32, "tmpi")
        tmpr, _ = RC.alloc([64, CH], F32, "tmpr")
        sinc, _ = RC.alloc([64, CH], F32, "sinc")
        cosc, _ = RC.alloc([64, CH], F32, "cosc")
        for ci in range(S // CH):
            self.A("pool", lambda e, ci=ci: e.iota(tok_i[:], pattern=[[1, CH]], base=ci * CH, channel_multiplier=0), [], [tb])
            self.CP("dve", tokf[:], tok_i[:], [tb], [tb])
            self.TS("dve", angF[:], tokf[:], invc[0:64, :], None, ALU.mult, None, [tb], [tb])
            sincos(angF[:], None, sinc[:], cosc[:], tmpf[:], tmpi[:], tmpr[:])
            self.DMA("sp", sinF_d[:, ci * CH:(ci + 1) * CH], sinc[:], [tb], [tab_b])
            self.DMA("sp", cosF_d[:, ci * CH:(ci + 1) * CH], cosc[:], [tb], [tab_b])
        RC2 = self.Region(self, persist_end, self.sb_top)
        tb2 = Buf("tb2")
        self.barrier([tb], [tb2])
        tb = tb2
        tokc_i, _ = RC2.alloc([128, NT], I32, "tokc_i")
        tokc, _ = RC2.alloc([128, NT], F32, "tokc")
        self.A("pool", lambda e: e.iota(tokc_i[:], pattern=[[128, NT]], base=0, channel_multiplier=1), [], [tb])
        self.CP("dve", tokc[:], tokc_i[:], [tb], [tb])
        jr_i, _ = RC2.alloc([128, 64], I32, "jr_i")
        jr, _ = RC2.alloc([128, 64], F32, "jr")
        self.A("pool", lambda e: e.iota(jr_i[:], pattern=[[1, 64]], base=0, channel_multiplier=0), [], [tb])
        self.CP("dve", jr[:], jr_i[:], [tb], [tb])
        invr, _ = RC2.alloc([128, 64], F32, "invr")
        self.ACT(invr[:], jr[:], AF.Exp, [tb], [tb], scale=-math.log(10000.0) / 64.0)
        angR, _ = RC2.alloc([128, NT, 64], F32, "angR")
        self.TT("dve", angR[:], tokc[:].unsqueeze(2).broadcast_to([128, NT, 64]),
                invr[:].unsqueeze(1).broadcast_to([128, NT, 64]), ALU.mult, [tb], [tb])
        tmpf2, _ = RC2.alloc([128, NT, 64], F32, "tmpf2")
        tmpi2, _ = RC2.alloc([128, NT, 64], I32, "tmpi2")
        tmpr2, _ = RC2.alloc([128, NT, 64], F32, "tmpr2")
        sinR, _ = RC2.alloc([128, NT, 64], F32, "sinR")
        cosR, _ = RC2.alloc([128, NT, 64], F32, "cosR")
        sincos(angR[:], None, sinR[:], cosR[:], tmpf2[:], tmpi2[:], tmpr2[:])
        self.DMA("sp", sinR_d, sinR[:], [tb], [tab_b])
        self.DMA("sp", cosR_d, cosR[:], [tb], [tab_b])

        cc, _ = RC2.alloc([128, 16], F32, "cc")
        self.DMA("sp", cc[:], c_col, [self.wsrc], [tb])
        self.ACT(silc[:], cc[:], AF.Silu, [tb], [silc_b])
        brow = [RC2.alloc([1, 512], F32, f"brow{i}") for i in range(2)]
        mrow = [RC2.alloc([1, 512], F32, f"mrow{i}") for i in range(2)]
        nblk = 0
        for l in range(self.n_layers):
            for cbk in range(24):
                c0 = cbk * 512
                wv, wb_ = self.ring_load(
                    ada_w[l, :, c0:c0 + 512].rearrange("(k p) c -> p k c", p=128),
                    lambda t: t[:].rearrange("p (k c) -> p k c", k=16))
                br, brb = brow[nblk % 2]
                mr, mrb = mrow[nblk % 2]
                pi = nblk % 2
                nblk += 1
                self.DMA("sp", br[:], ada_b[l:l + 1, c0:c0 + 512], [self.wsrc], [brb])
                for k in range(16):
                    self.MM(ps[pi][0:1, :], silc[:, k:k + 1], wv[:, k, :], k == 0, k == 15, [silc_b, wb_], [psb[pi]])
                self.TT("dve", mr[:], ps[pi][0:1, :], br[:], ALU.add, [psb[pi], brb], [mrb])
                self.DMA("sp", mod_d[l:l + 1, c0:c0 + 512], mr[:], [mrb], [mod_b])
        self.dbg("mod", None, None, None, None)
        if self.stop == "mod":
            return self.finish([mod_b], out_d)

        idn = identb
        RM = self.Region(self, persist_end, self.sb_top)
        yT, yT_b = RM.alloc([128, 16, G], BF16, "yT")
        xn_g = self.sb(persist_end, [128, GT, D], BF16, "xn_g")
        cqnT, cqnT_b = RM.alloc([128, 4, G], BF16, "cqnT")
        cosF_g, ropeF_b = RM.alloc([64, G], F32, "cosF_g")
        sinF_g, _ = RM.alloc([64, G], F32, "sinF_g")
        cosR_g, ropeR_b = RM.alloc([128, GT, 64], F32, "cosR_g")
        sinR_g, _ = RM.alloc([128, GT, 64], F32, "sinR_g")
        nsinR_g, _ = RM.alloc([128, GT, 64], F32, "nsinR_g")
        ov0 = RM.cur
        RA = self.Region(self, ov0, self.sb_top)
        hT, hT_b = RA.alloc([128, 16, G], BF16, "hT")
        xt, xt_b = RA.alloc([128, D], F32, "xt")
        st = [RA.alloc([128, 1], F32, f"st{i}") for i in range(12)]
        junk, junk_b = RA.alloc([128, 512], BF16, "junk")
        cqn_g, cqn_b = RA.alloc([128, GT, 512], BF16, "cqn_g")
        ckvn_g, ckvn_b = RA.alloc([128, GT, 256], BF16, "ckvn_g")
        ckvnT, ckvnT_b = RA.alloc([128, 2, G], BF16, "ckvnT")
        kperot, kperot_b = RA.alloc([128, 16, 64], BF16, "kperot")
        kt1, kt1_b = RA.alloc([64, G], F32, "kt1")
        kt2, kt2_b = RA.alloc([64, G], F32, "kt2")
        kpeo, kpeo_b = RA.alloc([64, G], BF16, "kpeo")
        wukv, wukv_b = RA.alloc([128, 2, 2048], BF16, "wukv")
        kTo = [RA.alloc([128, G], BF16, f"kTo{i}") for i in range(2)]
        vo = [RA.alloc([128, 1024], BF16, f"vo{i}") for i in range(2)]
        q_r, q_r_b = RA.alloc([128, GT, 512], BF16, "q_r")
        k_p, k_p_b = RA.alloc([128, GT, 512], BF16, "k_p")
        v_r, v_r_b = RA.alloc([128, GT, 512], BF16, "v_r")
        g_r, g_r_b = RA.alloc([128, GT, 512], F32, "g_r")
        rt1 = [RA.alloc([128, 512], F32, f"rt1_{i}") for i in range(2)]
        rt2 = [RA.alloc([128, 512], F32, f"rt2_{i}") for i in range(1)] * 2
        rt3 = [RA.alloc([128, 512], F32, f"rt3_{i}") for i in range(1)] * 2
        qT4, qT4_b = RA.alloc([128, 4, 128], BF16, "qT4")
        kT4, kT4_b = RA.alloc([128, 4, 128], BF16, "kT4")
        sT4, sT4_b = RA.alloc([128, 4, 128], BF16, "sT4")
        Sf, Sf_b = RA.alloc([128, 4, 128], F32, "Sf")
        Sb, Sb_b = RA.alloc([128, 4, 128], BF16, "Sb")
        ysq, ysq_b = RA.alloc([128, 4, 128], F32, "ysq")
        yc, yc_b = RA.alloc([128, 4, 128], F32, "yc")
        yg, yg_b = RA.alloc([128, 512], BF16, "yg")
        s4 = [RA.alloc([128, 4], F32, f"s4_{i}") for i in range(8)]
        RB = self.Region(self, ov0, self.sb_top)
        stash, stash_b = RB.alloc([128, S], F32, "stash")
        kTh, kTh_b = RB.alloc([128, S], BF16, "kTh")
        vh, vh_b = RB.alloc([128, NT, 128], BF16, "vh")
        kpeT, kpeT_b = RB.alloc([64, S], BF16, "kpeT")
        wuq, wuq_b = RB.alloc([128, 4, 1536], BF16, "wuq")
        wrot, wrot_b = RB.alloc([128, 4, 8, 64], BF16, "wrot")
        qTh, qTh_b = RB.alloc([128, G], BF16, "qTh")
        qpeTh, qpeTh_b = RB.alloc([64, G], BF16, "qpeTh")
        qt1, qt1_b = RB.alloc([64, G], F32, "qt1")
        qt2, qt2_b = RB.alloc([64, G], F32, "qt2")
        Pb = [RB.alloc([128, 512], BF16, f"Pb{i}") for i in range(2)]
        PT = [RB.alloc([128, 4, 128], BF16, f"PT{i}") for i in range(2)]
        ytok, ytok_b = RB.alloc([128, GT, 1024], BF16, "ytok")
        bst = [RB.alloc([128, 1], F32, f"bst{i}") for i in range(8)]
        rs8 = [RB.alloc([128, 8], F32, f"rs8_{i}") for i in range(2)]
        rs8b = [RB.alloc([128, 8], F32, f"rs8b_{i}") for i in range(2)]
        stash2, stash2_b = RB.alloc([128, S], F32, "stash2")
        stashes = [(stash, stash_b), (stash2, stash2_b)]
        RCo = self.Region(self, ov0, self.sb_top)
        xc = [RCo.alloc([128, 512], F32, f"xc{i}") for i in range(2)]
        xtmp = [RCo.alloc([128, 512], F32, f"xtmp{i}") for i in range(2)]
        xo = [RCo.alloc([128, 512], F32, f"xo{i}") for i in range(2)]
        RD = self.Region(self, persist_end, self.sb_top)
        h2T, h2T_b = RD.alloc([128, 16, MT * 128], BF16, "h2T")
        gates, gates_b = RD.alloc([128, MT, 32], F32, "gates")
        sa = [RD.alloc([128, 512], F32, f"sa{i}") for i in range(2)]
        hid = [RD.alloc([128, 512], BF16, f"hid{i}") for i in range(2)]
        hidT, hidT_b0 = RD.alloc([128, 4, MT * 128], BF16, "hidT")
        hidT_bs = [hidT_b0] + [Buf(f"hidT{i}") for i in range(1, MT)]
        RD.bufs.extend(hidT_bs[1:])
        dst = [RD.alloc([128, 1], F32, f"dst{i}") for i in range(16)]
        lg, lg_b = RD.alloc([128, 36], F32, "lg")
        rk = [RD.alloc([128, 32], F32, f"rk{i}") for i in range(6)]
        xc2 = [RD.alloc([128, 512], F32, f"xc2_{i}") for i in range(2)]
        xc3 = [RD.alloc([128, 512], F32, f"xc3_{i}") for i in range(2)]
        acc_off = RD.cur
        acc, acc_b = RD.alloc([128, MT, D], F32, "acc")
        RD2 = self.Region(self, acc_off, self.sb_top)
        xt2, xt2_b = RD2.alloc([128, D], F32, "xt2")
        xn2, xn2_b = RD2.alloc([128, D], F32, "xn2")
        h2f, h2f_b = RD2.alloc([128, 16, 128], F32, "h2f")
        xt2b, xt2b_b = RD2.alloc([128, D], F32, "xt2b")
        lgall, lgall_b = RD2.alloc([128, MT, 36], F32, "lgall")
        tk8 = [RD2.alloc([128, MT], F32, f"tk8_{i}")[0] for i in range(7)]
        tkg = [RD2.alloc([128, MT, 4], F32, f"tkg_{i}")[0] for i in range(2)]
        tke = [RD2.alloc([128, MT, 32], F32, f"tke_{i}")[0] for i in range(4)]
        if self.debug:
            print("SBUF map: persist_end", persist_end, "ov0", ov0, "A", RA.cur, "B", RB.cur, "C", RCo.cur, "D", RD.cur, "D2", RD2.cur)

        def rmsnorm_stats(src_ap, n, junk_ap, r, junk_w, sti):
            (ssq, ssq_b), (std, std_b), (rstd, rstd_b) = st[sti], st[sti + 1], st[sti + 2]
            self.ACT(junk_ap, src_ap, AF.Square, r, junk_w + [ssq_b], accum_out=ssq[:])
            self.ACT(std[:], ssq[:], AF.Sqrt, [ssq_b], [std_b], scale=1.0 / n, bias=EPS)
            self.A("dve", lambda e: e.reciprocal(out=rstd[:], in_=std[:]), [std_b], [rstd_b])
            return rstd, rstd_b

        alt = [0]

        def evac(out, in_, r, w):
            alt[0] ^= 1
            if alt[0]:
                return self.ACT(out, in_, AF.Copy, r, w)
            return self.CP("dve", out, in_, r, w)

        cur_overlay = [list(RC2.bufs) + [tb]]

        def switch(new_bufs):
            self.barrier(cur_overlay[0], new_bufs)
            cur_overlay[0] = list(new_bufs)

        final_ops = []
        for l in range(self.n_layers):
            last_layer = (l == self.n_layers - 1)
            RS_b = Buf("rows")
            switch([RS_b])
            rows = self.sb(ov0, [128, 128], F32, "rows")
            self.MS("pool", rows[:], 0.0, [RS_b])
            srcs = [(mod_d[l, 0:D], 16, [mod_b]), (mod_d[l, D:2 * D], 16, [mod_b]), (mod_d[l, 3 * D:4 * D], 16, [mod_b]),
                    (mod_d[l, 4 * D:5 * D], 16, [mod_b]), (norm1_g[l], 16, [self.wsrc]), (norm2_g[l], 16, [self.wsrc]),
                    (q_norm_g[l], 4, [self.wsrc]), (kv_norm_g[l], 2, [self.wsrc])]
            r0 = 0
            for (src, n, rb_) in srcs:
                self.DMA("sp", rows[r0:r0 + n, :], src.rearrange("(k c) -> k c", c=128), rb_, [RS_b])
                r0 += n
            self.TR(ps[0][:, 0:128], rows[:, :], identf[:], [RS_b, cst], [psb[0]])
            self.CP("dve", vecs[:, 0:104], ps[0][:, 0:104], [psb[0]], [vecs_b])
            sh1c, sh2c, qng, kvng = vecs[:, 0:16], vecs[:, 32:48], vecs[:, 96:100], vecs[:, 100:102]
            self.STT("dve", sc1c[:], vecs[:, 16:32], 1.0, vecs[:, 64:80], ALU.add, ALU.mult, [vecs_b], [vecs_b])
            self.STT("dve", sc2c[:], vecs[:, 48:64], 1.0, vecs[:, 80:96], ALU.add, ALU.mult, [vecs_b], [vecs_b])
            self.DMA("sp", wr[:, :, 0:4], rgw[l].rearrange("(k p) g -> p k g", p=128), [self.wsrc], [wr_b])
            self.DMA("sp", wr[:, :, 4:36], rew[l].rearrange("(k p) g -> p k g", p=128), [self.wsrc], [wr_b])
            self.DMA("sp", rbias[:, 0:4], rgb[l:l + 1, :].partition_broadcast(128), [self.wsrc], [wr_b])
            self.DMA("sp", rbias[:, 4:36], reb[l:l + 1, :].partition_broadcast(128), [self.wsrc], [wr_b])
            self.MS("pool", Tst[:], 0.0, [Tst_b])
            xsrc = x_in if l == 0 else xres

            def moe_pass(mp, sel):
                switch(RD.bufs[:-1] + RD2.bufs)
                for t in range(MT):
                    tile = mp * MT + t
                    self.DMA("sp", xt2[:], xres[tile * 128:(tile + 1) * 128, :], [xres_b[tile]], [xt2_b])
                    if sel:
                        tileB = NT // 2 + tile
                        self.DMA("sp", xt2b[:], xres[tileB * 128:(tileB + 1) * 128, :], [xres_b[tileB]], [xt2b_b])
                        self.TS("pool", xt2b[:], xt2b[:], flg[:, 1:2], None, ALU.mult, None, [xt2b_b, cst], [xt2b_b])
                        self.STT("dve", xt2[:], xt2[:], flg[:, 0:1], xt2b[:], ALU.mult, ALU.add, [xt2_b, xt2b_b, cst], [xt2_b])
                    (ssq, ssq_b), (std, std_b), (rstd, rstd_b) = dst[0:3]
                    self.ACT(xn2[:], xt2[:], AF.Square, [xt2_b], [xn2_b, ssq_b], accum_out=ssq[:])
                    self.ACT(std[:], ssq[:], AF.Sqrt, [ssq_b], [std_b], scale=1.0 / D, bias=EPS)
                    self.A("dve", lambda e, rstd=rstd, std=std: e.reciprocal(out=rstd[:], in_=std[:]), [std_b], [rstd_b])
                    self.ACT(xn2[:], xt2[:], AF.Copy, [xt2_b, rstd_b], [xn2_b], scale=rstd[:])
                    for k in range(16):
                        bk = k // 4
                        self.TR(ps[bk][:, (k % 4) * 128:(k % 4 + 1) * 128], xn2[:, k * 128:(k + 1) * 128], identf[:], [xn2_b, cst], [psb[bk]])
                        if k % 2 == 0:
                            self.ACT(h2f[:, k, :], ps[bk][:, (k % 4) * 128:(k % 4 + 1) * 128], AF.Identity, [psb[bk], vecs_b], [h2f_b], scale=sc2c[:, k:k + 1], bias=sh2c[:, k:k + 1])
                        else:
                            self.TS("dve", h2f[:, k, :], ps[bk][:, (k % 4) * 128:(k % 4 + 1) * 128], sc2c[:, k:k + 1], sh2c[:, k:k + 1], ALU.mult, ALU.add, [psb[bk], vecs_b], [h2f_b])
                    self.CP("pool", h2T[:, :, t * 128:(t + 1) * 128], h2f[:], [h2f_b], [h2T_b])
                    for k in range(16):
                        self.MM(ps[4][:, 0:36], h2f[:, k, :], wr[:, k, :], k == 0, k == 15, [h2f_b, wr_b], [psb[4]])
                    self.TT("dve", lgall[:, t, :], ps[4][:, 0:36], rbias[:], ALU.add, [psb[4], wr_b], [lgall_b])
                tk = Buf("topk")
                tk.last_w = lgall_b.last_w
                g4 = lgall[:, :, 0:4]
                e32 = lgall[:, :, 4:36]
                self.A("dve", lambda e: e.reduce_max(out=tk8[0][:], in_=g4, axis=AX.X), [lgall_b], [tk])
                self.TT("dve", tkg[0][:], g4, tk8[0][:].unsqueeze(2).broadcast_to([128, MT, 4]), ALU.subtract, [lgall_b, tk], [tk])
                self.ACT(tkg[1][:], tkg[0][:], AF.Exp, [tk], [tk])
                self.A("dve", lambda e: e.reduce_sum(out=tk8[1][:], in_=tkg[1][:], axis=AX.X), [tk], [tk])
                self.A("dve", lambda e: e.reciprocal(out=tk8[1][:], in_=tk8[1][:]), [tk], [tk])
                self.TS("dve", tkg[1][:], tkg[0][:], 0.0, None, ALU.is_ge, None, [tk], [tk])
                self.TS("dve", tkg[1][:], tkg[1][:], -1.0, 1e30, ALU.add, ALU.mult, [tk], [tk])
                self.TT("dve", tke[0][:].rearrange("p t (g e) -> p t g e", g=4), e32.rearrange("p t (g e) -> p t g e", g=4),
                        tkg[1][:].unsqueeze(3).broadcast_to([128, MT, 4, 8]), ALU.add, [lgall_b, tk], [tk])
                self.A("dve", lambda e: e.reduce_max(out=tk8[2][:], in_=tke[0][:], axis=AX.X), [tk], [tk])
                self.TT("dve", tke[1][:], tke[0][:], tk8[2][:].unsqueeze(2).broadcast_to([128, MT, 32]), ALU.subtract, [tk], [tk])
                self.TS("dve", tke[1][:], tke[1][:], 0.0, None, ALU.is_ge, None, [tk], [tk])
                self.STT("dve", tke[2][:], tke[1][:], -1e30, tke[0][:], ALU.mult, ALU.add, [tk], [tk])
                self.A("dve", lambda e: e.reduce_max(out=tk8[3][:], in_=tke[2][:], axis=AX.X), [tk], [tk])
                self.TT("dve", tke[3][:], tke[2][:], tk8[3][:].unsqueeze(2).broadcast_to([128, MT, 32]), ALU.subtract, [tk], [tk])
                self.TS("dve", tke[3][:], tke[3][:], 0.0, None, ALU.is_ge, None, [tk], [tk])
                self.TT("dve", tk8[4][:], tk8[3][:], tk8[2][:], ALU.subtract, [tk], [tk])
                self.ACT(tk8[4][:], tk8[4][:], AF.Exp, [tk], [tk])
                self.TS("dve", tk8[4][:], tk8[4][:], 1.0, None, ALU.add, None, [tk], [tk])
                self.A("dve", lambda e: e.reciprocal(out=tk8[4][:], in_=tk8[4][:]), [tk], [tk])
                self.TT("dve", tk8[5][:], tk8[4][:], tk8[1][:], ALU.mult, [tk], [tk])
                self.TT("dve", tk8[6][:], tk8[1][:], tk8[5][:], ALU.subtract, [tk], [tk])
                self.TT("dve", tke[1][:], tke[1][:], tk8[5][:].unsqueeze(2).broadcast_to([128, MT, 32]), ALU.mult, [tk], [tk])
                self.TT("dve", tke[3][:], tke[3][:], tk8[6][:].unsqueeze(2).broadcast_to([128, MT, 32]), ALU.mult, [tk], [tk])
                self.TT("dve", gates[:], tke[1][:], tke[3][:], ALU.add, [tk], [gates_b, tk])
                if l == 0 and mp == 0:
                    self.dbg("gates", gates[:], [128, MT, 32], F32, [gates_b])
                    self.dbg("h2T", h2T[:], [128, 16, MT * 128], BF16, [h2T_b])
                if self.stop == "router":
                    self.finish([gates_b, h2T_b], out_d)
                    return "stop"
                self.barrier(RD2.bufs, [acc_b])
                cur_overlay[0] = list(RD.bufs)

                def load_wg(e):
                    return self.ring_load(w_gate[l, e].rearrange("(k p) f -> p k f", p=128), slot16)

                def load_wu(e):
                    return self.ring_load(w_up[l, e].rearrange("(k p) f -> p k f", p=128), slot16)

                def load_wd(e):
                    return self.ring_load(w_down[l, e].rearrange("(k p) d -> p k d", p=128), lambda tt: tt[:].rearrange("p (k d) -> p k d", k=4))

                nxt = [load_wg(0), load_wu(0), load_wd(0)]
                for e in range(NE):
                    (wg, wgb), (wu, wub), (wd, wdb) = nxt
                    if e + 1 < NE:
                        nxt = [load_wg(e + 1), None, None]

                    def au_mm(t):
                        pa, pu = (0, 1) if t % 2 == 0 else (2, 3)
                        for k in range(16):
                            self.MM(ps[pa][:], h2T[:, k, t * 128:(t + 1) * 128], wg[:, k, :], k == 0, k == 15, [h2T_b, wgb], [psb[pa]])
                        for k in range(16):
                            self.MM(ps[pu][:], h2T[:, k, t * 128:(t + 1) * 128], wu[:, k, :], k == 0, k == 15, [h2T_b, wub], [psb[pu]])
                        sa_, sab = sa[t % 2]
                        hd_, hdb = hid[t % 2]
                        self.ACT(sa_[:], ps[pa][:], AF.Silu, [psb[pa]], [sab])
                        self.STT("dve", hd_[:], ps[pu][:], gates[:, t, e:e + 1], sa_[:], ALU.mult, ALU.mult, [psb[pu], gates_b, sab], [hdb])

                    def au_tr(t):
                        hd_, hdb = hid[t % 2]
                        bk = 4 + (t % 2)
                        for i in range(4):
                            self.TR(psbf(bk)[:, i * 128:(i + 1) * 128], hd_[:, i * 128:(i + 1) * 128], idn[:], [hdb, cst], [psb[bk]])
                        self.ACT(hidT[:, :, t * 128:(t + 1) * 128], psbf(bk)[:, 0:512].rearrange("p (i n) -> p i n", i=4), AF.Copy, [psb[bk]], [hidT_bs[t]])

                    for t in range(MT):
                        au_mm(t)
                        if t >= 1:
                            au_tr(t - 1)
                    au_tr(MT - 1)
                    if e + 1 < NE:
                        nxt[1] = load_wu(e + 1)
                        nxt[2] = load_wd(e + 1)
                    for t in range(MT):
                        b0 = 0 if t % 2 == 0 else 4
                        for dbk in range(4):
                            for fc in range(4):
                                self.MM(ps[b0 + dbk][:], hidT[:, fc, t * 128:(t + 1) * 128], wd[:, fc, dbk * 512:(dbk + 1) * 512], fc == 0, fc == 3, [hidT_bs[t], wdb], [psb[b0 + dbk]])
                        for dbk in range(4):
                            dst_ap = acc[:, t, dbk * 512:(dbk + 1) * 512]
                            if e == 0:
                                self.CP("dve", dst_ap, ps[b0 + dbk][:], [psb[b0 + dbk]], [acc_b])
                            else:
                                self.TT("dve", dst_ap, ps[b0 + dbk][:], dst_ap, ALU.add, [psb[b0 + dbk], acc_b], [acc_b])
                self.DMA("sp", gb[:], mod_d[l:l + 1, 5 * D:6 * D].partition_broadcast(128), [mod_b], [gb_b])
                n7 = 0
                for t in range(MT):
                    tile = mp * MT + t
                    for cbk in range(4):
                        xc_, xcb = xc2[n7 % 2]
                        xd_, xdb = xc3[n7 % 2]
                        n7 += 1
                        cs = slice(cbk * 512, (cbk + 1) * 512)
                        self.DMA("sp", xc_[:], xres[tile * 128:(tile + 1) * 128, cs], [xres_b[tile]], [xcb])
                        if sel:
                            tileB = NT // 2 + tile
                            self.DMA("sp", xd_[:], xres[tileB * 128:(tileB + 1) * 128, cs], [xres_b[tileB]], [xdb])
                            self.TS("dve", xd_[:], xd_[:], flg[:, 1:2], None, ALU.mult, None, [xdb, cst], [xdb])
                            self.STT("dve", xc_[:], xc_[:], flg[:, 0:1], xd_[:], ALU.mult, ALU.add, [xcb, xdb, cst], [xcb])
                        self.TT("pool", acc[:, t, cs], acc[:, t, cs], gb[:, cs], ALU.mult, [acc_b, gb_b], [acc_b])
                        self.TT("dve", xc_[:], xc_[:], acc[:, t, cs], ALU.add, [xcb, acc_b], [xcb])
                        if sel:
                            self.DMA("sp", xsel_d[tile * 128:(tile + 1) * 128, cs], xc_[:], [xcb], [xsel_b[tile]])
                        else:
                            self.DMA("sp", xres[tile * 128:(tile + 1) * 128, cs], xc_[:], [xcb], [xres_b[tile]])
                if self.stop == "moe":
                    if sel:
                        self.dbg("xout", xsel_d[0:MT * 128, :], [MT * 128, D], F32, xsel_b[0:MT])
                        self.finish(xsel_b[mp * MT:(mp + 1) * MT], out_d)
                    else:
                        self.dbg("xout", xres[0:MT * 128, :], [MT * 128, D], F32, xres_b[0:MT])
                        self.finish(xres_b[mp * MT:(mp + 1) * MT], out_d)
                    return "stop"
                return None


            for g in range(NG):
                switch(RA.bufs + [yT_b, cqnT_b, ropeF_b, ropeR_b])

                def slot16(tt):
                    return tt[:].rearrange("p (k c) -> p k c", k=16)

                self.wplan = []
                pl_cq = self.plan_load(w_in[l, :, 0:512].rearrange("(k p) c -> p k c", p=128), slot16)
                pl_ckv = self.plan_load(w_in[l, :, 512:832].rearrange("(k p) c -> p k c", p=128),
                                        lambda tt: tt[:, 0:16 * 320].rearrange("p (k c) -> p k c", k=16))
                pl_ret = {}
                for hh_ in range(2):
                    for kind_, cbase_ in (("q", 832), ("k", 1856), ("v", 2880), ("g", 3904)):
                        c0_ = cbase_ + hh_ * 512
                        pl_ret[(hh_, kind_)] = self.plan_load(w_in[l, :, c0_:c0_ + 512].rearrange("(k p) c -> p k c", p=128), slot16)
                pl_wo = [self.plan_load(w_o[l, :, cb_ * 512:(cb_ + 1) * 512].rearrange("(k p) c -> p k c", p=128), slot16) for cb_ in range(4)]
                self.DMA("sp", cosF_g[:], cosF_d[:, g * G:(g + 1) * G], [tab_b], [ropeF_b])
                self.DMA("sp", sinF_g[:], sinF_d[:, g * G:(g + 1) * G], [tab_b], [ropeF_b])
                self.DMA("sp", cosR_g[:], cosR_d[:, g * GT:(g + 1) * GT, :], [tab_b], [ropeR_b])
                self.DMA("sp", sinR_g[:], sinR_d[:, g * GT:(g + 1) * GT, :], [tab_b], [ropeR_b])
                self.TS("dve", nsinR_g[:], sinR_g[:], -1.0, None, ALU.mult, None, [ropeR_b], [ropeR_b])
                self.DMA("pool", wukv[:], w_ukv[l].rearrange("(k p) c -> p k c", p=128), [self.wsrc], [wukv_b])
                for t in range(GT):
                    tile = g * GT + t
                    rsrc = [self.wsrc] if l == 0 else [xres_b[tile]]
                    self.DMA("sp", xt[:], xsrc[tile * 128:(tile + 1) * 128, :], rsrc, [xt_b])
                    rstd, rstd_b = rmsnorm_stats(xt[:], D, xn_g[:, t, :], [xt_b], [yT_b], 0)
                    self.ACT(xn_g[:, t, :], xt[:], AF.Copy, [xt_b, rstd_b], [yT_b], scale=rstd[:])
                for k in range(16):
                    bk = 2 + (k % 2)
                    for t in range(GT):
                        self.TR(psbf(bk)[:, t * 128:(t + 1) * 128], xn_g[:, t, k * 128:(k + 1) * 128], idn[:], [yT_b, cst], [psb[bk]])
                    if k % 2 == 0:
                        self.ACT(hT[:, k, :], psbf(bk)[:, 0:G], AF.Identity, [psb[bk], vecs_b], [hT_b], scale=sc1c[:, k:k + 1], bias=sh1c[:, k:k + 1])
                    else:
                        self.TS("dve", hT[:, k, :], psbf(bk)[:, 0:G], sc1c[:, k:k + 1], sh1c[:, k:k + 1], ALU.mult, ALU.add, [psb[bk], vecs_b], [hT_b])
                if l == 0 and g == 0:
                    self.dbg("hT", hT[:], [128, 16, G], BF16, [hT_b])
                if self.stop == "hT":
                    return self.finish([hT_b], out_d)

                wv, wb_ = self.get_load(pl_cq)
                for t in range(GT):
                    pi = t % 2
                    for k in range(16):
                        self.MM(ps[pi][:], hT[:, k, t * 128:(t + 1) * 128], wv[:, k, :], k == 0, k == 15, [hT_b, wb_], [psb[pi]])
                    rstd, rstd_b = rmsnorm_stats(ps[pi][:], 512, junk[:], [psb[pi]], [junk_b], 3 * (t % 2))
                    self.ACT(cqn_g[:, t, :], ps[pi][:], AF.Copy, [psb[pi], rstd_b], [cqn_b], scale=rstd[:])
                for kc in range(4):
                    bk = 2 + (kc % 2)
                    for t in range(GT):
                        self.TR(psbf(bk)[:, t * 128:(t + 1) * 128], cqn_g[:, t, kc * 128:(kc + 1) * 128], idn[:], [cqn_b, cst], [psb[bk]])
                    self.TS("dve", cqnT[:, kc, :], psbf(bk)[:, 0:G], qng[:, kc:kc + 1], None, ALU.mult, None, [psb[bk], vecs_b], [cqnT_b])
                wv, wb_ = self.get_load(pl_ckv)
                for t in range(GT):
                    pi = t % 2
                    for k in range(16):
                        self.MM(ps[pi][:, 0:256], hT[:, k, t * 128:(t + 1) * 128], wv[:, k, 0:256], k == 0, k == 15, [hT_b, wb_], [psb[pi]])
                    rstd, rstd_b = rmsnorm_stats(ps[pi][:, 0:256], 256, junk[:, 0:256], [psb[pi]], [junk_b], 3 * (t % 2))
                    self.ACT(ckvn_g[:, t, :], ps[pi][:, 0:256], AF.Copy, [psb[pi], rstd_b], [ckvn_b], scale=rstd[:])
                for kc in range(2):
                    bk = 2 + (kc % 2)
                    for t in range(GT):
                        self.TR(psbf(bk)[:, t * 128:(t + 1) * 128], ckvn_g[:, t, kc * 128:(kc + 1) * 128], idn[:], [ckvn_b, cst], [psb[bk]])
                    self.TS("dve", ckvnT[:, kc, :], psbf(bk)[:, 0:G], kvng[:, kc:kc + 1], None, ALU.mult, None, [psb[bk], vecs_b], [ckvnT_b])
                self.TS("dve", kperot[:, :, 0:32], wv[:, :, 288:320], -1.0, None, ALU.mult, None, [wb_], [kperot_b])
                self.CP("dve", kperot[:, :, 32:64], wv[:, :, 256:288], [wb_], [kperot_b])
                for k in range(16):
                    self.MM(ps[0][0:64, :], wv[:, k, 256:320], hT[:, k, :], k == 0, k == 15, [hT_b, wb_], [psb[0]])
                for k in range(16):
                    self.MM(ps[1][0:64, :], kperot[:, k, :], hT[:, k, :], k == 0, k == 15, [hT_b, kperot_b], [psb[1]])
                self.TT("dve", kt1[:], ps[0][0:64, :], cosF_g[:], ALU.mult, [psb[0], ropeF_b], [kt1_b])
                self.TT("dve", kt2[:], ps[1][0:64, :], sinF_g[:], ALU.mult, [psb[1], ropeF_b], [kt2_b])
                self.TT("dve", kpeo[:], kt1[:], kt2[:], ALU.add, [kt1_b, kt2_b], [kpeo_b])
                self.DMA("sp", kpe_d[:, g * G:(g + 1) * G], kpeo[:], [kpeo_b], [kv_b[g]])
                for h in range(8):
                    pi = h % 2
                    for kc in range(2):
                        self.MM(ps[pi][:], wukv[:, kc, h * 256:h * 256 + 128], ckvnT[:, kc, :], kc == 0, kc == 1, [wukv_b, ckvnT_b], [psb[pi]])
                    kt_, ktb_ = kTo[h % 2]
                    evac(kt_[:], ps[pi][:], [psb[pi]], [ktb_])
                    self.DMA("sp", kT_d[h, :, g * G:(g + 1) * G], kt_[:], [ktb_], [kv_b[g]])
                wukv_v = wukv[:].rearrange("p k (h c) -> p k h c", h=8)
                for t in range(GT):
                    tile = g * GT + t
                    vt_, vtb_ = vo[t % 2]
                    for hf in range(2):
                        pi = hf
                        for kc in range(2):
                            self.MM(ps[pi][:].rearrange("p (h c) -> p h c", h=4), ckvnT[:, kc, t * 128:(t + 1) * 128],
                                    wukv_v[:, kc, 4 * hf:4 * hf + 4, 128:256], kc == 0, kc == 1, [wukv_b, ckvnT_b], [psb[pi]])
                        evac(vt_[:, hf * 512:(hf + 1) * 512], ps[pi][:], [psb[pi]], [vtb_])
                    self.DMA("sp", v_d[:, :, tile, :].rearrange("h p d -> p h d"), vt_[:].rearrange("p (h d) -> p h d", h=8), [vtb_], [kv_b[g]])
                if l == 0 and g == 0:
                    self.dbg("cqnT", cqnT[:], [128, 4, G], BF16, [cqnT_b])
                    self.dbg("ckvnT", ckvnT[:], [128, 2, G], BF16, [ckvnT_b])
                if self.stop == "kv":
                    return self.finish([kv_b[g], cqnT_b], out_d)

                for hh in range(2):
                    for kind, cbase in (("q", 832), ("k", 1856), ("v", 2880), ("g", 3904)):
                        wv, wb_ = self.get_load(pl_ret[(hh, kind)])
                        for t in range(GT):
                            pi = t % 2
                            for k in range(16):
                                self.MM(ps[pi][:], hT[:, k, t * 128:(t + 1) * 128], wv[:, k, :], k == 0, k == 15, [hT_b, wb_], [psb[pi]])
                            if kind in ("q", "k"):
                                (a1, a1b), (a2, a2b), (a3, a3b) = rt1[t % 2], rt2[t % 2], rt3[t % 2]
                                psv = ps[pi][:].rearrange("p (h two j) -> p h two j", h=4, two=2)
                                a1v = a1[:].rearrange("p (h two j) -> p h two j", h=4, two=2)
                                a2v = a2[:].rearrange("p (h two j) -> p h two j", h=4, two=2)
                                cosb = cosR_g[:, t, :].unsqueeze(1).unsqueeze(1).broadcast_to([128, 4, 2, 64])
                                sinb = sinR_g[:, t, :].unsqueeze(1).broadcast_to([128, 4, 64])
                                nsinb = nsinR_g[:, t, :].unsqueeze(1).broadcast_to([128, 4, 64])
                                self.TT("dve", a1v, psv, cosb, ALU.mult, [psb[pi], ropeR_b], [a1b])
                                self.TT("dve", a2v[:, :, 0, :], psv[:, :, 1, :], nsinb, ALU.mult, [psb[pi], ropeR_b], [a2b])
                                self.TT("dve", a2v[:, :, 1, :], psv[:, :, 0, :], sinb, ALU.mult, [psb[pi], ropeR_b], [a2b])
                                if kind == "q":
                                    self.TT("pool", q_r[:, t, :], a1[:], a2[:], ALU.add, [a1b, a2b], [q_r_b])
                                else:
                                    self.TT("pool", a3[:], a1[:], a2[:], ALU.add, [a1b, a2b], [a3b])
                                    self.TT("pool", k_p[:, t, :].rearrange("p (h d) -> p h d", h=4), a3[:].rearrange("p (h d) -> p h d", h=4),
                                            gk_col[:, 4 * hh:4 * hh + 4].unsqueeze(2).broadcast_to([128, 4, 128]), ALU.mult, [a3b, cst], [k_p_b])
                            elif kind == "v":
                                self.ACT(v_r[:, t, :], ps[pi][:], AF.Copy, [psb[pi]], [v_r_b])
                            else:
                                self.ACT(g_r[:, t, :], ps[pi][:], AF.Silu, [psb[pi]], [g_r_b])
                    if l == 0 and g == 0 and hh == 0:
                        self.dbg("q_r", q_r[:], [128, GT, 512], BF16, [q_r_b])
                        self.dbg("k_p", k_p[:], [128, GT, 512], BF16, [k_p_b])
                    if self.stop == "qk":
                        return self.finish([q_r_b, k_p_b, v_r_b, g_r_b], out_d)
                    for t in range(GT):
                        for j in range(4):
                            self.TR(psbf(4)[:, j * 128:(j + 1) * 128], q_r[:, t, j * 128:(j + 1) * 128], idn[:], [q_r_b, cst], [psb[4]])
                        for j in range(4):
                            self.TR(psbf(4)[:, 512 + j * 128:512 + (j + 1) * 128], k_p[:, t, j * 128:(j + 1) * 128], idn[:], [k_p_b, cst], [psb[4]])
                        self.ACT(ysq[:], psbf(4)[:, 0:512].rearrange("p (h n) -> p h n", h=4), AF.Copy, [psb[4]], [ysq_b])
                        self.TT("dve", qT4[:], ysq[:], Gq[:, 4 * hh:4 * hh + 4, :], ALU.mult, [ysq_b, cst], [qT4_b])
                        self.ACT(kT4[:], psbf(4)[:, 512:1024].rearrange("p (h n) -> p h n", h=4), AF.Copy, [psb[4]], [kT4_b])
                        if self.stop == "r1":
                            return self.finish([qT4_b, kT4_b], out_d)
                        for j in range(4):
                            self.MM(ps[5][:, j * 128:(j + 1) * 128], kT4[:, j, :], qT4[:, j, :], True, True, [kT4_b, qT4_b], [psb[5]])
                        self.TT("dve", sT4[:], ps[5][:].rearrange("p (h n) -> p h n", h=4), maskT[:].unsqueeze(1).broadcast_to([128, 4, 128]), ALU.mult, [psb[5], cst], [sT4_b])
                        self.TT("pool", Sf[:], Tst[:, 4 * hh:4 * hh + 4, :], g128[:, 4 * hh:4 * hh + 4].unsqueeze(2).broadcast_to([128, 4, 128]), ALU.mult, [Tst_b, cst], [Sf_b])
                        self.CP("pool", Sb[:], Sf[:], [Sf_b], [Sb_b])
                        if self.stop == "r2":
                            return self.finish([sT4_b, Sb_b], out_d)
                        for j in range(4):
                            self.MM(ps[6][:, j * 128:(j + 1) * 128], sT4[:, j, :], v_r[:, t, j * 128:(j + 1) * 128], True, False, [sT4_b, v_r_b], [psb[6]])
                            self.MM(ps[6][:, j * 128:(j + 1) * 128], qT4[:, j, :], Sb[:, j, :], False, True, [qT4_b, Sb_b], [psb[6]])
                        for j in range(4):
                            self.MM(ps[7][:, j * 128:(j + 1) * 128], k_p[:, t, j * 128:(j + 1) * 128], v_r[:, t, j * 128:(j + 1) * 128], True, True, [k_p_b, v_r_b], [psb[7]])
                        self.TT("dve", Tst[:, 4 * hh:4 * hh + 4, :], ps[7][:].rearrange("p (h n) -> p h n", h=4), Sf[:], ALU.add, [Sf_b, psb[7]], [Tst_b])
                        if self.stop == "r3":
                            return self.finish([Tst_b, psb[6]], out_d)
                        y3 = ps[6][:].rearrange("p (h n) -> p h n", h=4)
                        (s1, s1b), (s2, s2b), (mean, meanb), (msq, msqb), (var, varb), (sd, sdb), (rs_, rsb) = s4[0:7]
                        self.A("dve", lambda e, s1=s1, y3=y3: e.reduce_sum(out=s1[:], in_=y3, axis=AX.X), [psb[6]], [s1b])
                        self.ACT(ysq[:], y3, AF.Square, [psb[6]], [ysq_b])
                        self.A("dve", lambda e, s2=s2: e.reduce_sum(out=s2[:], in_=ysq[:], axis=AX.X), [ysq_b], [s2b])
                        self.TS("dve", mean[:], s1[:], 1.0 / 128, None, ALU.mult, None, [s1b], [meanb])
                        self.TT("dve", msq[:], mean[:], mean[:], ALU.mult, [meanb], [msqb])
                        self.STT("dve", var[:], s2[:], 1.0 / 128, msq[:], ALU.mult, ALU.subtract, [s2b, msqb], [varb])
                        self.ACT(sd[:], var[:], AF.Sqrt, [varb], [sdb], bias=EPS)
                        self.A("dve", lambda e, rs_=rs_, sd=sd: e.reciprocal(out=rs_[:], in_=sd[:]), [sdb], [rsb])
                        self.TT("dve", yc[:], y3, mean[:].unsqueeze(2).broadcast_to([128, 4, 128]), ALU.subtract, [psb[6], meanb], [yc_b])
                        self.TT("pool", yc[:], yc[:], rs_[:].unsqueeze(2).broadcast_to([128, 4, 128]), ALU.mult, [yc_b, rsb], [yc_b])
                        self.TT("pool", yg[:], yc[:].rearrange("p h n -> p (h n)"), g_r[:, t, :], ALU.mult, [yc_b, g_r_b], [yg_b])
                        if self.stop == "r4":
                            return self.finish([yg_b], out_d)
                        for j in range(4):
                            self.TR(psbf(3)[:, j * 128:(j + 1) * 128], yg[:, j * 128:(j + 1) * 128], idn[:], [yg_b, cst], [psb[3]])
                        self.ACT(yT[:, 8 + 4 * hh:12 + 4 * hh, t * 128:(t + 1) * 128], psbf(3)[:, 0:512].rearrange("p (h n) -> p h n", h=4), AF.Copy, [psb[3]], [yT_b])
                if l == 0 and g == 0:
                    self.dbg("yT_ret", yT[:, 8:16, :], [128, 8, G], BF16, [yT_b])
                if self.stop == "ret":
                    return self.finish([yT_b, kv_b[g]], out_d)

                switch(RB.bufs)
                nk = (g + 1) * G
                nkt = nk // 128
                SC = 192.0 ** -0.5
                self.DMA("pool", wuq[:], w_uq[l].rearrange("(k p) c -> p k c", p=128), [self.wsrc], [wuq_b])
                wuq_v = wuq[:].rearrange("p k (h c) -> p k h c", h=8)
                self.TS("dve", wrot[:, :, :, 0:32], wuq_v[:, :, :, 160:192], -1.0, None, ALU.mult, None, [wuq_b], [wrot_b])
                self.CP("dve", wrot[:, :, :, 32:64], wuq_v[:, :, :, 128:160], [wuq_b], [wrot_b])
                self.DMA("sp", kpeT[:, 0:nk], kpe_d[:, 0:nk], kv_b[:g + 1], [kpeT_b])
                for h in range(8):
                    self.DMA("sp", kTh[:, 0:nk], kT_d[h, :, 0:nk], kv_b[:g + 1], [kTh_b])
                    self.DMA("sp", vh[:, 0:nkt, :], v_d[h, :, 0:nkt, :], kv_b[:g + 1], [vh_b])
                    for kc in range(4):
                        self.MM(ps[0][:], wuq[:, kc, h * 192:h * 192 + 128], cqnT[:, kc, :], kc == 0, kc == 3, [wuq_b, cqnT_b], [psb[0]])
                    self.ACT(qTh[:], ps[0][:], AF.Copy, [psb[0]], [qTh_b])
                    for kc in range(4):
                        self.MM(ps[1][0:64, :], wuq[:, kc, h * 192 + 128:h * 192 + 192], cqnT[:, kc, :], kc == 0, kc == 3, [wuq_b, cqnT_b], [psb[1]])
                    for kc in range(4):
                        self.MM(ps[2][0:64, :], wrot[:, kc, h, :], cqnT[:, kc, :], kc == 0, kc == 3, [wrot_b, cqnT_b], [psb[2]])
                    self.TT("dve", qt1[:], ps[1][0:64, :], cosF_g[:], ALU.mult, [psb[1], ropeF_b], [qt1_b])
                    self.TT("dve", qt2[:], ps[2][0:64, :], sinF_g[:], ALU.mult, [psb[2], ropeF_b], [qt2_b])
                    self.TT("dve", qpeTh[:], qt1[:], qt2[:], ALU.add, [qt1_b, qt2_b], [qpeTh_b])
                    def tile_vars(t):
                        J = g * GT + t
                        nkeys = (J + 1) * 128
                        nblk = (nkeys + 511) // 512
                        return J, nkeys, nblk

                    def S_phase(t, h=h):
                        J, nkeys, nblk = tile_vars(t)
                        stash, stash_b = stashes[t % 2]
                        (mx, mxb), (nb, nbb), (rsum, rsumb), (rinv, rinvb) = bst[4 * (t % 2):4 * (t % 2) + 4]
                        bmx, bmxb = rs8b[t % 2]
                        for kb in range(nblk):
                            w = min(512, nkeys - kb * 512)
                            bk = 3 + (kb % 2)
                            self.MM(ps[bk][:, 0:w], qTh[:, t * 128:(t + 1) * 128], kTh[:, kb * 512:kb * 512 + w], True, False, [qTh_b, kTh_b], [psb[bk]])
                            self.MM(ps[bk][:, 0:w], qpeTh[:, t * 128:(t + 1) * 128], kpeT[:, kb * 512:kb * 512 + w], False, True, [qpeTh_b, kpeT_b], [psb[bk]])
                            if kb == nblk - 1:
                                if w > 128:
                                    evac(stash[:, kb * 512:kb * 512 + w - 128], ps[bk][:, 0:w - 128], [psb[bk]], [stash_b])
                                self.TT("dve", stash[:, nkeys - 128:nkeys], ps[bk][:, w - 128:w], maskD[:], ALU.add, [psb[bk], cst], [stash_b])
                                self.A("dve", lambda e, kb=kb, w=w, bmx=bmx: e.reduce_max(out=bmx[:, kb:kb + 1], in_=stash[:, kb * 512:kb * 512 + w], axis=AX.X), [stash_b], [bmxb])
                            else:
                                evac(stash[:, kb * 512:kb * 512 + w], ps[bk][:, 0:w], [psb[bk]], [stash_b])
                                self.A("dve", lambda e, kb=kb, bmx=bmx: e.reduce_max(out=bmx[:, kb:kb + 1], in_=stash[:, kb * 512:(kb + 1) * 512], axis=AX.X), [stash_b], [bmxb])
                        self.A("dve", lambda e, mx=mx, nblk=nblk, bmx=bmx: e.reduce_max(out=mx[:], in_=bmx[:, 0:nblk], axis=AX.X), [bmxb], [mxb])
                        self.TS("dve", nb[:], mx[:], -SC, None, ALU.mult, None, [mxb], [nbb])

                    def P_phase(t, h=h):
                        J, nkeys, nblk = tile_vars(t)
                        stash, stash_b = stashes[t % 2]
                        (mx, mxb), (nb, nbb), (rsum, rsumb), (rinv, rinvb) = bst[4 * (t % 2):4 * (t % 2) + 4]
                        rs_t, rs_tb = rs8[t % 2]
                        ob = 6 if t % 2 == 0 else 2
                        for kb in range(nblk):
                            w = min(512, nkeys - kb * 512)
                            pbt, pbb = Pb[kb % 2]
                            ptt, ptb = PT[kb % 2]
                            tbk = 5 if kb % 2 == 0 else 7
                            self.ACT(pbt[:, 0:w], stash[:, kb * 512:kb * 512 + w], AF.Exp, [stash_b, nbb], [pbb, rs_tb], bias=nb[:], scale=SC, accum_out=rs_t[:, kb:kb + 1])
                            nti = w // 128
                            for i in range(nti):
                                self.TR(psbf(tbk)[:, i * 128:(i + 1) * 128], pbt[:, i * 128:(i + 1) * 128], idn[:], [pbb, cst], [psb[tbk]])
                            if kb % 2 == 0:
                                self.CP("dve", ptt[:, 0:nti, :], psbf(tbk)[:, 0:nti * 128].rearrange("p (i n) -> p i n", i=nti), [psb[tbk]], [ptb])
                            else:
                                self.ACT(ptt[:, 0:nti, :], psbf(tbk)[:, 0:nti * 128].rearrange("p (i n) -> p i n", i=nti), AF.Copy, [psb[tbk]], [ptb])
                            for i in range(nti):
                                kt_i = kb * 4 + i
                                self.MM(ps[ob][:, 0:128], ptt[:, i, :], vh[:, kt_i, :], kt_i == 0, kt_i == J, [ptb, vh_b], [psb[ob]])
                        self.A("dve", lambda e, rsum=rsum, rs_t=rs_t, nblk=nblk: e.reduce_sum(out=rsum[:], in_=rs_t[:, 0:nblk], axis=AX.X), [rs_tb], [rsumb])
                        self.A("dve", lambda e, rinv=rinv, rsum=rsum: e.reciprocal(out=rinv[:], in_=rsum[:]), [rsumb], [rinvb])
                        self.ACT(ytok[:, t, h * 128:(h + 1) * 128], ps[ob][:, 0:128], AF.Copy, [psb[ob], rinvb], [ytok_b], scale=rinv[:])

                    S_phase(0)
                    for t in range(GT):
                        if t + 1 < GT:
                            S_phase(t + 1)
                        P_phase(t)
                for t in range(GT):
                    bk = 0 if t % 2 == 0 else 1
                    for h in range(8):
                        self.TR(psbf(bk)[:, h * 128:(h + 1) * 128], ytok[:, t, h * 128:(h + 1) * 128], idn[:], [ytok_b, cst], [psb[bk]])
                    evac(yT[:, 0:8, t * 128:(t + 1) * 128], psbf(bk)[:, 0:1024].rearrange("p (h n) -> p h n", h=8), [psb[bk]], [yT_b])
                if l == 0 and g == 0:
                    self.dbg("yT", yT[:], [128, 16, G], BF16, [yT_b])
                if self.stop == "attn" or (self.stop == "attn1" and g == 1):
                    return self.finish([yT_b], out_d)

                switch(RCo.bufs)
                self.DMA("sp", gb[:], mod_d[l:l + 1, 2 * D:3 * D].partition_broadcast(128), [mod_b], [gb_b])
                n6 = 0
                if self.stop == "w6a":
                    return self.finish([gb_b], out_d)
                for cbk in range(4):
                    wv, wb_ = self.get_load(pl_wo[cbk])
                    for t in range(GT):
                        tile = g * GT + t
                        pi = n6 % 2
                        (xc_, xcb), (xm_, xmb), (xo_, xob) = xc[pi], xtmp[pi], xo[pi]
                        n6 += 1
                        rsrc = [self.wsrc] if l == 0 else [xres_b[tile]]
                        self.DMA("sp", xc_[:], xsrc[tile * 128:(tile + 1) * 128, cbk * 512:(cbk + 1) * 512], rsrc, [xcb])
                        for k in range(16):
                            self.MM(ps[pi][:], yT[:, k, t * 128:(t + 1) * 128], wv[:, k, :], k == 0, k == 15, [yT_b, wb_], [psb[pi]])
                        self.TT("dve", xm_[:], ps[pi][:], gb[:, cbk * 512:(cbk + 1) * 512], ALU.mult, [psb[pi], gb_b], [xmb])
                        self.TT("pool", xo_[:], xm_[:], xc_[:], ALU.add, [xmb, xcb], [xob])
                        if self.stop == "w6b":
                            return self.finish([xob], out_d)
                        self.DMA("sp", xres[tile * 128:(tile + 1) * 128, cbk * 512:(cbk + 1) * 512], xo_[:], [xob], [xres_b[tile]])
                if self.stop == "wo":
                    self.dbg("xmid", xres[0:G, :], [G, D], F32, xres_b[0:GT])
                    return self.finish(xres_b[g * GT:(g + 1) * GT], out_d)
                if self.stop == "wo_all" and g == NG - 1:
                    return self.finish(xres_b, out_d)

                if last_layer or (g + 1) % (MT // GT) != 0:
                    continue
                if moe_pass((g + 1) // (MT // GT) - 1, False) == "stop":
                    return self
            if last_layer:
                for mp in range(NMP // 2):
                    if moe_pass(mp, True) == "stop":
                        return self

        RF_b = [Buf("fin_x"), Buf("fin_o")]
        switch(RF_b + [gb_b])
        fx = [self.sb(persist_end + i * 8192, [128, D], F32, f"fx{i}") for i in range(2)]
        fo = [self.sb(persist_end + 16384 + i * 8192, [128, D], F32, f"fo{i}") for i in range(2)]
        fxb = [Buf(), Buf()]
        fob = [Buf(), Buf()]
        fst = [(self.sb(persist_end + 40000 + i * 64, [128, 1], F32, f"fst{i}"), Buf()) for i in range(6)]
        for b in fxb + fob + [x[1] for x in fst]:
            b.last_w = RF_b[0].last_w
        self.DMA("sp", gb[:], fng.rearrange("(o d) -> o d", o=1).partition_broadcast(128), [self.wsrc], [gb_b])
        outs = []
        for tile in range(NT // 2):
            i = tile % 2
            self.DMA("sp", fx[i][:], xsel_d[tile * 128:(tile + 1) * 128, :], [xsel_b[tile]], [fxb[i]])
            (ssq, ssq_b), (std, std_b), (rstd, rstd_b) = fst[3 * i:3 * i + 3]
            self.ACT(fo[i][:], fx[i][:], AF.Square, [fxb[i]], [fob[i], ssq_b], accum_out=ssq[:])
            self.ACT(std[:], ssq[:], AF.Sqrt, [ssq_b], [std_b], scale=1.0 / D, bias=EPS)
            self.A("dve", lambda e, rstd=rstd, std=std: e.reciprocal(out=rstd[:], in_=std[:]), [std_b], [rstd_b])
            self.STT("dve", fo[i][:], fx[i][:], rstd[:], gb[:], ALU.mult, ALU.mult, [fxb[i], rstd_b, gb_b], [fob[i]])
            outs.append(self.DMA("sp", out_d[tile * 128:(tile + 1) * 128, :], fo[i][:], [fob[i]], [Buf()]))
        self.P.emit(final_wait_ops=outs + self.dbg_ops)
        return self

    def finish(self, bufs, out_d):
        t = self.sb(self.sb_top - 4096, [128, 8], F32, "fin")
        b = Buf()
        self.MS("pool", t[:], 0.0, [b])
        op = self.DMA("sp", out_d[0:128, 0:8], t[:], [b] + list(bufs), [Buf()])
        self.P.emit(final_wait_ops=[op] + self.dbg_ops)
        return self


def _core_role(cidx):
    die, idx = cidx // 4, cidx % 4
    return die * 2 + idx % 2, idx // 2


def _in_maps(inputs, n_cores=8):
    shared = {k: np.ascontiguousarray(np.asarray(v, dtype=np.float32)) for k, v in inputs.items() if k not in ("x", "c")}
    maps = []
    for cidx in range(n_cores):
        b, half = _core_role(cidx)
        m = dict(shared)
        xb = np.asarray(inputs["x"][b], dtype=np.float32)
        if half == 0:
            xs = np.zeros_like(xb)
            xs[:S // 2] = xb[:S // 2]
            xb = xs
        m["x"] = np.ascontiguousarray(xb)
        m["c_col"] = np.ascontiguousarray(np.asarray(inputs["c"][b], dtype=np.float32).reshape(16, 128).T)
        fl = np.zeros((128, 2), np.float32)
        fl[:, half] = 1.0
        m["flag"] = fl
        maps.append(m)
    return maps


def kernel(**inputs):
    bld = Builder()
    maps = _in_maps(inputs)
    res = run_bass_kernel_spmd(bld.nc, maps, core_ids=list(range(8)))
    out = np.zeros((4, S, D), np.float32)
    for cidx in range(8):
        b, half = _core_role(cidx)
        out[b, half * (S // 2):(half + 1) * (S // 2)] = np.asarray(res.results[cidx]["out"], dtype=np.float32)
    return out
```
